# Optimizing a Trainium2 kernel written in Bass

```python
import jax, jax.numpy as jnp
from jax import lax
import numpy as np

D_MODEL = 1024
BATCH = 8
SEQ = 4096
DEPTH = 1

ATTN_HEADS = 8
ATTN_HEAD_DIM = 64
RET_HEADS = 8
RET_KEY_DIM = 64
RET_VALUE_DIM = 64
ATTN_WIDTH = ATTN_HEADS * ATTN_HEAD_DIM
RET_KEY_WIDTH = RET_HEADS * RET_KEY_DIM
RET_WIDTH = RET_HEADS * RET_VALUE_DIM
MIX_WIDTH = ATTN_WIDTH + RET_WIDTH
IN_PROJ_WIDTH = 3 * ATTN_WIDTH + 2 * RET_KEY_WIDTH + 2 * RET_WIDTH

DILATED_BRANCHES = ((128, 1), (512, 4), (2048, 16))
ROPE_THETA = 500000.0
ROPE_DIM = ATTN_HEAD_DIM // 4

RET_THETA = 10000.0
RET_CHUNK = 128
RET_DECAY_BASE = 5.0

MOE_GROUPS = 4
EXPERTS_PER_GROUP = 8
N_EXPERTS = MOE_GROUPS * EXPERTS_PER_GROUP
EXPERT_FF = D_MODEL // 2
MOE_TOP_K = 2
MOE_BLOCK = 256

NORM_EPS = 1e-6
NEG_INF = -1e30

kernel_name = "hymba_dilated_retention_hiermoe_encoder"


def rms_norm(x, gain):
    xf = x.astype(jnp.float32)
    y = xf * lax.rsqrt(jnp.mean(xf * xf, axis=-1, keepdims=True) + NORM_EPS)
    return (y * gain.astype(jnp.float32)).astype(x.dtype)


def apply_rotary(t, pos, freqs):
    half = freqs.shape[0]
    ang = pos[:, None] * freqs[None, :]
    cos, sin = jnp.cos(ang), jnp.sin(ang)
    t1, t2, rest = t[..., :half], t[..., half:2 * half], t[..., 2 * half:]
    return jnp.concatenate([t1 * cos - t2 * sin, t2 * cos + t1 * sin, rest], axis=-1)


def dilated_window_branch(q, k, v, window, dilation):
    b, h, s, dh = q.shape
    reach = (window // 2) // dilation
    blk = reach
    length = s // dilation
    nb = -(-length // blk)
    lp = nb * blk

    def by_stride(t):
        t = t.reshape(b, h, length, dilation, dh).transpose(0, 1, 3, 2, 4)
        return jnp.pad(t, ((0, 0), (0, 0), (0, 0), (0, lp - length), (0, 0)))

    def neighbours(t):
        t = jnp.pad(t, ((0, 0), (0, 0), (0, 0), (blk, blk), (0, 0)))
        t = t.reshape(b, h, dilation, nb + 2, blk, dh)
        return jnp.concatenate([t[:, :, :, :-2], t[:, :, :, 1:-1], t[:, :, :, 2:]], axis=-2)

    qb = by_stride(q).reshape(b, h, dilation, nb, blk, dh)
    kn = neighbours(by_stride(k))
    vn = neighbours(by_stride(v))

    t_idx = jnp.arange(blk)[:, None]
    u_idx = jnp.arange(3 * blk)[None, :]
    key_pos = (jnp.arange(nb)[:, None, None] - 1) * blk + u_idx
    offset = u_idx - blk - t_idx
    valid = (jnp.abs(offset) <= reach)[None] & (key_pos >= 0) & (key_pos < length)

    scores = jnp.einsum('bhrjtd,bhrjud->bhrjtu', qb, kn)
    scores = jnp.where(valid, scores, NEG_INF)
    m = jnp.max(scores, axis=-1, keepdims=True)
    p = jnp.exp(scores - m)
    denom = jnp.sum(p, axis=-1, keepdims=True)
    o = jnp.einsum('bhrjtu,bhrjud->bhrjtd', p, vn) / denom
    lse = (m + jnp.log(denom))[..., 0]

    o = o.reshape(b, h, dilation, lp, dh)[:, :, :, :length]
    o = o.transpose(0, 1, 3, 2, 4).reshape(b, h, s, dh)
    lse = lse.reshape(b, h, dilation, lp)[:, :, :, :length]
    lse = lse.transpose(0, 1, 3, 2).reshape(b, h, s)
    return o, lse


def dilated_attention(q, k, v):
    outs, lses = [], []
    for window, dilation in DILATED_BRANCHES:
        o, lse = dilated_window_branch(q, k, v, window, dilation)
        outs.append(o)
        lses.append(lse)
    weights = jax.nn.softmax(jnp.stack(lses), axis=0)
    return jnp.einsum('nbhs,nbhsd->bhsd', weights, jnp.stack(outs))


def retention_chunkwise(q, k, v, log_gamma, include_diag):
    b, h, s, dk = q.shape
    dv = v.shape[-1]
    c = RET_CHUNK
    n = s // c
    qc = q.reshape(b, h, n, c, dk)
    kc = k.reshape(b, h, n, c, dk)
    vc = v.reshape(b, h, n, c, dv)
    idx = jnp.arange(c, dtype=jnp.float32)
    diff = idx[:, None] - idx[None, :]
    mask = (diff >= 0) if include_diag else (diff > 0)
    decay = jnp.where(mask, jnp.exp(jnp.maximum(diff, 0.0) * log_gamma[:, None, None]), 0.0)
    inner = jnp.einsum('bhnid,bhnjd->bhnij', qc, kc) * decay[:, None]
    o_inner = jnp.einsum('bhnij,bhnje->bhnie', inner, vc)

    k_decay = jnp.exp((c - 1 - idx)[None, :] * log_gamma[:, None])
    q_decay = jnp.exp((idx + 1)[None, :] * log_gamma[:, None])
    chunk_kv = jnp.einsum('bhnjd,bhnje->nbhde', kc * k_decay[:, None, :, None], vc)
    chunk_decay = jnp.exp(c * log_gamma)[:, None, None]

    def step(state, kv):
        return state * chunk_decay + kv, state

    _, prev_states = lax.scan(step, jnp.zeros((b, h, dk, dv), jnp.float32), chunk_kv)
    o_cross = jnp.einsum('bhnid,nbhde->bhnie', qc * q_decay[:, None, :, None], prev_states)
    return (o_inner + o_cross).reshape(b, h, s, dv)


def hybrid_mixer(xn, w_in, attn_out_gain, ret_decay_fwd, ret_decay_bwd, ret_out_gain, w_out):
    b, s, _ = xn.shape
    proj = (xn @ w_in).astype(jnp.float32)
    cuts = np.cumsum([ATTN_WIDTH, ATTN_WIDTH, ATTN_WIDTH, RET_KEY_WIDTH, RET_KEY_WIDTH, RET_WIDTH]).tolist()
    qa, ka, va, qr, kr, vr, gr = jnp.split(proj, cuts, axis=-1)

    def heads(t, n_heads):
        return t.reshape(b, s, n_heads, -1).transpose(0, 2, 1, 3)

    pos = jnp.arange(s, dtype=jnp.float32)

    rope_freqs = ROPE_THETA ** (-jnp.arange(0, ROPE_DIM, 2, dtype=jnp.float32) / ROPE_DIM)
    qa = apply_rotary(heads(qa, ATTN_HEADS), pos, rope_freqs) * (ATTN_HEAD_DIM ** -0.5)
    ka = apply_rotary(heads(ka, ATTN_HEADS), pos, rope_freqs)
    oa = dilated_attention(qa, ka, heads(va, ATTN_HEADS))
    oa = rms_norm(oa.transpose(0, 2, 1, 3).reshape(b, s, ATTN_WIDTH), attn_out_gain)

    ret_freqs = RET_THETA ** (-jnp.linspace(0.0, 1.0, RET_KEY_DIM // 2, dtype=jnp.float32))
    qr = apply_rotary(heads(qr, RET_HEADS), pos, ret_freqs)
    kr = apply_rotary(heads(kr, RET_HEADS), pos, ret_freqs) * (RET_KEY_DIM ** -0.5)
    vr = heads(vr, RET_HEADS)
    lg_f = jnp.log1p(-jnp.exp2(ret_decay_fwd.astype(jnp.float32)))
    lg_b = jnp.log1p(-jnp.exp2(ret_decay_bwd.astype(jnp.float32)))
    o_f = retention_chunkwise(qr, kr, vr, lg_f, True)
    o_b = jnp.flip(retention_chunkwise(jnp.flip(qr, 2), jnp.flip(kr, 2), jnp.flip(vr, 2), lg_b, False), 2)
    orr = o_f + o_b
    mu = jnp.mean(orr, axis=-1, keepdims=True)
    var = jnp.mean(jnp.square(orr - mu), axis=-1, keepdims=True)
    orr = (orr - mu) * lax.rsqrt(var + NORM_EPS)
    orr = orr.transpose(0, 2, 1, 3).reshape(b, s, RET_WIDTH)
    orr = orr * ret_out_gain.astype(jnp.float32) * jax.nn.silu(gr)

    mixed = jnp.concatenate([oa.astype(jnp.float32), orr], axis=-1).astype(xn.dtype)
    return mixed @ w_out


def hierarchical_moe(xn, w_route_group, b_route_group, w_route_expert, b_route_expert,
                     w_expert_gate, w_expert_up, w_expert_down):
    b, s, d = xn.shape
    t = b * s
    xt = xn.reshape(t, d)
    xf = xt.astype(jnp.float32)
    group_prob = jax.nn.softmax(xf @ w_route_group.astype(jnp.float32)
                                + b_route_group.astype(jnp.float32), axis=-1)
    group_idx = jnp.argmax(group_prob, axis=-1).astype(jnp.int32)
    group_gate = jnp.take_along_axis(group_prob, group_idx[:, None], axis=-1)
    expert_logits = (xf @ w_route_expert.astype(jnp.float32)
                     + b_route_expert.astype(jnp.float32)).reshape(t, MOE_GROUPS, EXPERTS_PER_GROUP)
    in_group = jnp.take_along_axis(expert_logits, group_idx[:, None, None], axis=1)[:, 0]
    top_logits, top_idx = lax.top_k(in_group, MOE_TOP_K)
    gates = group_gate * jax.nn.softmax(top_logits, axis=-1)

    expert_ids = (group_idx[:, None] * EXPERTS_PER_GROUP + top_idx).reshape(-1).astype(jnp.int32)
    token_ids = jnp.repeat(jnp.arange(t, dtype=jnp.int32), MOE_TOP_K)
    gate_flat = gates.reshape(-1)
    n_assign = t * MOE_TOP_K
    n_blocks = -(-n_assign // MOE_BLOCK) + N_EXPERTS
    n_slots = n_blocks * MOE_BLOCK

    counts = jax.ops.segment_sum(jnp.ones((n_assign,), jnp.int32), expert_ids, num_segments=N_EXPERTS)
    padded = ((counts + MOE_BLOCK - 1) // MOE_BLOCK) * MOE_BLOCK
    pad_end = jnp.cumsum(padded)
    pad_start = pad_end - padded
    start = jnp.cumsum(counts) - counts
    order = jnp.argsort(expert_ids)
    sorted_e = expert_ids[order]
    dest = pad_start[sorted_e] + (jnp.arange(n_assign, dtype=jnp.int32) - start[sorted_e])
    slot_tok = jnp.full((n_slots,), t, jnp.int32).at[dest].set(token_ids[order])
    slot_gate = jnp.zeros((n_slots,), jnp.float32).at[dest].set(gate_flat[order])
    block_start = jnp.arange(n_blocks, dtype=jnp.int32) * MOE_BLOCK
    block_expert = jnp.clip(jnp.searchsorted(pad_end, block_start, side='right'), 0, N_EXPERTS - 1)

    x_pad = jnp.concatenate([xt, jnp.zeros((1, d), xt.dtype)], axis=0)
    xs = x_pad[slot_tok].reshape(n_blocks, MOE_BLOCK, d)

    def expert_block(args):
        xb, e = args
        hid = jax.nn.silu(xb @ w_expert_gate[e]) * (xb @ w_expert_up[e])
        return hid @ w_expert_down[e]

    ys = lax.map(expert_block, (xs, block_expert)).reshape(n_slots, d)
    out = jax.ops.segment_sum(ys * slot_gate[:, None].astype(ys.dtype), slot_tok, num_segments=t + 1)[:t]
    return out.reshape(b, s, d)


def setup_inputs(seed: int = 0) -> dict:
    key = jax.random.key(seed)
    ks = jax.random.split(key, 20)
    f32 = jnp.float32
    nrm = lambda k, shape, scale: jax.random.normal(k, shape, f32) * scale
    decay_base = -(RET_DECAY_BASE + jnp.arange(RET_HEADS, dtype=f32))
    return {
        "x": nrm(ks[0], (BATCH, SEQ, D_MODEL), 1.0),
        "mix_norm_gain": 1.0 + nrm(ks[1], (DEPTH, D_MODEL), 0.02),
        "w_in": nrm(ks[2], (DEPTH, D_MODEL, IN_PROJ_WIDTH), D_MODEL ** -0.5),
        "attn_out_gain": 1.0 + nrm(ks[3], (DEPTH, ATTN_WIDTH), 0.02),
        "ret_decay_fwd": decay_base + nrm(ks[4], (DEPTH, RET_HEADS), 0.1),
        "ret_decay_bwd": decay_base + nrm(ks[5], (DEPTH, RET_HEADS), 0.1),
        "ret_out_gain": 1.0 + nrm(ks[6], (DEPTH, RET_WIDTH), 0.02),
        "w_out": nrm(ks[7], (DEPTH, MIX_WIDTH, D_MODEL), MIX_WIDTH ** -0.5),
        "ffn_norm_gain": 1.0 + nrm(ks[8], (DEPTH, D_MODEL), 0.02),
        "w_route_group": nrm(ks[9], (DEPTH, D_MODEL, MOE_GROUPS), D_MODEL ** -0.5),
        "b_route_group": nrm(ks[10], (DEPTH, MOE_GROUPS), 0.01),
        "w_route_expert": nrm(ks[11], (DEPTH, D_MODEL, N_EXPERTS), D_MODEL ** -0.5),
        "b_route_expert": nrm(ks[12], (DEPTH, N_EXPERTS), 0.01),
        "w_expert_gate": nrm(ks[13], (DEPTH, N_EXPERTS, D_MODEL, EXPERT_FF), D_MODEL ** -0.5),
        "w_expert_up": nrm(ks[14], (DEPTH, N_EXPERTS, D_MODEL, EXPERT_FF), D_MODEL ** -0.5),
        "w_expert_down": nrm(ks[15], (DEPTH, N_EXPERTS, EXPERT_FF, D_MODEL), EXPERT_FF ** -0.5),
        "final_norm_gain": 1.0 + nrm(ks[16], (D_MODEL,), 0.02),
    }


def reference(x, mix_norm_gain, w_in, attn_out_gain, ret_decay_fwd, ret_decay_bwd, ret_out_gain,
              w_out, ffn_norm_gain, w_route_group, b_route_group, w_route_expert, b_route_expert,
              w_expert_gate, w_expert_up, w_expert_down, final_norm_gain):
    h = x
    for l in range(DEPTH):
        hn = rms_norm(h, mix_norm_gain[l])
        h = h + hybrid_mixer(hn, w_in[l], attn_out_gain[l], ret_decay_fwd[l], ret_decay_bwd[l],
                             ret_out_gain[l], w_out[l]).astype(h.dtype)
        hn = rms_norm(h, ffn_norm_gain[l])
        h = h + hierarchical_moe(hn, w_route_group[l], b_route_group[l], w_route_expert[l],
                                 b_route_expert[l], w_expert_gate[l], w_expert_up[l],
                                 w_expert_down[l]).astype(h.dtype)
    return rms_norm(h, final_norm_gain)
```

```python
import contextlib
import numpy as np
import ml_dtypes
import concourse.bass as bass
import concourse.mybir as mybir
from concourse.bass_utils import run_bass_kernel_spmd

F32 = mybir.dt.float32
BF16 = mybir.dt.bfloat16
I32 = mybir.dt.int32
U32 = mybir.dt.uint32
ALU = mybir.AluOpType
AF = mybir.ActivationFunctionType
AX = mybir.AxisListType

S = 4096
D = 1024
NT = 32
NG = 8
EPS = 1e-6
CAP = 512
NEXP = 32
FF = 512

LIMIT = {}
DEBUG = {}

ENGS = ("pe", "act", "dve", "pool", "sp")


class Buf:
    __slots__ = ("name", "writers", "readers", "excl")

    def __init__(self, name, excl=False):
        self.name = name
        self.writers = {}
        self.readers = {}
        self.excl = excl


class Op:
    __slots__ = ("eng", "fn", "deps", "dma", "tok", "signal", "idx")


class Prog:
    def __init__(self, nc):
        self.nc = nc
        self.ops = {e: [] for e in ENGS}
        self.dma_sems = {}
        self.same_engine_sync = True
        self.bar = set()

    def buf(self, name="b"):
        return Buf(name)

    def bufs(self, name, n, excl=False):
        return [Buf("%s%d" % (name, i), excl) for i in range(n)]

    def barrier(self):
        toks = set()
        for e in ENGS:
            for o in reversed(self.ops[e]):
                if o.dma is None:
                    toks.add(("c", e, o.idx))
                    break
        for k, c in self.dma_sems.items():
            toks.add(("d", k, c[0]))
        self.bar = toks

    def op(self, eng, fn, reads=(), writes=(), pwrites=(), dma=None):
        o = Op()
        o.eng = eng
        o.fn = fn
        o.dma = dma
        o.signal = False
        lst = self.ops[eng]
        o.idx = len(lst)
        if dma is None:
            tok = ("c", eng, o.idx)
            key = eng
        else:
            cnt = self.dma_sems.setdefault(dma, [0])
            cnt[0] += 1
            tok = ("d", dma, cnt[0])
            key = "dma:" + dma
        deps = set(self.bar)
        for b in reads:
            deps.update(b.writers.values())
            if b.excl:
                deps.update(v for k, v in b.readers.items() if k != eng)
        for b in writes:
            deps.update(b.writers.values())
            deps.update(b.readers.values())
        for b in pwrites:
            deps.update(b.readers.values())
            deps.update(v for k, v in b.writers.items() if k != key)
        o.tok = tok
        o.deps = deps
        for b in reads:
            b.readers[key] = tok
        for b in writes:
            b.writers = {key: tok}
            b.readers = {}
        for b in pwrites:
            b.writers[key] = tok
        lst.append(o)
        return o

    def emit(self, block, stack):
        nc = self.nc
        plan = {}
        for e in ENGS:
            known = {}
            plist = []
            for o in self.ops[e]:
                need = {}
                for t in o.deps:
                    if t[0] == "c":
                        if t[1] == e and (e == "pe" or e == "sp" or not self.same_engine_sync):
                            continue
                        if t[1] == e and t[2] >= o.idx:
                            continue
                        k = ("c", t[1])
                    else:
                        k = ("d", t[1])
                    if known.get(k, -1) >= t[2]:
                        continue
                    if need.get(k, -1) < t[2]:
                        need[k] = t[2]
                for k, v in need.items():
                    known[k] = v
                    if k[0] == "c":
                        self.ops[k[1]][v].signal = True
                plist.append(need)
            plan[e] = plist
        self.plan = plan
        sems = {}
        for e in ENGS:
            sems[("c", e)] = stack.enter_context(nc.semaphore("s_" + e))
        for k in self.dma_sems:
            sems[("d", k)] = stack.enter_context(nc.semaphore("d_" + k))
        sigcount = {}
        self.sigcount = sigcount
        for e in ENGS:
            c = 0
            arr = []
            for o in self.ops[e]:
                if o.signal:
                    c += 1
                arr.append(c)
            sigcount[e] = arr
        handles = {"pe": "tensor", "act": "scalar", "dve": "vector", "pool": "gpsimd", "sp": "sync"}
        dma_totals = {k: v[0] for k, v in self.dma_sems.items()}

        def make(e):
            def body(eng):
                for o, need in zip(self.ops[e], plan[e]):
                    for k, v in need.items():
                        if k[0] == "c":
                            eng.wait_ge(sems[k], sigcount[k[1]][v])
                        else:
                            eng.wait_ge(sems[k], 16 * v)
                    inst = o.fn(eng)
                    if o.dma is not None:
                        inst.then_inc(sems[("d", o.dma)], 16)
                    elif o.signal:
                        inst.then_inc(sems[("c", e)], 1)
                if e == "sp":
                    for k, v in dma_totals.items():
                        eng.wait_ge(sems[("d", k)], 16 * v)
            return body

        for e in ENGS:
            getattr(block, handles[e])(make(e))
        self.stats = {e: len(self.ops[e]) for e in ENGS}
        self.stats["nsem"] = len(sems)


class Arena:
    def __init__(self, nc, stack, nbytes):
        self.t = stack.enter_context(nc.sbuf_tensor("arena", [128, nbytes // 4], F32))
        self.top = 0
        self.cap = nbytes
        self.peak = 0

    def mark(self):
        return self.top

    def release(self, m):
        self.top = m

    def alloc(self, free_shape, dt, parts=128):
        esz = 4 if dt in (F32, I32, U32) else 2
        n = int(np.prod(free_shape)) * esz
        n = (n + 63) // 64 * 64
        off = self.top
        self.top += n
        self.peak = max(self.peak, self.top)
        assert self.top <= self.cap, ("SBUF arena overflow", self.top, self.cap)
        a = self.t[:, off // 4:(off + n) // 4]
        if dt != F32:
            a = a.bitcast(dt)
        a = a[:, 0:int(np.prod(free_shape))]
        if len(free_shape) == 2:
            a = a.rearrange("p (a b) -> p a b", b=free_shape[1])
        elif len(free_shape) == 3:
            a = a.rearrange("p (a b c) -> p a b c", b=free_shape[1], c=free_shape[2])
        return a


def _consts():
    c = {}
    c["ident_bf"] = np.eye(128, dtype=np.float32).astype(ml_dtypes.bfloat16)
    c["ident_f"] = np.eye(128, dtype=np.float32)
    pos = np.arange(S, dtype=np.float32)
    fa = (np.float32(500000.0) ** (-np.arange(0, 16, 2, dtype=np.float32) / np.float32(16))).astype(np.float32)
    fr = (np.float32(10000.0) ** (-np.linspace(0.0, 1.0, 32, dtype=np.float32))).astype(np.float32)
    cosA = np.ones((128, S), np.float32)
    sinA = np.zeros((128, S), np.float32)
    cosR = np.zeros((128, S), np.float32)
    sinR = np.zeros((128, S), np.float32)
    RA = np.zeros((128, 128), np.float32)
    RR = np.zeros((128, 128), np.float32)
    for p in range(128):
        dd = p % 64
        hb = p - dd
        if dd < 16:
            ang = (pos * fa[dd % 8]).astype(np.float32)
            cosA[p] = np.cos(ang)
            sinA[p] = np.sin(ang)
            if dd < 8:
                RA[hb + dd + 8, p] = -1.0
            else:
                RA[hb + dd - 8, p] = 1.0
        ang = (pos * fr[dd % 32]).astype(np.float32)
        cosR[p] = np.cos(ang)
        sinR[p] = np.sin(ang)
        if dd < 32:
            RR[hb + dd + 32, p] = -1.0
        else:
            RR[hb + dd - 32, p] = 1.0
    c["cosA"], c["sinA"], c["cosR"], c["sinR"] = cosA, sinA, cosR, sinR
    BIG = 1.0e7
    jj = np.arange(128, dtype=np.float32)[:, None]
    ii = np.arange(128, dtype=np.float32)[None, :]
    c["Mf"] = np.where(ii >= jj, ii - jj, BIG).astype(np.float32)
    c["Mb"] = np.where(jj > ii, jj - ii, BIG).astype(np.float32)
    c["iq"] = np.broadcast_to(ii + 1.0, (128, 128)).astype(np.float32).copy()
    c["iqb"] = np.broadcast_to(128.0 - ii, (128, 128)).astype(np.float32).copy()
    bav = np.zeros((128, 128), np.float32)
    bav[0:64, 0:64] = 1.0 / 64
    bav[64:128, 64:128] = 1.0 / 64
    c["Bavg"] = bav
    c["pidx"] = np.stack([127.0 - np.arange(128), np.arange(128)], axis=1).astype(np.float32)
    c["Ltri"] = (jj < ii).astype(np.float32)
    c["ones_f"] = np.ones((128, 128), np.float32)
    c["eidx"] = np.broadcast_to(np.arange(32, dtype=np.float32)[None, :], (128, 32)).copy()
    c["tokid"] = (np.arange(32, dtype=np.int32)[None, :] * 128 + np.arange(128, dtype=np.int32)[:, None]).astype(np.int32)
    c["slotinit"] = np.full((128, (NSLOT + 128) // 128), S, np.int32)
    aa = np.arange(128)[:, None]
    cc_ = np.arange(256)[None, :]
    c["amask"] = ((cc_ >= aa) & (cc_ <= aa + 128)).astype(np.float32).astype(ml_dtypes.bfloat16)
    c["RA"] = RA.astype(ml_dtypes.bfloat16)
    c["RR"] = RR.astype(ml_dtypes.bfloat16)
    return c


class KB:
    pass


def _dram_in(K, name, shape, dt):
    K.in_names.append(name)
    return K.nc.dram_tensor(name, list(shape), dt, kind="ExternalInput").ap()


def build(debug=()):
    nc = bass.Bass("TRN2", target_bir_lowering=False)
    K = KB()
    K.nc = nc
    K.in_names = []
    K.debug = set(debug)
    K.outs = []
    stack = contextlib.ExitStack()
    K.stack = stack
    P = Prog(nc)
    K.P = P
    x = _dram_in(K, "x", [S, D], F32)
    w_in = _dram_in(K, "w_in", [D, 3584], F32)
    gain1 = _dram_in(K, "gain1_bc", [128, D], F32)
    cst = {}
    for nm, shp, dt in [("ident_bf", [128, 128], BF16), ("ident_f", [128, 128], F32),
                        ("cosA", [128, S], F32), ("sinA", [128, S], F32),
                        ("cosR", [128, S], F32), ("sinR", [128, S], F32),
                        ("RA", [128, 128], BF16), ("RR", [128, 128], BF16)]:
        cst[nm] = _dram_in(K, nm, shp, dt)
    for nm, shp in [("Mf", [128, 128]), ("Mb", [128, 128]), ("iq", [128, 128]), ("iqb", [128, 128]),
                    ("Bavg", [128, 128]), ("pidx", [128, 2])]:
        cst[nm] = _dram_in(K, nm, shp, F32)
    cst["amask"] = _dram_in(K, "amask", [128, 256], BF16)
    K.cst = cst
    K.ins = {}
    K.ins["dec_bc"] = _dram_in(K, "dec_bc", [128, 24], F32)
    for nm, shp in [("w_out", [D, D]), ("gmix", [128, 8]), ("gain2_bc", [128, D]), ("br_bc", [128, 36]), ("wr", [D, 36]),
                    ("w_eg", [NEXP, D, FF]), ("w_eu", [NEXP, D, FF]), ("w_ed", [NEXP, FF, D]), ("gain3_bc", [128, D])]:
        K.ins[nm] = _dram_in(K, nm, shp, F32)
    for nm, shp in [("Ltri", [128, 128]), ("ones_f", [128, 128]), ("eidx", [128, 32])]:
        cst[nm] = _dram_in(K, nm, shp, F32)
    cst["tokid"] = _dram_in(K, "tokid", [128, 32], I32)
    cst["slotinit"] = _dram_in(K, "slotinit", [128, (NSLOT + 128) // 128], I32)
    qkv_kind = "ExternalOutput" if "qkv" in K.debug else "Internal"
    QKV = nc.dram_tensor("QKV", [28, 128, S], BF16, kind=qkv_kind).ap()
    if "qkv" in K.debug:
        K.outs.append("QKV")
    y = nc.dram_tensor("y", [S, D], F32, kind="ExternalOutput").ap()
    K.outs.append("y")

    A = Arena(nc, stack, 196 * 1024)
    K.A = A
    banks = [stack.enter_context(nc.psum_tensor("bank%d" % i, [128, 512], F32)) for i in range(8)]
    bbank = P.bufs("bank", 8, excl=True)

    ident_bf = A.alloc([128], BF16)
    b_ident = P.buf("ident")
    P.op("sp", lambda e: e.dma_start(out=ident_bf, in_=cst["ident_bf"]), writes=[b_ident], dma="c_ident")

    WEB = {nm: nc.dram_tensor("WEB_" + nm, shp, BF16, kind="Internal").ap()
           for nm, shp in (("g", [NEXP, D, FF]), ("u", [NEXP, D, FF]), ("d", [NEXP, FF, D]))}
    K.WEB = WEB
    b_web = P.buf("web")
    K.b_web = b_web
    K.precast_next = 0

    def precast(n):
        for _ in range(n):
            ex = K.precast_next
            if ex >= NEXP:
                return
            K.precast_next += 1
            for nm, src in (("g", "w_eg"), ("u", "w_eu"), ("d", "w_ed")):
                P.op("pool", lambda e, nm=nm, src=src, ex=ex: e.dma_start(out=WEB[nm][ex], in_=K.ins[src][ex]),
                     pwrites=[b_web], dma="precast")
    K.precast = precast
    if not LIMIT.get("skip_ab"):
        phase_AB(K, x, w_in, gain1, QKV, banks, bbank, ident_bf, b_ident)
    keep = {"gates": A.alloc([NT, 2], F32), "dest": A.alloc([NT, 2], I32),
            "b_gates": P.buf("gates"), "b_dest": P.buf("dest"),
            "b_H": P.buf("H"), "b_HN": P.buf("HN"), "b_slot": P.buf("slot"), "b_Y": P.buf("Y")}
    m_mixed = A.mark()
    mixedT = A.alloc([8, S], BF16)
    b_mixed = P.buf("mixedT")
    if not LIMIT.get("skip_c"):
        phase_C(K, QKV, mixedT, b_mixed, banks, bbank, ident_bf, b_ident)
    if not LIMIT.get("skip_d"):
        phase_D(K, QKV, mixedT, b_mixed, banks, bbank, ident_bf, b_ident)
    if "mixed" in K.debug:
        MIX = nc.dram_tensor("MIX", [8, 128, S], BF16, kind="ExternalOutput").ap()
        K.outs.append("MIX")
        for c in range(8):
            P.op("sp", lambda e, c=c: e.dma_start(out=MIX[c], in_=mixedT[:, c, :]), reads=[b_mixed], dma="mixdump")

    dk = "ExternalOutput" if "hdump" in K.debug else "Internal"
    H = nc.dram_tensor("H", [S, D], F32, kind=dk).ap()
    if "hdump" in K.debug:
        K.outs.append("H")
    HN = nc.dram_tensor("HN", [S + 128, D], BF16, kind="Internal").ap()
    SLOT_TOK = nc.dram_tensor("SLOT_TOK", [NSLOT + 128, 1], I32, kind=("ExternalOutput" if "hdump" in K.debug else "Internal")).ap()
    if "hdump" in K.debug:
        K.outs.append("SLOT_TOK")
    Y = nc.dram_tensor("Y", [NSLOT + 128, D], BF16, kind="Internal").ap()
    K.precast(NEXP)
    if not LIMIT.get("skip_e"):
        phase_E(K, x, mixedT, b_mixed, banks, bbank, H, HN, SLOT_TOK, keep)
    A.release(m_mixed)
    if not LIMIT.get("skip_f"):
        phase_F(K, banks, bbank, ident_bf, b_ident, HN, SLOT_TOK, Y, keep)
    phase_G(K, H, Y, y, keep)

    with nc.Block() as block:
        P.emit(block, stack)
    stack.close()
    K.stats = P.stats
    K.stats["sbuf_peak"] = A.peak
    return nc, K


def phase_AB(K, x, w_in, gain1, QKV, banks, bbank, ident_bf, b_ident):
    nc, P, A, cst = K.nc, K.P, K.A, K.cst
    m0 = A.mark()
    gain_sb = A.alloc([D], F32)
    b_gain = P.buf("gain")
    P.op("sp", lambda e: e.dma_start(out=gain_sb, in_=gain1), writes=[b_gain], dma="c_gain")
    tabs = {}
    b_tabs = {}
    for nm in ("cosA", "sinA", "cosR", "sinR")[:LIMIT.get('ntab', 4)]:
        tabs[nm] = A.alloc([S], F32)
        b_tabs[nm] = P.buf(nm)
        P.op("sp", lambda e, nm=nm: e.dma_start(out=tabs[nm], in_=cst[nm]), writes=[b_tabs[nm]], dma="c_" + nm)
    rmat = {}
    b_rmat = {}
    for nm in ("RA", "RR"):
        rmat[nm] = A.alloc([128], BF16)
        b_rmat[nm] = P.buf(nm)
        P.op("sp", lambda e, nm=nm: e.dma_start(out=rmat[nm], in_=cst[nm]), writes=[b_rmat[nm]], dma="c_" + nm)

    xnT = A.alloc([8, S], BF16)
    b_xnT = P.bufs("xnT", NG)
    xin = [A.alloc([D], F32) for _ in range(4)]
    b_xin = P.bufs("xin", 4)
    xs = [A.alloc([D], BF16) for _ in range(2)]
    b_xs = P.bufs("xs", 2)
    junk = A.alloc([D], F32)
    b_junk = P.buf("junk")
    stat = A.alloc([NT, 4], F32)
    b_stat = P.bufs("stat", NT)

    for t in range(LIMIT.get('nt', NT)):
        sl = t % 2
        x4 = t % 4
        g = t // 4
        P.op("sp", lambda e, t=t, x4=x4: e.dma_start(out=xin[x4], in_=x[t * 128:(t + 1) * 128, :]),
             writes=[b_xin[x4]], dma="xin%d" % x4)
        P.op("dve", lambda e, t=t, sl=sl, x4=x4: e.scalar_tensor_tensor(
            out=junk, in0=xin[x4], scalar=1.0, in1=xin[x4], op0=ALU.mult, op1=ALU.mult,
            accum_out=stat[:, t, 0:1]), reads=[b_xin[x4]], writes=[b_junk, b_stat[t]])
        P.op("act", lambda e, t=t: e.activation(out=stat[:, t, 1:2], in_=stat[:, t, 0:1], func=AF.Ln,
                                                 scale=1.0 / D, bias=EPS),
             reads=[b_stat[t]], writes=[b_stat[t]])
        P.op("act", lambda e, t=t: e.activation(out=stat[:, t, 2:3], in_=stat[:, t, 1:2], func=AF.Exp, scale=-0.5),
             reads=[b_stat[t]], writes=[b_stat[t]])
        P.op("dve", lambda e, t=t, sl=sl, x4=x4: e.scalar_tensor_tensor(
            out=xs[sl], in0=xin[x4], scalar=stat[:, t, 2:3], in1=gain_sb, op0=ALU.mult, op1=ALU.mult),
            reads=[b_xin[x4], b_stat[t], b_gain], writes=[b_xs[sl]])
        pb = banks[sl][:].bitcast(BF16).rearrange("p (a b) -> p a b", b=128)
        for dc in range(8):
            P.op("pe", lambda e, sl=sl, dc=dc, pb=pb: e.transpose(pb[:, dc, :], xs[sl][:, dc * 128:(dc + 1) * 128], ident_bf),
                 reads=[b_xs[sl], b_ident], **({"writes": [bbank[sl]]} if dc == 0 else {"pwrites": [bbank[sl]]}))
        P.op("act", lambda e, t=t, pb=pb: e.copy(out=xnT[:, :, t * 128:(t + 1) * 128], in_=pb),
             reads=[bbank[sl]], pwrites=[b_xnT[g]])

    wch = [A.alloc([8, 128], BF16) for _ in range(3)]
    b_wch = P.bufs("wch", 3)
    stg = [A.alloc([S], BF16) for _ in range(2)]
    b_stg = P.bufs("stg", 2)
    qsb = [A.alloc([512], BF16) for _ in range(2)]
    b_qsb = P.bufs("qsb", 2)
    tmpa = [A.alloc([512], F32) for _ in range(2)]
    b_tmpa = P.bufs("tmpa", 2)
    tmpb = [A.alloc([512], F32) for _ in range(2)]
    b_tmpb = P.bufs("tmpb", 2)
    w_v = w_in.rearrange("(dc p) c -> p dc c", p=128)
    work = []
    for cc in range(LIMIT.get('ncc', 28)):
        for g in range(NG):
            work.append((cc, g))
    pending = []

    def stageA(itn, cc, g):
        ws = cc % 3
        seg = cc // 4
        if g == 0:
            P.op("pool", lambda e, cc=cc, ws=ws: e.dma_start(out=wch[ws], in_=w_v[:, :, cc * 128:(cc + 1) * 128]),
                 writes=[b_wch[ws]], dma="wch%d" % ws)
        rot = seg in (0, 1, 3, 4)
        scale = 0.125 if seg in (0, 4) else 1.0
        ss = cc % 2
        mb = 2 + (itn % 3)
        q2 = itn % 2
        gs = slice(g * 512, (g + 1) * 512)
        for dc in range(8):
            P.op("pe", lambda e, mb=mb, ws=ws, dc=dc, gs=gs: e.matmul(
                banks[mb][:], lhsT=wch[ws][:, dc, :], rhs=xnT[:, dc, gs], start=(dc == 0), stop=(dc == 7)),
                reads=[b_wch[ws], b_xnT[g]], **({"writes": [bbank[mb]]} if dc == 0 else {"pwrites": [bbank[mb]]}))
        if not rot:
            P.op("act", lambda e, mb=mb, ss=ss, gs=gs: e.copy(out=stg[ss][:, gs], in_=banks[mb][:]),
                 reads=[bbank[mb]], pwrites=[b_stg[ss]])
        else:
            P.op("act", lambda e, mb=mb, q2=q2, scale=scale: e.activation(out=qsb[q2], in_=banks[mb][:], func=AF.Copy, scale=scale),
                 reads=[bbank[mb]], writes=[b_qsb[q2]])

    def stageB(itn, cc, g):
        seg = cc // 4
        rot = seg in (0, 1, 3, 4)
        ss = cc % 2
        gs = slice(g * 512, (g + 1) * 512)
        if rot:
            tabc, tabs_ = ("cosA", "sinA") if seg in (0, 1) else ("cosR", "sinR")
            rm = "RA" if seg in (0, 1) else "RR"
            rb = 5 + (itn % 2)
            q2 = itn % 2
            P.op("pe", lambda e, rb=rb, q2=q2, rm=rm: e.matmul(banks[rb][:], lhsT=rmat[rm], rhs=qsb[q2], start=True, stop=True),
                 reads=[b_qsb[q2], b_rmat[rm]], writes=[bbank[rb]])
            P.op("dve", lambda e, q2=q2, tabc=tabc, gs=gs: e.tensor_tensor(
                out=tmpa[q2], in0=qsb[q2], in1=tabs[tabc][:, gs], op=ALU.mult),
                reads=[b_qsb[q2], b_tabs[tabc]], writes=[b_tmpa[q2]])
            P.op("dve", lambda e, rb=rb, q2=q2, tabs_=tabs_, gs=gs: e.tensor_tensor(out=tmpb[q2], in0=banks[rb][:], in1=tabs[tabs_][:, gs], op=ALU.mult),
                 reads=[bbank[rb], b_tabs[tabs_]], writes=[b_tmpb[q2]])
            P.op("pool", lambda e, q2=q2, ss=ss, gs=gs: e.tensor_tensor(out=stg[ss][:, gs], in0=tmpa[q2], in1=tmpb[q2], op=ALU.add),
                 reads=[b_tmpa[q2], b_tmpb[q2]], pwrites=[b_stg[ss]])
        if g == NG - 1 and not LIMIT.get('skip_store'):
            P.op("sp", lambda e, cc=cc, ss=ss: e.dma_start(out=QKV[cc], in_=stg[ss]), reads=[b_stg[ss]], dma="stg%d" % ss)

    for itn, (cc, g) in enumerate(work):
        stageA(itn, cc, g)
        if itn >= 1:
            stageB(itn - 1, *work[itn - 1])
    if work:
        stageB(len(work) - 1, *work[-1])
    P.barrier()
    A.release(m0)


def phase_C(K, QKV, mixedT, b_mixed, banks, bbank, ident_bf, b_ident):
    nc, P, A, cst = K.nc, K.P, K.A, K.cst
    m0 = A.mark()
    LN2 = 0.6931471805599453
    dec = A.alloc([24], F32)
    b_dec = P.buf("dec")
    P.op("sp", lambda e: e.dma_start(out=dec, in_=K.ins["dec_bc"]), writes=[b_dec], dma="c_dec")
    cn = {}
    b_cn = {}
    for nm, w in (("Mf", 128), ("Mb", 128), ("iq", 128), ("iqb", 128), ("Bavg", 128), ("pidx", 2)):
        cn[nm] = A.alloc([w], F32)
        b_cn[nm] = P.buf(nm)
        P.op("sp", lambda e, nm=nm: e.dma_start(out=cn[nm], in_=cst[nm]), writes=[b_cn[nm]], dma="c_" + nm)
    x2 = A.alloc([24], F32)
    tt = A.alloc([24], F32)
    lg = A.alloc([24], F32)
    b_lg = P.buf("lg")
    P.op("act", lambda e: e.activation(out=x2, in_=dec, func=AF.Exp, scale=LN2), reads=[b_dec], writes=[b_lg])
    P.op("dve", lambda e: e.tensor_scalar(out=tt, in0=x2, scalar1=0.2, scalar2=None, op0=ALU.mult), reads=[b_lg], writes=[b_lg])
    for c in (0.25, 1.0 / 3.0, 0.5, 1.0):
        P.op("dve", lambda e, c=c: e.scalar_tensor_tensor(out=tt, in0=tt, scalar=c, in1=x2, op0=ALU.add, op1=ALU.mult),
             reads=[b_lg], writes=[b_lg])
    P.op("dve", lambda e: e.tensor_scalar(out=lg, in0=tt, scalar1=-1.0, scalar2=None, op0=ALU.mult), reads=[b_lg], writes=[b_lg])
    DT4 = A.alloc([8, 4, 128], F32)
    b_DT = P.buf("DT")
    e1 = A.alloc([128], F32)
    e2 = A.alloc([128], F32)
    b_e = P.buf("e12")
    for h in range(8):
        P.op("act", lambda e, h=h: e.activation(out=e1, in_=cn["Mf"], func=AF.Exp, scale=lg[:, h:h + 1]),
             reads=[b_lg, b_cn["Mf"]], writes=[b_e])
        P.op("act", lambda e, h=h: e.activation(out=e2, in_=cn["Mb"], func=AF.Exp, scale=lg[:, 8 + h:9 + h]),
             reads=[b_lg, b_cn["Mb"]], pwrites=[b_e])
        P.op("dve", lambda e, h=h: e.tensor_tensor(out=DT4[:, h], in0=e1.unsqueeze(1).to_broadcast([128, 4, 128]),
                                                    in1=e2.unsqueeze(1).to_broadcast([128, 4, 128]), op=ALU.add),
             reads=[b_e], pwrites=[b_DT])
    kd = A.alloc([16], F32)
    b_kd = P.buf("kd")
    P.op("act", lambda e: e.activation(out=kd[:, 0:8], in_=lg[:, 0:8], func=AF.Exp, scale=cn["pidx"][:, 0:1]),
         reads=[b_lg, b_cn["pidx"]], writes=[b_kd])
    P.op("act", lambda e: e.activation(out=kd[:, 8:16], in_=lg[:, 8:16], func=AF.Exp, scale=cn["pidx"][:, 1:2]),
         reads=[b_lg, b_cn["pidx"]], pwrites=[b_kd])
    qd = A.alloc([8, 128], F32)
    b_qd = P.buf("qd")
    gc = A.alloc([8], F32)
    b_gc = P.buf("gc")
    for hp in range(4):
        P.op("act", lambda e, hp=hp: e.activation(out=qd[:, hp], in_=cn["iq"], func=AF.Exp, scale=lg[:, 16 + hp:17 + hp]),
             reads=[b_lg, b_cn["iq"]], pwrites=[b_qd])
        P.op("act", lambda e, hp=hp: e.activation(out=qd[:, 4 + hp], in_=cn["iqb"], func=AF.Exp, scale=lg[:, 20 + hp:21 + hp]),
             reads=[b_lg, b_cn["iqb"]], pwrites=[b_qd])
    P.op("act", lambda e: e.activation(out=gc, in_=lg[:, 16:24], func=AF.Exp, scale=128.0), reads=[b_lg], writes=[b_gc])

    qT, kT, vT, gT = [A.alloc([S], BF16) for _ in range(4)]
    b_q, b_k, b_v, b_g = P.buf("qT"), P.buf("kT"), P.buf("vT"), P.buf("gT")
    kf = A.alloc([32, 128], BF16)
    kb = A.alloc([32, 128], BF16)
    vt = A.alloc([32, 128], BF16)
    b_kf, b_kb, b_vt = P.buf("kf"), P.buf("kb"), P.buf("vt")
    SBf = A.alloc([32, 128], BF16)
    SBb = A.alloc([32, 128], BF16)
    b_SBf, b_SBb = P.buf("SBf"), P.buf("SBb")
    stf = A.alloc([2, 128], F32)
    stb = A.alloc([2, 128], F32)
    b_stf, b_stb = P.bufs("stf", 2), P.bufs("stb", 2)
    SD = [A.alloc([4, 128], BF16) for _ in range(2)]
    b_SD = P.bufs("SD", 2)
    qdf_t = A.alloc([4, 128], BF16)
    qdb_t = A.alloc([4, 128], BF16)
    b_qdf, b_qdb = P.buf("qdf"), P.buf("qdb")
    o_sb, cen, sq, sd, rs_, sg, y1 = [A.alloc([512], F32) for _ in range(7)]
    b_o, b_cen, b_sq, b_sd, b_rs, b_sg, b_y1 = [P.buf(n) for n in "o cen sq sd rs sg y1".split()]
    o_sb2 = [o_sb, A.alloc([512], F32)]
    b_o2 = [b_o, P.buf("o2")]

    def bfv(i):
        return banks[i][:].bitcast(BF16)[:, 0:512].rearrange("p (a b) -> p a b", b=128)

    def f4(i):
        return banks[i][:].rearrange("p (a b) -> p a b", b=128)

    for hp in range(4):
        K.precast(4)
        for ap_, bb, ci, nm in ((qT, b_q, 12, "q"), (kT, b_k, 16, "k"), (vT, b_v, 20, "v"), (gT, b_g, 24, "g")):
            P.op("sp", lambda e, ap_=ap_, ci=ci, hp=hp: e.dma_start(out=ap_, in_=QKV[ci + hp]), writes=[bb], dma="ld_" + nm)
        for bt in range(8):
            bk = 6 + (bt % 2)
            for j in range(4):
                n = 4 * bt + j
                P.op("pe", lambda e, bk=bk, j=j, n=n: e.transpose(bfv(bk)[:, j, :], kT[:, n * 128:(n + 1) * 128], ident_bf),
                     reads=[b_k, b_ident], **({"writes": [bbank[bk]]} if j == 0 else {"pwrites": [bbank[bk]]}))
            for h in range(2):
                hs = slice(h * 64, (h + 1) * 64)
                P.op("dve", lambda e, bk=bk, bt=bt, hs=hs, h=h, hp=hp: e.tensor_scalar(
                    out=kf[:, 4 * bt:4 * bt + 4, hs], in0=bfv(bk)[:, :, hs], scalar1=kd[:, 2 * hp + h:2 * hp + h + 1],
                    scalar2=None, op0=ALU.mult), reads=[bbank[bk], b_kd], pwrites=[b_kf])
                P.op("dve", lambda e, bk=bk, bt=bt, hs=hs, h=h, hp=hp: e.tensor_scalar(
                    out=kb[:, 4 * bt:4 * bt + 4, hs], in0=bfv(bk)[:, :, hs], scalar1=kd[:, 8 + 2 * hp + h:8 + 2 * hp + h + 1],
                    scalar2=None, op0=ALU.mult), reads=[bbank[bk], b_kd], pwrites=[b_kb])
        for bt in range(8):
            bk = 6 + (bt % 2)
            for j in range(4):
                n = 4 * bt + j
                P.op("pe", lambda e, bk=bk, j=j, n=n: e.transpose(bfv(bk)[:, j, :], vT[:, n * 128:(n + 1) * 128], ident_bf),
                     reads=[b_v, b_ident], **({"writes": [bbank[bk]]} if j == 0 else {"pwrites": [bbank[bk]]}))
            P.op("act", lambda e, bk=bk, bt=bt: e.copy(out=vt[:, 4 * bt:4 * bt + 4, :], in_=bfv(bk)),
                 reads=[bbank[bk]], pwrites=[b_vt])
        P.op("pool", lambda e: e.memset(stf[:, 0, :], 0.0), writes=[b_stf[0]])
        P.op("pool", lambda e: e.memset(SBf[:, 0, :], 0.0), writes=[b_SBf])
        P.op("pool", lambda e: e.memset(stb[:, 1, :], 0.0), writes=[b_stb[1]])
        P.op("pool", lambda e: e.memset(SBb[:, 31, :], 0.0), writes=[b_SBb])
        for bt in range(8):
            bk = 6 + (bt % 2)
            for j in range(4):
                n = 4 * bt + j
                P.op("pe", lambda e, bk=bk, j=j, n=n: e.matmul(f4(bk)[:, j, :], lhsT=kf[:, n, :], rhs=vt[:, n, :], start=True, stop=True),
                     reads=[b_kf, b_vt], **({"writes": [bbank[bk]]} if j == 0 else {"pwrites": [bbank[bk]]}))
            for j in range(4):
                n = 4 * bt + j
                if n == 31:
                    continue
                a, b2 = n % 2, (n + 1) % 2
                P.op("dve", lambda e, bk=bk, j=j, a=a, b2=b2, hp=hp: e.scalar_tensor_tensor(
                    out=stf[:, b2, :], in0=stf[:, a, :], scalar=gc[:, hp:hp + 1], in1=f4(bk)[:, j, :], op0=ALU.mult, op1=ALU.add),
                    reads=[b_stf[a], b_gc, bbank[bk]], writes=[b_stf[b2]])
                P.op("act", lambda e, b2=b2, n=n: e.copy(out=SBf[:, n + 1, :], in_=stf[:, b2, :]), reads=[b_stf[b2]], pwrites=[b_SBf])
        for bt in range(7, -1, -1):
            bk = 6 + (bt % 2)
            for j in range(4):
                n = 4 * bt + j
                P.op("pe", lambda e, bk=bk, j=j, n=n: e.matmul(f4(bk)[:, j, :], lhsT=kb[:, n, :], rhs=vt[:, n, :], start=True, stop=True),
                     reads=[b_kb, b_vt], **({"writes": [bbank[bk]]} if j == 0 else {"pwrites": [bbank[bk]]}))
            for j in range(3, -1, -1):
                n = 4 * bt + j
                if n == 0:
                    continue
                a, b2 = n % 2, (n + 1) % 2
                P.op("dve", lambda e, bk=bk, j=j, n=n, hp=hp: e.scalar_tensor_tensor(
                    out=stb[:, (n - 1) % 2, :], in0=stb[:, n % 2, :], scalar=gc[:, 4 + hp:5 + hp], in1=f4(bk)[:, j, :], op0=ALU.mult, op1=ALU.add),
                    reads=[b_stb[n % 2], b_gc, bbank[bk]], writes=[b_stb[(n - 1) % 2]])
                P.op("act", lambda e, n=n: e.copy(out=SBb[:, n - 1, :], in_=stb[:, (n - 1) % 2, :]), reads=[b_stb[(n - 1) % 2]], pwrites=[b_SBb])
        def A1(g):
            gs = slice(g * 512, (g + 1) * 512)
            q3 = qT[:, gs].rearrange("p (a b) -> p a b", b=128)
            P.op("pool", lambda e, q3=q3, hp=hp: e.tensor_tensor(out=qdf_t, in0=q3, in1=qd[:, hp].unsqueeze(1).to_broadcast([128, 4, 128]), op=ALU.mult),
                 reads=[b_q, b_qd], writes=[b_qdf])
            P.op("pool", lambda e, q3=q3, hp=hp: e.tensor_tensor(out=qdb_t, in0=q3, in1=qd[:, 4 + hp].unsqueeze(1).to_broadcast([128, 4, 128]), op=ALU.mult),
                 reads=[b_q, b_qd], writes=[b_qdb])
            for h in range(2):
                hs = slice(h * 64, (h + 1) * 64)
                for j in range(4):
                    n = 4 * g + j
                    cs = slice(n * 128, (n + 1) * 128)
                    P.op("pe", lambda e, h=h, hs=hs, j=j, cs=cs: e.matmul(f4(h)[:, j, :], lhsT=kT[hs, cs], rhs=qT[hs, cs], start=True, stop=True),
                         reads=[b_k, b_q], **({"writes": [bbank[h]]} if j == 0 else {"pwrites": [bbank[h]]}))
                P.op("dve", lambda e, h=h, hp=hp: e.tensor_tensor(out=SD[h], in0=f4(h), in1=DT4[:, 2 * hp + h], op=ALU.mult),
                     reads=[bbank[h], b_DT], writes=[b_SD[h]])

        def A2(g):
            bo = 2 + (g % 2)
            first = True
            for j in range(4):
                n = 4 * g + j
                for h in range(2):
                    hs = slice(h * 64, (h + 1) * 64)
                    P.op("pe", lambda e, bo=bo, j=j, n=n, h=h, hs=hs: e.matmul(f4(bo)[hs, j, :], lhsT=vt[:, n, hs], rhs=SD[h][:, j, :],
                                                                                 start=True, stop=False, skip_group_check=True),
                         reads=[b_vt, b_SD[h]], **({"writes": [bbank[bo]]} if first else {"pwrites": [bbank[bo]]}))
                    first = False
                for h in range(2):
                    hs = slice(h * 64, (h + 1) * 64)
                    P.op("pe", lambda e, bo=bo, j=j, n=n, hs=hs: e.matmul(f4(bo)[hs, j, :], lhsT=SBf[hs, n, hs], rhs=qdf_t[hs, j, :],
                                                                          start=False, stop=False, skip_group_check=True),
                         reads=[b_SBf, b_qdf], pwrites=[bbank[bo]])
                    P.op("pe", lambda e, bo=bo, j=j, n=n, hs=hs: e.matmul(f4(bo)[hs, j, :], lhsT=SBb[hs, n, hs], rhs=qdb_t[hs, j, :],
                                                                          start=False, stop=True, skip_group_check=True),
                         reads=[b_SBb, b_qdb], pwrites=[bbank[bo]])
            o2 = g % 2
            P.op("act", lambda e, bo=bo, o2=o2: e.copy(out=o_sb2[o2], in_=banks[bo][:]), reads=[bbank[bo]], writes=[b_o2[o2]])

        def B1(g):
            o2 = g % 2
            P.op("pe", lambda e, o2=o2: e.matmul(banks[4][:], lhsT=cn["Bavg"], rhs=o_sb2[o2], start=True, stop=True), reads=[b_cn["Bavg"], b_o2[o2]], writes=[bbank[4]])
            P.op("dve", lambda e, o2=o2: e.tensor_tensor(out=cen, in0=o_sb2[o2], in1=banks[4][:], op=ALU.subtract), reads=[b_o2[o2], bbank[4]], writes=[b_cen])
            P.op("pool", lambda e: e.tensor_tensor(out=sq, in0=cen, in1=cen, op=ALU.mult), reads=[b_cen], writes=[b_sq])

        def B2(g):
            gs = slice(g * 512, (g + 1) * 512)
            P.op("pe", lambda e: e.matmul(banks[5][:], lhsT=cn["Bavg"], rhs=sq, start=True, stop=True), reads=[b_cn["Bavg"], b_sq], writes=[bbank[5]])
            P.op("act", lambda e: e.activation(out=sd, in_=banks[5][:], func=AF.Sqrt, bias=EPS, scale=1.0), reads=[bbank[5]], writes=[b_sd])
            P.op("dve", lambda e: e.reciprocal(out=rs_, in_=sd), reads=[b_sd], writes=[b_rs])
            P.op("act", lambda e, gs=gs: e.activation(out=sg, in_=gT[:, gs], func=AF.Silu), reads=[b_g], writes=[b_sg])
            P.op("dve", lambda e: e.tensor_tensor(out=y1, in0=cen, in1=rs_, op=ALU.mult), reads=[b_cen, b_rs], writes=[b_y1])
            P.op("pool", lambda e, hp=hp, gs=gs: e.tensor_tensor(out=mixedT[:, 4 + hp, gs], in0=y1, in1=sg, op=ALU.mult),
                 reads=[b_y1, b_sg], pwrites=[b_mixed])

        A1(0)
        A2(0)
        for g in range(NG):
            if g + 1 < NG:
                A1(g + 1)
            B1(g)
            if g + 1 < NG:
                A2(g + 1)
            B2(g)
    P.barrier()
    A.release(m0)


def _sl(start, n, step):
    return slice(start, start + (n - 1) * step + 1, step)


def phase_D(K, QKV, mixedT, b_mixed, banks, bbank, ident_bf, b_ident):
    nc, P, A, cst = K.nc, K.P, K.A, K.cst
    m0 = A.mark()
    mask = A.alloc([256], BF16)
    b_mask = P.buf("mask")
    P.op("sp", lambda e: e.dma_start(out=mask, in_=cst["amask"]), writes=[b_mask], dma="c_amask")
    qT, kT, vT = [A.alloc([S], BF16) for _ in range(3)]
    b_q, b_k, b_v = P.buf("aq"), P.buf("ak"), P.buf("av")
    DIL = (1, 4, 16)
    Vp = [[A.alloc([32, 128], BF16) for _ in DIL] for _ in range(2)]
    b_Vp = [[P.buf("Vp%d_%d" % (h, d)) for d in DIL] for h in range(2)]
    for h in range(2):
        for di in range(3):
            P.op("pool", lambda e, h=h, di=di: e.memset(Vp[h][di], 1.0), writes=[b_Vp[h][di]])
    acc = [A.alloc([S], F32) for _ in range(2)]
    b_acc = P.bufs("acc", 2)
    tmp = A.alloc([S], F32)
    b_tmp = P.buf("atmp")
    NSL = 5
    SBANK = (5, 6, 7, 0, 1)
    pt = [A.alloc([256], BF16) for _ in range(NSL)]
    pm = [A.alloc([256], BF16) for _ in range(NSL)]
    b_pt, b_pm = P.bufs("pt", NSL), P.bufs("pm", NSL)

    def bfv(i):
        return banks[i][:].bitcast(BF16)[:, 0:512].rearrange("p (a b) -> p a b", b=128)

    it = 0
    ob = 0
    for hp in range(4):
        for ap_, bb, ci, nm in ((qT, b_q, 0, "aq"), (kT, b_k, 4, "ak"), (vT, b_v, 8, "av")):
            P.op("sp", lambda e, ap_=ap_, ci=ci, hp=hp: e.dma_start(out=ap_, in_=QKV[ci + hp]), writes=[bb], dma="ld_" + nm)
        tb = 0
        for di, d in enumerate(DIL):
            L = S // d
            for bt in range(8):
                bk = tb % 2
                tb += 1
                for j in range(4):
                    i = 4 * bt + j
                    r, s0 = (128 * i) // L, (128 * i) % L
                    st0 = r + d * s0
                    P.op("pe", lambda e, bk=bk, j=j, st0=st0, d=d: e.transpose(bfv(bk)[:, j, :], vT[:, _sl(st0, 128, d)], ident_bf),
                         reads=[b_v, b_ident], **({"writes": [bbank[bk]]} if j == 0 else {"pwrites": [bbank[bk]]}))
                for h in range(2):
                    hs = slice(h * 64, (h + 1) * 64)
                    eng = "act" if h == 0 else "dve"
                    fn = (lambda e, bk=bk, bt=bt, hs=hs, h=h, di=di: e.copy(out=Vp[h][di][:, 4 * bt:4 * bt + 4, hs], in_=bfv(bk)[:, :, hs])) if h == 0 else \
                         (lambda e, bk=bk, bt=bt, hs=hs, h=h, di=di: e.tensor_copy(out=Vp[h][di][:, 4 * bt:4 * bt + 4, hs], in_=bfv(bk)[:, :, hs]))
                    P.op(eng, fn, reads=[bbank[bk]], pwrites=[b_Vp[h][di]])
        for h in range(2):
            K.precast(2)
            hs = slice(h * 64, (h + 1) * 64)
            items = []
            for di, d in enumerate(DIL):
                L = S // d
                for i in range(32):
                    items.append((di, d, L, i))
            started = {}
            LA = NSL - 1

            def stage1(di, d, L, i, slot):
                r, s0 = (128 * i) // L, (128 * i) % L
                qlo, qhi = max(0, s0 - 64), min(L, s0 + 192)
                N = qhi - qlo
                mo = qlo - (s0 - 64)
                sb = SBANK[slot]
                p3 = slot
                k0 = r + d * s0
                q0 = r + d * qlo
                P.op("pe", lambda e, sb=sb, hs=hs, k0=k0, q0=q0, N=N, d=d: e.matmul(
                    banks[sb][:, 0:N], lhsT=kT[hs, _sl(k0, 128, d)], rhs=qT[hs, _sl(q0, N, d)], start=True, stop=True),
                    reads=[b_k, b_q], writes=[bbank[sb]])
                P.op("act", lambda e, sb=sb, p3=p3, N=N: e.activation(out=pt[p3][:, 0:N], in_=banks[sb][:, 0:N], func=AF.Exp),
                     reads=[bbank[sb]], writes=[b_pt[p3]])
                P.op("pool" if (i % 2 == 0) else "dve", lambda e, p3=p3, N=N, mo=mo: e.tensor_tensor(out=pm[p3][:, 0:N], in0=pt[p3][:, 0:N], in1=mask[:, mo:mo + N], op=ALU.mult),
                     reads=[b_pt[p3], b_mask], writes=[b_pm[p3]])

            def stage2(di, d, L, i, slot):
                nonlocal ob
                r, s0 = (128 * i) // L, (128 * i) % L
                qlo, qhi = max(0, s0 - 64), min(L, s0 + 192)
                p3 = slot
                ulo, uhi = r * L + qlo, r * L + qhi
                u = ulo
                while u < uhi:
                    lb = (di, u // 512)
                    ue = min(uhi, (lb[1] + 1) * 512)
                    if lb not in started:
                        started[lb] = 2 + (ob % 3)
                        ob += 1
                        first = True
                    else:
                        first = False
                    pb = started[lb]
                    c0, c1 = u - lb[1] * 512, ue - lb[1] * 512
                    m0_, m1_ = u - ulo, ue - ulo
                    P.op("pe", lambda e, pb=pb, c0=c0, c1=c1, m0_=m0_, m1_=m1_, h=h, di=di, i=i, p3=p3, first=first: e.matmul(
                        banks[pb][:, c0:c1], lhsT=Vp[h][di][:, i, :], rhs=pm[p3][:, m0_:m1_], start=first, stop=False, skip_group_check=True),
                        reads=[b_Vp[h][di], b_pm[p3]], **({"writes": [bbank[pb]]} if first else {"pwrites": [bbank[pb]]}))
                    u = ue
                for lbk in list(started.keys()):
                    if lbk[0] != di:
                        continue
                    lb = lbk[1]
                    rr_, ss_ = (512 * lb) // L, (512 * lb) % L
                    if L >= 512:
                        last_s0 = min(L - 128, ss_ + 512)
                        last_i = (rr_ * L + last_s0) // 128
                    else:
                        last_i = ((rr_ + 1) * L + L - 128) // 128
                    if i == last_i:
                        pb = started.pop(lbk)
                        if d == 1:
                            P.op("act", lambda e, pb=pb, lb=lb, h=h: e.copy(out=acc[h][:, lb * 512:(lb + 1) * 512], in_=banks[pb][:]),
                                 reads=[bbank[pb]], pwrites=[b_acc[h]])
                        elif d == 4:
                            t0 = rr_ + 4 * ss_
                            P.op("dve", lambda e, pb=pb, t0=t0, h=h: e.tensor_tensor(
                                out=acc[h][:, _sl(t0, 512, 4)], in0=acc[h][:, _sl(t0, 512, 4)], in1=banks[pb][:], op=ALU.add),
                                reads=[bbank[pb], b_acc[h]], pwrites=[b_acc[h]])
                        else:
                            av = acc[h].rearrange("p (s r) -> p r s", r=16)[:, rr_:rr_ + 2, :]
                            P.op("dve", lambda e, pb=pb, av=av, h=h: e.tensor_tensor(
                                out=av, in0=av, in1=banks[pb][:].rearrange("p (a b) -> p a b", b=256), op=ALU.add),
                                reads=[bbank[pb], b_acc[h]], pwrites=[b_acc[h]])

            n_it = len(items)
            for idx in range(n_it + LA):
                if idx < n_it:
                    stage1(*items[idx], idx % NSL)
                if idx - LA >= 0:
                    stage2(*items[idx - LA], (idx - LA) % NSL)
            assert not started, started
            os_ = slice((1 - h) * 64, (2 - h) * 64)
            P.op("dve", lambda e, h=h, hs=hs, os_=os_: e.reciprocal(out=tmp[hs, :], in_=acc[h][os_, :]), reads=[b_acc[h]], writes=[b_tmp])
            P.op("pool", lambda e, h=h, hs=hs, hp=hp: e.tensor_tensor(out=mixedT[hs, hp, :], in0=acc[h][hs, :], in1=tmp[hs, :], op=ALU.mult),
                 reads=[b_acc[h], b_tmp], pwrites=[b_mixed])
    P.barrier()
    A.release(m0)


NSLOT = NEXP * CAP
TRASH = NSLOT


def phase_E(K, x, mixedT, b_mixed, banks, bbank, H, HN, SLOT_TOK, keep):
    nc, P, A, cst, ins = K.nc, K.P, K.A, K.cst, K.ins
    m0 = A.mark()
    ones_bf = A.alloc([128], BF16)
    b_ones = P.buf("ones")
    P.op("pool", lambda e: e.memset(ones_bf, 1.0), writes=[b_ones])
    cn = {}
    b_cn = {}
    for nm, w, dt in (("ident_f", 128, F32), ("Ltri", 128, F32), ("ones_f", 128, F32), ("eidx", 32, F32), ("tokid", 32, I32)):
        cn[nm] = A.alloc([w], dt)
        b_cn[nm] = P.buf(nm)
        P.op("sp", lambda e, nm=nm: e.dma_start(out=cn[nm], in_=cst[nm]), writes=[b_cn[nm]], dma="c_" + nm)
    gmix = A.alloc([8], F32)
    gain2 = A.alloc([D], F32)
    br = A.alloc([36], F32)
    wr = A.alloc([8, 36], F32)
    b_gmix, b_gain2, b_br, b_wr = P.buf("gmix"), P.buf("gain2"), P.buf("br"), P.buf("wr")
    P.op("sp", lambda e: e.dma_start(out=gmix, in_=ins["gmix"]), writes=[b_gmix], dma="c_gmix")
    P.op("sp", lambda e: e.dma_start(out=gain2, in_=ins["gain2_bc"]), writes=[b_gain2], dma="c_gain2")
    P.op("sp", lambda e: e.dma_start(out=br, in_=ins["br_bc"]), writes=[b_br], dma="c_br")
    P.op("sp", lambda e: e.dma_start(out=wr, in_=ins["wr"].rearrange("(dc p) c -> p dc c", p=128)), writes=[b_wr], dma="c_wr")
    Wout = A.alloc([8, D], BF16)
    b_Wout = P.buf("Wout")
    m1 = A.mark()
    wtmp = A.alloc([4, D], F32)
    b_wtmp = P.buf("wtmp")
    wo_v = ins["w_out"].rearrange("(c p) d -> p c d", p=128)
    for half in range(2):
        P.op("sp", lambda e, half=half: e.dma_start(out=wtmp, in_=wo_v[:, 4 * half:4 * half + 4, :]), writes=[b_wtmp], dma="c_wout")
        for c in range(4):
            cc = 4 * half + c
            P.op("dve", lambda e, c=c, cc=cc: e.tensor_scalar(out=Wout[:, cc, :], in0=wtmp[:, c, :], scalar1=gmix[:, cc:cc + 1], scalar2=None, op0=ALU.mult),
                 reads=[b_wtmp, b_gmix], pwrites=[b_Wout])
    zb = A.alloc([D], BF16)
    b_zb = P.buf("zb")
    P.op("pool", lambda e: e.memset(zb, 0.0), writes=[b_zb])
    b_HN, b_H, b_slot = keep["b_HN"], keep["b_H"], keep["b_slot"]
    P.op("sp", lambda e: e.dma_start(out=HN[S:S + 128, :], in_=zb), reads=[b_zb], pwrites=[b_HN], dma="hnz")
    nsl = (NSLOT + 128) // 128
    sinit = A.alloc([nsl], I32)
    b_sinit = P.buf("sinit")
    P.op("sp", lambda e: e.dma_start(out=sinit, in_=cst["slotinit"]), writes=[b_sinit], dma="c_sinit")
    P.op("sp", lambda e: e.dma_start(out=SLOT_TOK.rearrange("(p a) o -> p (a o)", p=128), in_=sinit), reads=[b_sinit], writes=[b_slot], dma="slot_init")

    sqb = A.alloc([4, 512], BF16)
    b_sqb = P.buf("sqb")
    lnv = A.alloc([512], F32)
    rbc = A.alloc([512], F32)
    b_lnv, b_rbc = P.buf("lnv"), P.buf("rbc")
    for g in range(NG):
        gs = slice(g * 512, (g + 1) * 512)
        P.op("pool", lambda e, gs=gs: e.tensor_tensor(out=sqb, in0=mixedT[:, 0:4, gs], in1=mixedT[:, 0:4, gs], op=ALU.mult),
             reads=[b_mixed], writes=[b_sqb])
        for c in range(4):
            P.op("pe", lambda e, c=c: e.matmul(banks[0][:], lhsT=ones_bf, rhs=sqb[:, c, :], start=(c == 0), stop=(c == 3)),
                 reads=[b_ones, b_sqb], **({"writes": [bbank[0]]} if c == 0 else {"pwrites": [bbank[0]]}))
        P.op("act", lambda e: e.activation(out=lnv, in_=banks[0][:], func=AF.Ln, scale=1.0 / 512, bias=EPS), reads=[bbank[0]], writes=[b_lnv])
        P.op("act", lambda e: e.activation(out=rbc, in_=lnv, func=AF.Exp, scale=-0.5), reads=[b_lnv], writes=[b_rbc])
        P.op("dve", lambda e, gs=gs: e.tensor_tensor(out=mixedT[:, 0:4, gs], in0=mixedT[:, 0:4, gs],
                                                      in1=rbc.unsqueeze(1).to_broadcast([128, 4, 512]), op=ALU.mult),
             reads=[b_rbc, b_mixed], pwrites=[b_mixed])
    P.barrier()
    A.release(m1)
    xin = [A.alloc([D], F32) for _ in range(2)]
    b_xin = P.bufs("exin", 2)
    hsb = [A.alloc([D], F32) for _ in range(2)]
    b_hsb = P.bufs("hsb", 2)
    junk = A.alloc([D], F32)
    b_junk = P.buf("ejunk")
    hn32_ = [A.alloc([D], F32) for _ in range(2)]
    b_hn32_ = P.bufs("hn32", 2)
    hnb = [A.alloc([D], BF16) for _ in range(2)]
    b_hnb = P.bufs("hnb", 2)
    hnT_ = [A.alloc([8, 128], F32) for _ in range(2)]
    b_hnT_ = P.bufs("hnT", 2)
    st = A.alloc([NT, 4], F32)
    b_st = P.bufs("est", NT)
    TB = 8
    LG = A.alloc([TB, 36], F32)
    gmax, gsum, ggate, m1_, m2_, dd, ee, den, p1, p2 = [A.alloc([TB], F32) for _ in range(10)]
    goh, gsh = A.alloc([TB, 4], F32), A.alloc([TB, 4], F32)
    sel, oh1, sel2, oh2 = [A.alloc([TB, 8], F32) for _ in range(4)]
    T32, E1, E2, Osum, base = [A.alloc([TB, 32], F32) for _ in range(5)]
    rk, eid, vv, dst = [A.alloc([TB, 2], F32) for _ in range(4)]
    Ocum = A.alloc([32], F32)
    b_Ocum = P.buf("Ocum")
    P.op("pool", lambda e: e.memset(Ocum, 0.0), writes=[b_Ocum])
    gates, dest = keep["gates"], keep["dest"]
    b_gates, b_dest = keep["b_gates"], keep["b_dest"]
    for t in range(NT):
        sl = t % 2
        hn32, b_hn32, hnT, b_hnT = hn32_[sl], b_hn32_[sl], hnT_[sl], b_hnT_[sl]
        ts_ = slice(t * 128, (t + 1) * 128)
        P.op("sp", lambda e, sl=sl, ts_=ts_: e.dma_start(out=xin[sl], in_=x[ts_, :]), writes=[b_xin[sl]], dma="exin%d" % sl)
        for half in range(2):
            bk = 1 + half
            for c in range(8):
                P.op("pe", lambda e, bk=bk, c=c, ts_=ts_, half=half: e.matmul(
                    banks[bk][:], lhsT=mixedT[:, c, ts_], rhs=Wout[:, c, half * 512:(half + 1) * 512], start=(c == 0), stop=(c == 7)),
                    reads=[b_mixed, b_Wout], **({"writes": [bbank[bk]]} if c == 0 else {"pwrites": [bbank[bk]]}))
            P.op("dve", lambda e, bk=bk, sl=sl, half=half: e.tensor_tensor(
                out=hsb[sl][:, half * 512:(half + 1) * 512], in0=xin[sl][:, half * 512:(half + 1) * 512], in1=banks[bk][:], op=ALU.add),
                reads=[bbank[bk], b_xin[sl]], **({"writes": [b_hsb[sl]]} if half == 0 else {"pwrites": [b_hsb[sl]]}))
        P.op("sp", lambda e, sl=sl, ts_=ts_: e.dma_start(out=H[ts_, :], in_=hsb[sl]), reads=[b_hsb[sl]], pwrites=[b_H], dma="hst%d" % sl)
        P.op("dve", lambda e, sl=sl, t=t: e.scalar_tensor_tensor(out=junk, in0=hsb[sl], scalar=1.0, in1=hsb[sl], op0=ALU.mult, op1=ALU.mult,
                                                             accum_out=st[:, t, 0:1]), reads=[b_hsb[sl]], writes=[b_junk, b_st[t]])
        P.op("act", lambda e, t=t: e.activation(out=st[:, t, 1:2], in_=st[:, t, 0:1], func=AF.Ln, scale=1.0 / D, bias=EPS), reads=[b_st[t]], writes=[b_st[t]])
        P.op("act", lambda e, t=t: e.activation(out=st[:, t, 2:3], in_=st[:, t, 1:2], func=AF.Exp, scale=-0.5), reads=[b_st[t]], writes=[b_st[t]])
        P.op("dve", lambda e, sl=sl, t=t, hn32=hn32: e.scalar_tensor_tensor(out=hn32, in0=hsb[sl], scalar=st[:, t, 2:3], in1=gain2, op0=ALU.mult, op1=ALU.mult),
             reads=[b_hsb[sl], b_st[t], b_gain2], writes=[b_hn32])
        P.op("act", lambda e, sl=sl, hn32=hn32: e.copy(out=hnb[sl], in_=hn32), reads=[b_hn32], writes=[b_hnb[sl]])
        P.op("sp", lambda e, sl=sl, ts_=ts_: e.dma_start(out=HN[ts_, :], in_=hnb[sl]), reads=[b_hnb[sl]], pwrites=[b_HN], dma="hnst%d" % sl)
        for half in range(2):
            bk = 3 + half
            pv = banks[bk][:].rearrange("p (a b) -> p a b", b=128)
            for j in range(4):
                dc = 4 * half + j
                P.op("pe", lambda e, pv=pv, j=j, dc=dc, hn32=hn32: e.transpose(pv[:, j, :], hn32[:, dc * 128:(dc + 1) * 128], cn["ident_f"]),
                     reads=[b_hn32, b_cn["ident_f"]], **({"writes": [bbank[bk]]} if j == 0 else {"pwrites": [bbank[bk]]}))
            P.op("act", lambda e, pv=pv, half=half, hnT=hnT: e.copy(out=hnT[:, 4 * half:4 * half + 4, :], in_=pv), reads=[bbank[bk]],
                 **({"writes": [b_hnT]} if half == 0 else {"pwrites": [b_hnT]}))
        for dc in range(8):
            P.op("pe", lambda e, dc=dc, hnT=hnT: e.matmul(banks[5][:, 0:36], lhsT=hnT[:, dc, :], rhs=wr[:, dc, :], start=(dc == 0), stop=(dc == 7)),
                 reads=[b_hnT, b_wr], **({"writes": [bbank[5]]} if dc == 0 else {"pwrites": [bbank[5]]}))
        j8 = t % TB
        if j8 == 0:
            bLG = P.buf("LG%d" % (t // TB))
        P.op("dve", lambda e, j8=j8: e.tensor_tensor(out=LG[:, j8, :], in0=banks[5][:, 0:36], in1=br, op=ALU.add),
             reads=[bbank[5], b_br], **({"writes": [bLG]} if j8 == 0 else {"pwrites": [bLG]}))
        if j8 != TB - 1:
            continue
        t0 = t - (TB - 1)
        bR = P.buf("RB%d" % (t // TB))

        def dv(fn, reads=(), writes=(), eng="dve"):
            P.op(eng, fn, reads=[bR, bLG] + list(reads), writes=[bR] + list(writes))
        gl = LG[:, :, 0:4]
        bc3 = lambda ap2, n: ap2.unsqueeze(2).to_broadcast([128, TB, n])
        dv(lambda e: e.tensor_reduce(out=gmax, in_=gl, axis=AX.X, op=ALU.max))
        dv(lambda e: e.tensor_tensor(out=goh, in0=gl, in1=bc3(gmax, 4), op=ALU.is_equal))
        dv(lambda e: e.tensor_tensor(out=gsh, in0=gl, in1=bc3(gmax, 4), op=ALU.subtract))
        dv(lambda e: e.activation(out=gsh, in_=gsh, func=AF.Exp), eng="act")
        dv(lambda e: e.tensor_reduce(out=gsum, in_=gsh, axis=AX.X, op=ALU.add))
        dv(lambda e: e.reciprocal(out=ggate, in_=gsum))
        for g in range(4):
            dv(lambda e, g=g: e.tensor_tensor(out=T32[:, :, g * 8:(g + 1) * 8], in0=LG[:, :, 4 + g * 8:12 + g * 8],
                                              in1=goh[:, :, g:g + 1].to_broadcast([128, TB, 8]), op=ALU.mult))
        dv(lambda e: e.tensor_reduce(out=sel, in_=T32.rearrange("p t (g j) -> p t j g", j=8), axis=AX.X, op=ALU.add))
        dv(lambda e: e.tensor_reduce(out=m1_, in_=sel, axis=AX.X, op=ALU.max))
        dv(lambda e: e.tensor_tensor(out=oh1, in0=sel, in1=bc3(m1_, 8), op=ALU.is_equal))
        dv(lambda e: e.scalar_tensor_tensor(out=sel2, in0=oh1, scalar=-1.0e30, in1=sel, op0=ALU.mult, op1=ALU.add))
        dv(lambda e: e.tensor_reduce(out=m2_, in_=sel2, axis=AX.X, op=ALU.max))
        dv(lambda e: e.tensor_tensor(out=oh2, in0=sel2, in1=bc3(m2_, 8), op=ALU.is_equal))
        dv(lambda e: e.tensor_tensor(out=dd, in0=m2_, in1=m1_, op=ALU.subtract))
        dv(lambda e: e.activation(out=ee, in_=dd, func=AF.Exp), eng="act")
        dv(lambda e: e.tensor_scalar(out=den, in0=ee, scalar1=1.0, scalar2=None, op0=ALU.add))
        dv(lambda e: e.reciprocal(out=p1, in_=den))
        dv(lambda e: e.tensor_tensor(out=p2, in0=ee, in1=p1, op=ALU.mult))
        P.op("dve", lambda e, t0=t0: e.tensor_tensor(out=gates[:, t0:t0 + TB, 0], in0=p1, in1=ggate, op=ALU.mult), reads=[bR], pwrites=[b_gates])
        P.op("dve", lambda e, t0=t0: e.tensor_tensor(out=gates[:, t0:t0 + TB, 1], in0=p2, in1=ggate, op=ALU.mult), reads=[bR], pwrites=[b_gates])
        for g in range(4):
            dv(lambda e, g=g: e.tensor_tensor(out=E1[:, :, g * 8:(g + 1) * 8], in0=oh1, in1=goh[:, :, g:g + 1].to_broadcast([128, TB, 8]), op=ALU.mult))
            dv(lambda e, g=g: e.tensor_tensor(out=E2[:, :, g * 8:(g + 1) * 8], in0=oh2, in1=goh[:, :, g:g + 1].to_broadcast([128, TB, 8]), op=ALU.mult))
        dv(lambda e: e.tensor_tensor(out=Osum, in0=E1, in1=E2, op=ALU.add))
        Of = Osum.rearrange("p t e -> p (t e)")
        P.op("pe", lambda e, Of=Of: e.matmul(banks[6][:, 0:TB * 32], lhsT=cn["Ltri"], rhs=Of, start=True, stop=True),
             reads=[bR, b_cn["Ltri"]], writes=[bbank[6]])
        P.op("pe", lambda e, Of=Of: e.matmul(banks[7][:, 0:TB * 32], lhsT=cn["ones_f"], rhs=Of, start=True, stop=True),
             reads=[bR, b_cn["ones_f"]], writes=[bbank[7]])
        cs3 = banks[7][:, 0:TB * 32].rearrange("p (t e) -> p t e", e=32)
        dv(lambda e: e.tensor_copy(out=base[:, 0, :], in_=Ocum), reads=[b_Ocum])
        for jj in range(1, TB):
            dv(lambda e, jj=jj: e.tensor_tensor(out=base[:, jj, :], in0=base[:, jj - 1, :], in1=cs3[:, jj - 1, :], op=ALU.add), reads=[bbank[7]])
        P.op("dve", lambda e: e.tensor_tensor(out=Ocum, in0=base[:, TB - 1, :], in1=cs3[:, TB - 1, :], op=ALU.add), reads=[bR, bbank[7]], writes=[b_Ocum])
        dv(lambda e: e.tensor_tensor(out=base, in0=base, in1=banks[6][:, 0:TB * 32].rearrange("p (t e) -> p t e", e=32), op=ALU.add), reads=[bbank[6]])
        for k, Ek in ((0, E1), (1, E2)):
            dv(lambda e, Ek=Ek: e.tensor_tensor(out=T32, in0=Ek, in1=base, op=ALU.mult))
            dv(lambda e, k=k: e.tensor_reduce(out=rk[:, :, k], in_=T32, axis=AX.X, op=ALU.add))
            dv(lambda e, Ek=Ek: e.tensor_tensor(out=T32, in0=Ek, in1=cn["eidx"].unsqueeze(1).to_broadcast([128, TB, 32]), op=ALU.mult), reads=[b_cn["eidx"]])
            dv(lambda e, k=k: e.tensor_reduce(out=eid[:, :, k], in_=T32, axis=AX.X, op=ALU.add))
        dv(lambda e: e.tensor_scalar(out=vv, in0=rk, scalar1=float(CAP), scalar2=None, op0=ALU.is_lt))
        dv(lambda e: e.scalar_tensor_tensor(out=dst, in0=eid, scalar=float(CAP), in1=rk, op0=ALU.mult, op1=ALU.add))
        dv(lambda e: e.scalar_tensor_tensor(out=dst, in0=dst, scalar=-float(TRASH), in1=vv, op0=ALU.add, op1=ALU.mult))
        dv(lambda e: e.tensor_scalar(out=dst, in0=dst, scalar1=float(TRASH), scalar2=None, op0=ALU.add))
        P.op("dve", lambda e, t0=t0: e.tensor_copy(out=dest[:, t0:t0 + TB, :], in_=dst), reads=[bR], pwrites=[b_dest])
        for tt_ in range(t0, t0 + TB):
            for k in range(2):
                P.op("pool", lambda e, tt_=tt_, k=k: e.indirect_dma_start(
                    out=SLOT_TOK[:, :], out_offset=bass.IndirectOffsetOnAxis(ap=dest[:, tt_, k:k + 1], axis=0),
                    in_=cn["tokid"][:, tt_:tt_ + 1], in_offset=None),
                    reads=[b_dest, b_cn["tokid"], b_slot], pwrites=[b_slot], dma="scat")
    P.barrier()
    A.release(m0)


def phase_F(K, banks, bbank, ident_bf, b_ident, HN, SLOT_TOK, Y, keep):
    nc, P, A, cst, ins = K.nc, K.P, K.A, K.cst, K.ins
    m0 = A.mark()
    b_HN, b_slot, b_Y = keep["b_HN"], keep["b_slot"], keep["b_Y"]
    NM = CAP // 128
    zf = A.alloc([D], BF16)
    b_zf = P.buf("zf")
    P.op("pool", lambda e: e.memset(zf, 0.0), writes=[b_zf])
    P.op("sp", lambda e: e.dma_start(out=Y[NSLOT:NSLOT + 128, :], in_=zf), reads=[b_zf], pwrites=[b_Y], dma="yz")
    NW = 3
    Wg = [A.alloc([8, FF], BF16) for _ in range(NW)]
    Wu = [A.alloc([8, FF], BF16) for _ in range(NW)]
    Wd = [A.alloc([4, D], BF16) for _ in range(NW)]
    b_Wg, b_Wu, b_Wd = P.bufs("Wg", NW), P.bufs("Wu", NW), P.bufs("Wd", NW)
    idx = [A.alloc([NM], I32) for _ in range(NW)]
    b_idx = P.bufs("idx", NW)
    Xe = [A.alloc([D], BF16) for _ in range(NM)]
    b_Xe = P.bufs("Xe", NM)
    XeT = [A.alloc([8, CAP], BF16) for _ in range(2)]
    b_XeT = P.bufs("XeT", 2)
    sgt = [A.alloc([CAP], F32) for _ in range(2)]
    b_sgt = P.bufs("sgt", 2)
    hT = [A.alloc([4, CAP], BF16) for _ in range(2)]
    b_hT = P.bufs("hT", 2)
    ysb = [A.alloc([D], BF16) for _ in range(2)]
    b_ysb = P.bufs("ysb", 2)
    wg_v = K.WEB["g"].rearrange("e (dc p) f -> e p dc f", p=128)
    wu_v = K.WEB["u"].rearrange("e (dc p) f -> e p dc f", p=128)
    wd_v = K.WEB["d"].rearrange("e (fc p) d -> e p fc d", p=128)
    slot_v = SLOT_TOK[0:NSLOT, :].rearrange("(e p m) o -> e p (m o)", p=128, m=NM)
    y_v = Y[0:NSLOT, :].rearrange("(e p m) d -> e p m d", p=128, m=NM)
    xi = 0
    yi = 0
    gi = 0
    xslot = {}

    def stage_load(ex):
        ww = ex % NW
        P.op("sp", lambda e: e.dma_start(out=Wg[ww], in_=wg_v[ex]), reads=[K.b_web], writes=[b_Wg[ww]], dma="wg%d" % ww)
        P.op("sp", lambda e: e.dma_start(out=Wu[ww], in_=wu_v[ex]), reads=[K.b_web], writes=[b_Wu[ww]], dma="wu%d" % ww)
        P.op("sp", lambda e: e.dma_start(out=Wd[ww], in_=wd_v[ex]), reads=[K.b_web], writes=[b_Wd[ww]], dma="wd%d" % ww)
        P.op("sp", lambda e: e.dma_start(out=idx[ww], in_=slot_v[ex]), reads=[b_slot], writes=[b_idx[ww]], dma="idx%d" % ww)

    def stage_gather(ex):
        ww = ex % NW
        for m in range(NM):
            P.op("pool", lambda e, ww=ww, m=m: e.indirect_dma_start(
                out=Xe[m], out_offset=None, in_=HN[:, :], in_offset=bass.IndirectOffsetOnAxis(ap=idx[ww][:, m:m + 1], axis=0)),
                reads=[b_idx[ww], b_HN], writes=[b_Xe[m]], dma="gx%d" % m)

    def stage_transpose(ex):
        ws = ex % 2
        for m in range(NM):
            bk = m % 2
            pb = banks[bk][:].bitcast(BF16).rearrange("p (a b) -> p a b", b=128)
            for dc in range(8):
                P.op("pe", lambda e, pb=pb, dc=dc, m=m: e.transpose(pb[:, dc, :], Xe[m][:, dc * 128:(dc + 1) * 128], ident_bf),
                     reads=[b_Xe[m], b_ident], **({"writes": [bbank[bk]]} if dc == 0 else {"pwrites": [bbank[bk]]}))
            P.op("act", lambda e, pb=pb, ws=ws, m=m: e.copy(out=XeT[ws][:, :, m * 128:(m + 1) * 128], in_=pb), reads=[bbank[bk]],
                 **({"writes": [b_XeT[ws]]} if m == 0 else {"pwrites": [b_XeT[ws]]}))

    stage_load(0)
    stage_load(1)
    stage_gather(0)
    stage_transpose(0)
    for ex in range(NEXP):
        ws = ex % 2
        ww = ex % NW
        if ex + 2 < NEXP:
            stage_load(ex + 2)
        if ex + 1 < NEXP:
            stage_gather(ex + 1)
        for fc in range(4):
            bg = 2 + (gi % 2)
            bu = 4 + (gi % 2)
            s2 = gi % 2
            gi += 1
            for dc in range(8):
                P.op("pe", lambda e, bg=bg, ws=ws, ww=ww, dc=dc, fc=fc: e.matmul(banks[bg][:, 0:CAP], lhsT=Wg[ww][:, dc, fc * 128:(fc + 1) * 128], rhs=XeT[ws][:, dc, :],
                                                                         start=(dc == 0), stop=(dc == 7)),
                     reads=[b_Wg[ww], b_XeT[ws]], **({"writes": [bbank[bg]]} if dc == 0 else {"pwrites": [bbank[bg]]}))
            for dc in range(8):
                P.op("pe", lambda e, bu=bu, ws=ws, ww=ww, dc=dc, fc=fc: e.matmul(banks[bu][:, 0:CAP], lhsT=Wu[ww][:, dc, fc * 128:(fc + 1) * 128], rhs=XeT[ws][:, dc, :],
                                                                         start=(dc == 0), stop=(dc == 7)),
                     reads=[b_Wu[ww], b_XeT[ws]], **({"writes": [bbank[bu]]} if dc == 0 else {"pwrites": [bbank[bu]]}))
            P.op("act", lambda e, bg=bg, s2=s2: e.activation(out=sgt[s2], in_=banks[bg][:, 0:CAP], func=AF.Silu), reads=[bbank[bg]], writes=[b_sgt[s2]])
            P.op("dve", lambda e, bu=bu, s2=s2, ws=ws, fc=fc: e.tensor_tensor(out=hT[ws][:, fc, :], in0=sgt[s2], in1=banks[bu][:, 0:CAP], op=ALU.mult),
                 reads=[b_sgt[s2], bbank[bu]], **({"writes": [b_hT[ws]]} if fc == 0 else {"pwrites": [b_hT[ws]]}))
        if ex + 1 < NEXP:
            stage_transpose(ex + 1)
        for m in range(NM):
            y2 = yi % 2
            yi += 1
            for half in range(2):
                by_ = 6 + half
                for fc in range(4):
                    P.op("pe", lambda e, by_=by_, ws=ws, ww=ww, fc=fc, m=m, half=half: e.matmul(
                        banks[by_][:], lhsT=hT[ws][:, fc, m * 128:(m + 1) * 128], rhs=Wd[ww][:, fc, half * 512:(half + 1) * 512], start=(fc == 0), stop=(fc == 3)),
                        reads=[b_hT[ws], b_Wd[ww]], **({"writes": [bbank[by_]]} if fc == 0 else {"pwrites": [bbank[by_]]}))
                if half == 0:
                    P.op("act", lambda e, by_=by_, y2=y2: e.copy(out=ysb[y2][:, 0:512], in_=banks[by_][:]), reads=[bbank[by_]], writes=[b_ysb[y2]])
                else:
                    P.op("dve", lambda e, by_=by_, y2=y2: e.tensor_copy(out=ysb[y2][:, 512:1024], in_=banks[by_][:]), reads=[bbank[by_]], pwrites=[b_ysb[y2]])
            P.op("sp", lambda e, ex=ex, m=m, y2=y2: e.dma_start(out=y_v[ex][:, m, :], in_=ysb[y2]), reads=[b_ysb[y2]], pwrites=[b_Y], dma="yst%d" % y2)
    P.barrier()
    A.release(m0)


def phase_G(K, H, Y, y, keep):
    nc, P, A, cst, ins = K.nc, K.P, K.A, K.cst, K.ins
    m0 = A.mark()
    gates, dest = keep["gates"], keep["dest"]
    b_gates, b_dest, b_H, b_Y = keep["b_gates"], keep["b_dest"], keep["b_H"], keep["b_Y"]
    fg = A.alloc([D], F32)
    b_fg = P.buf("fg")
    P.op("sp", lambda e: e.dma_start(out=fg, in_=ins["gain3_bc"]), writes=[b_fg], dma="c_gain3")
    hs_ = [A.alloc([D], F32) for _ in range(3)]
    y1 = [A.alloc([D], BF16) for _ in range(3)]
    y2 = [A.alloc([D], BF16) for _ in range(3)]
    ot = [A.alloc([D], F32) for _ in range(3)]
    b_hs, b_y1, b_y2, b_ot = P.bufs("ghs", 3), P.bufs("gy1", 3), P.bufs("gy2", 3), P.bufs("got", 3)
    junk = A.alloc([D], F32)
    b_junk = P.buf("gjunk")
    st = A.alloc([NT, 4], F32)
    b_st = P.bufs("gst", NT)
    for t in range(NT):
        sl = t % 3
        ts_ = slice(t * 128, (t + 1) * 128)
        P.op("sp", lambda e, sl=sl, ts_=ts_: e.dma_start(out=hs_[sl], in_=H[ts_, :]), reads=[b_H], writes=[b_hs[sl]], dma="gh%d" % sl)
        for k, (yy, bb) in enumerate(((y1, b_y1), (y2, b_y2))):
            P.op("pool", lambda e, sl=sl, t=t, k=k, yy=yy: e.indirect_dma_start(
                out=yy[sl], out_offset=None, in_=Y[:, :], in_offset=bass.IndirectOffsetOnAxis(ap=dest[:, t, k:k + 1], axis=0)), reads=[b_dest, b_Y], writes=[bb[sl]], dma="gy%d_%d" % (k, sl))
        P.op("dve", lambda e, sl=sl, t=t: e.scalar_tensor_tensor(out=ot[sl], in0=y1[sl], scalar=gates[:, t, 0:1], in1=hs_[sl], op0=ALU.mult, op1=ALU.add),
             reads=[b_y1[sl], b_gates, b_hs[sl]], writes=[b_ot[sl]])
        P.op("dve", lambda e, sl=sl, t=t: e.scalar_tensor_tensor(out=ot[sl], in0=y2[sl], scalar=gates[:, t, 1:2], in1=ot[sl], op0=ALU.mult, op1=ALU.add),
             reads=[b_y2[sl], b_gates, b_ot[sl]], writes=[b_ot[sl]])
        P.op("dve", lambda e, sl=sl, t=t: e.scalar_tensor_tensor(out=junk, in0=ot[sl], scalar=1.0, in1=ot[sl], op0=ALU.mult, op1=ALU.mult,
                                                             accum_out=st[:, t, 0:1]), reads=[b_ot[sl]], writes=[b_junk, b_st[t]])
        P.op("act", lambda e, t=t: e.activation(out=st[:, t, 1:2], in_=st[:, t, 0:1], func=AF.Ln, scale=1.0 / D, bias=EPS), reads=[b_st[t]], writes=[b_st[t]])
        P.op("act", lambda e, t=t: e.activation(out=st[:, t, 2:3], in_=st[:, t, 1:2], func=AF.Exp, scale=-0.5), reads=[b_st[t]], writes=[b_st[t]])
        P.op("dve", lambda e, sl=sl, t=t: e.scalar_tensor_tensor(out=ot[sl], in0=ot[sl], scalar=st[:, t, 2:3], in1=fg, op0=ALU.mult, op1=ALU.mult),
             reads=[b_ot[sl], b_st[t], b_fg], writes=[b_ot[sl]])
        P.op("sp", lambda e, sl=sl, ts_=ts_: e.dma_start(out=y[ts_, :], in_=ot[sl]), reads=[b_ot[sl]], dma="yout%d" % sl)
    A.release(m0)


_CACHE = {}


def kernel(**inputs):
    x = np.ascontiguousarray(np.asarray(inputs["x"], dtype=np.float32))
    dbg = tuple(sorted(k for k, v in DEBUG.items() if v))
    if dbg not in _CACHE:
        _CACHE[dbg] = build(dbg)
    nc, K = _CACHE[dbg]
    cst = _consts()
    common = {
        "w_in": np.ascontiguousarray(inputs["w_in"][0]),
        "gain1_bc": np.ascontiguousarray(np.broadcast_to(inputs["mix_norm_gain"][0][None, :], (128, D))),
    }
    df = np.asarray(inputs["ret_decay_fwd"][0], np.float32)
    db = np.asarray(inputs["ret_decay_bwd"][0], np.float32)
    dec = np.zeros((128, 24), np.float32)
    dec[:, 0:8] = df[None, :]
    dec[:, 8:16] = db[None, :]
    for hp in range(4):
        dec[0:64, 16 + hp] = df[2 * hp]
        dec[64:128, 16 + hp] = df[2 * hp + 1]
        dec[0:64, 20 + hp] = db[2 * hp]
        dec[64:128, 20 + hp] = db[2 * hp + 1]
    common["dec_bc"] = dec
    f32 = lambda a: np.ascontiguousarray(np.asarray(a, dtype=np.float32))
    bc = lambda v: np.ascontiguousarray(np.broadcast_to(np.asarray(v, np.float32)[None, :], (128, len(v))))
    common["w_out"] = f32(inputs["w_out"][0])
    gcat = np.concatenate([np.asarray(inputs["attn_out_gain"][0], np.float32), np.asarray(inputs["ret_out_gain"][0], np.float32)])
    common["gmix"] = np.ascontiguousarray(gcat.reshape(8, 128).T)
    common["gain2_bc"] = bc(inputs["ffn_norm_gain"][0])
    common["gain3_bc"] = bc(inputs["final_norm_gain"])
    common["br_bc"] = bc(np.concatenate([np.asarray(inputs["b_route_group"][0], np.float32), np.asarray(inputs["b_route_expert"][0], np.float32)]))
    common["wr"] = np.ascontiguousarray(np.concatenate([np.asarray(inputs["w_route_group"][0], np.float32),
                                                        np.asarray(inputs["w_route_expert"][0], np.float32)], axis=1))
    common["w_eg"] = f32(inputs["w_expert_gate"][0])
    common["w_eu"] = f32(inputs["w_expert_up"][0])
    common["w_ed"] = f32(inputs["w_expert_down"][0])
    common.update(cst)
    in_maps = []
    ncores = LIMIT.get("ncores", 8)
    for b in range(ncores):
        m = {"x": x[b]}
        m.update(common)
        in_maps.append({k: m[k] for k in K.in_names})
    res = run_bass_kernel_spmd(nc, in_maps, core_ids=list(range(ncores)))
    kernel.last = res
    out = np.stack([np.asarray(r["y"]) for r in res.results] + [np.zeros((S, D), np.float32)] * (8 - ncores), axis=0).astype(np.float32)
    return out
```

```python
import contextlib
import numpy as np
import ml_dtypes
import concourse.bass as bass
import concourse.mybir as mybir
from concourse.bass_utils import run_bass_kernel_spmd

F32 = mybir.dt.float32
BF16 = mybir.dt.bfloat16
I32 = mybir.dt.int32
U32 = mybir.dt.uint32
ALU = mybir.AluOpType
AF = mybir.ActivationFunctionType
AX = mybir.AxisListType

S = 4096
D = 1024
NT = 32
NG = 8
EPS = 1e-6
CAP = 512
NEXP = 32
FF = 512

LIMIT = {}
DEBUG = {}

ENGS = ("pe", "act", "dve", "pool", "sp")


class Buf:
    __slots__ = ("name", "writers", "readers", "excl")

    def __init__(self, name, excl=False):
        self.name = name
        self.writers = {}
        self.readers = {}
        self.excl = excl


class Op:
    __slots__ = ("eng", "fn", "deps", "dma", "tok", "signal", "idx")


class Prog:
    def __init__(self, nc):
        self.nc = nc
        self.ops = {e: [] for e in ENGS}
        self.dma_sems = {}
        self.same_engine_sync = True
        self.bar = set()

    def buf(self, name="b"):
        return Buf(name)

    def bufs(self, name, n, excl=False):
        return [Buf("%s%d" % (name, i), excl) for i in range(n)]

    def barrier(self):
        toks = set()
        for e in ENGS:
            for o in reversed(self.ops[e]):
                if o.dma is None:
                    toks.add(("c", e, o.idx))
                    break
        for k, c in self.dma_sems.items():
            toks.add(("d", k, c[0]))
        self.bar = toks

    def op(self, eng, fn, reads=(), writes=(), pwrites=(), dma=None):
        o = Op()
        o.eng = eng
        o.fn = fn
        o.dma = dma
        o.signal = False
        lst = self.ops[eng]
        o.idx = len(lst)
        if dma is None:
            tok = ("c", eng, o.idx)
            key = eng
        else:
            cnt = self.dma_sems.setdefault(dma, [0])
            cnt[0] += 1
            tok = ("d", dma, cnt[0])
            key = "dma:" + dma
        deps = set(self.bar)
        for b in reads:
            deps.update(b.writers.values())
            if b.excl:
                deps.update(v for k, v in b.readers.items() if k != eng)
        for b in writes:
            deps.update(b.writers.values())
            deps.update(b.readers.values())
        for b in pwrites:
            deps.update(b.readers.values())
            deps.update(v for k, v in b.writers.items() if k != key)
        o.tok = tok
        o.deps = deps
        for b in reads:
            b.readers[key] = tok
        for b in writes:
            b.writers = {key: tok}
            b.readers = {}
        for b in pwrites:
            b.writers[key] = tok
        lst.append(o)
        return o

    def emit(self, block, stack):
        nc = self.nc
        plan = {}
        for e in ENGS:
            known = {}
            plist = []
            for o in self.ops[e]:
                need = {}
                for t in o.deps:
                    if t[0] == "c":
                        if t[1] == e and (e == "pe" or e == "sp" or not self.same_engine_sync):
                            continue
                        if t[1] == e and t[2] >= o.idx:
                            continue
                        k = ("c", t[1])
                    else:
                        k = ("d", t[1])
                    if known.get(k, -1) >= t[2]:
                        continue
                    if need.get(k, -1) < t[2]:
                        need[k] = t[2]
                for k, v in need.items():
                    known[k] = v
                    if k[0] == "c":
                        self.ops[k[1]][v].signal = True
                plist.append(need)
            plan[e] = plist
        self.plan = plan
        sems = {}
        for e in ENGS:
            sems[("c", e)] = stack.enter_context(nc.semaphore("s_" + e))
        for k in self.dma_sems:
            sems[("d", k)] = stack.enter_context(nc.semaphore("d_" + k))
        sigcount = {}
        self.sigcount = sigcount
        for e in ENGS:
            c = 0
            arr = []
            for o in self.ops[e]:
                if o.signal:
                    c += 1
                arr.append(c)
            sigcount[e] = arr
        handles = {"pe": "tensor", "act": "scalar", "dve": "vector", "pool": "gpsimd", "sp": "sync"}
        dma_totals = {k: v[0] for k, v in self.dma_sems.items()}

        def make(e):
            def body(eng):
                for o, need in zip(self.ops[e], plan[e]):
                    for k, v in need.items():
                        if k[0] == "c":
                            eng.wait_ge(sems[k], sigcount[k[1]][v])
                        else:
                            eng.wait_ge(sems[k], 16 * v)
                    inst = o.fn(eng)
                    if o.dma is not None:
                        inst.then_inc(sems[("d", o.dma)], 16)
                    elif o.signal:
                        inst.then_inc(sems[("c", e)], 1)
                if e == "sp":
                    for k, v in dma_totals.items():
                        eng.wait_ge(sems[("d", k)], 16 * v)
            return body

        for e in ENGS:
            getattr(block, handles[e])(make(e))
        self.stats = {e: len(self.ops[e]) for e in ENGS}
        self.stats["nsem"] = len(sems)


class Arena:
    def __init__(self, nc, stack, nbytes):
        self.t = stack.enter_context(nc.sbuf_tensor("arena", [128, nbytes // 4], F32))
        self.top = 0
        self.cap = nbytes
        self.peak = 0

    def mark(self):
        return self.top

    def release(self, m):
        self.top = m

    def alloc(self, free_shape, dt, parts=128):
        esz = 4 if dt in (F32, I32, U32) else 2
        n = int(np.prod(free_shape)) * esz
        n = (n + 63) // 64 * 64
        off = self.top
        self.top += n
        self.peak = max(self.peak, self.top)
        assert self.top <= self.cap, ("SBUF arena overflow", self.top, self.cap)
        a = self.t[:, off // 4:(off + n) // 4]
        if dt != F32:
            a = a.bitcast(dt)
        a = a[:, 0:int(np.prod(free_shape))]
        if len(free_shape) == 2:
            a = a.rearrange("p (a b) -> p a b", b=free_shape[1])
        elif len(free_shape) == 3:
            a = a.rearrange("p (a b c) -> p a b c", b=free_shape[1], c=free_shape[2])
        return a


def _consts():
    c = {}
    c["ident_bf"] = np.eye(128, dtype=np.float32).astype(ml_dtypes.bfloat16)
    c["ident_f"] = np.eye(128, dtype=np.float32)
    pos = np.arange(S, dtype=np.float32)
    fa = (np.float32(500000.0) ** (-np.arange(0, 16, 2, dtype=np.float32) / np.float32(16))).astype(np.float32)
    fr = (np.float32(10000.0) ** (-np.linspace(0.0, 1.0, 32, dtype=np.float32))).astype(np.float32)
    cosA = np.ones((128, S), np.float32)
    sinA = np.zeros((128, S), np.float32)
    cosR = np.zeros((128, S), np.float32)
    sinR = np.zeros((128, S), np.float32)
    RA = np.zeros((128, 128), np.float32)
    RR = np.zeros((128, 128), np.float32)
    for p in range(128):
        dd = p % 64
        hb = p - dd
        if dd < 16:
            ang = (pos * fa[dd % 8]).astype(np.float32)
            cosA[p] = np.cos(ang)
            sinA[p] = np.sin(ang)
            if dd < 8:
                RA[hb + dd + 8, p] = -1.0
            else:
                RA[hb + dd - 8, p] = 1.0
        ang = (pos * fr[dd % 32]).astype(np.float32)
        cosR[p] = np.cos(ang)
        sinR[p] = np.sin(ang)
        if dd < 32:
            RR[hb + dd + 32, p] = -1.0
        else:
            RR[hb + dd - 32, p] = 1.0
    c["cosA"], c["sinA"], c["cosR"], c["sinR"] = cosA, sinA, cosR, sinR
    BIG = 1.0e7
    jj = np.arange(128, dtype=np.float32)[:, None]
    ii = np.arange(128, dtype=np.float32)[None, :]
    c["Mf"] = np.where(ii >= jj, ii - jj, BIG).astype(np.float32)
    c["Mb"] = np.where(jj > ii, jj - ii, BIG).astype(np.float32)
    c["iq"] = np.broadcast_to(ii + 1.0, (128, 128)).astype(np.float32).copy()
    c["iqb"] = np.broadcast_to(128.0 - ii, (128, 128)).astype(np.float32).copy()
    bav = np.zeros((128, 128), np.float32)
    bav[0:64, 0:64] = 1.0 / 64
    bav[64:128, 64:128] = 1.0 / 64
    c["Bavg"] = bav
    c["pidx"] = np.stack([127.0 - np.arange(128), np.arange(128)], axis=1).astype(np.float32)
    c["Ltri"] = (jj < ii).astype(np.float32)
    c["ones_f"] = np.ones((128, 128), np.float32)
    c["eidx"] = np.broadcast_to(np.arange(32, dtype=np.float32)[None, :], (128, 32)).copy()
    c["tokid"] = (np.arange(32, dtype=np.int32)[None, :] * 128 + np.arange(128, dtype=np.int32)[:, None]).astype(np.int32)
    c["slotinit"] = np.full((128, (NSLOT + 128) // 128), S, np.int32)
    aa = np.arange(128)[:, None]
    cc_ = np.arange(256)[None, :]
    c["amask"] = ((cc_ >= aa) & (cc_ <= aa + 128)).astype(np.float32).astype(ml_dtypes.bfloat16)
    c["RA"] = RA.astype(ml_dtypes.bfloat16)
    c["RR"] = RR.astype(ml_dtypes.bfloat16)
    return c


class KB:
    pass


def _dram_in(K, name, shape, dt):
    K.in_names.append(name)
    return K.nc.dram_tensor(name, list(shape), dt, kind="ExternalInput").ap()


def build(debug=()):
    nc = bass.Bass("TRN2", target_bir_lowering=False)
    K = KB()
    K.nc = nc
    K.in_names = []
    K.debug = set(debug)
    K.outs = []
    stack = contextlib.ExitStack()
    K.stack = stack
    P = Prog(nc)
    K.P = P
    x = _dram_in(K, "x", [S, D], F32)
    w_in = _dram_in(K, "w_in", [D, 3584], F32)
    gain1 = _dram_in(K, "gain1_bc", [128, D], F32)
    cst = {}
    for nm, shp, dt in [("ident_bf", [128, 128], BF16), ("ident_f", [128, 128], F32),
                        ("cosA", [128, S], F32), ("sinA", [128, S], F32),
                        ("cosR", [128, S], F32), ("sinR", [128, S], F32),
                        ("RA", [128, 128], BF16), ("RR", [128, 128], BF16)]:
        cst[nm] = _dram_in(K, nm, shp, dt)
    for nm, shp in [("Mf", [128, 128]), ("Mb", [128, 128]), ("iq", [128, 128]), ("iqb", [128, 128]),
                    ("Bavg", [128, 128]), ("pidx", [128, 2])]:
        cst[nm] = _dram_in(K, nm, shp, F32)
    cst["amask"] = _dram_in(K, "amask", [128, 256], BF16)
    K.cst = cst
    K.ins = {}
    K.ins["dec_bc"] = _dram_in(K, "dec_bc", [128, 24], F32)
    for nm, shp in [("w_out", [D, D]), ("gmix", [128, 8]), ("gain2_bc", [128, D]), ("br_bc", [128, 36]), ("wr", [D, 36]),
                    ("w_eg", [NEXP, D, FF]), ("w_eu", [NEXP, D, FF]), ("w_ed", [NEXP, FF, D]), ("gain3_bc", [128, D])]:
        K.ins[nm] = _dram_in(K, nm, shp, F32)
    for nm, shp in [("Ltri", [128, 128]), ("ones_f", [128, 128]), ("eidx", [128, 32])]:
        cst[nm] = _dram_in(K, nm, shp, F32)
    cst["tokid"] = _dram_in(K, "tokid", [128, 32], I32)
    cst["slotinit"] = _dram_in(K, "slotinit", [128, (NSLOT + 128) // 128], I32)
    qkv_kind = "ExternalOutput" if "qkv" in K.debug else "Internal"
    QKV = nc.dram_tensor("QKV", [28, 128, S], BF16, kind=qkv_kind).ap()
    if "qkv" in K.debug:
        K.outs.append("QKV")
    y = nc.dram_tensor("y", [S, D], F32, kind="ExternalOutput").ap()
    K.outs.append("y")

    A = Arena(nc, stack, 196 * 1024)
    K.A = A
    banks = [stack.enter_context(nc.psum_tensor("bank%d" % i, [128, 512], F32)) for i in range(8)]
    bbank = P.bufs("bank", 8, excl=True)

    ident_bf = A.alloc([128], BF16)
    b_ident = P.buf("ident")
    P.op("sp", lambda e: e.dma_start(out=ident_bf, in_=cst["ident_bf"]), writes=[b_ident], dma="c_ident")

    WEB = {nm: nc.dram_tensor("WEB_" + nm, shp, BF16, kind="Internal").ap()
           for nm, shp in (("g", [NEXP, D, FF]), ("u", [NEXP, D, FF]), ("d", [NEXP, FF, D]))}
    K.WEB = WEB
    b_web = P.buf("web")
    K.b_web = b_web
    K.precast_next = 0

    def precast(n):
        for _ in range(n):
            ex = K.precast_next
            if ex >= NEXP:
                return
            K.precast_next += 1
            for nm, src in (("g", "w_eg"), ("u", "w_eu"), ("d", "w_ed")):
                P.op("pool", lambda e, nm=nm, src=src, ex=ex: e.dma_start(out=WEB[nm][ex], in_=K.ins[src][ex]),
                     pwrites=[b_web], dma="precast")
    K.precast = precast
    if not LIMIT.get("skip_ab"):
        phase_AB(K, x, w_in, gain1, QKV, banks, bbank, ident_bf, b_ident)
    keep = {"gates": A.alloc([NT, 2], F32), "dest": A.alloc([NT, 2], I32),
            "b_gates": P.buf("gates"), "b_dest": P.buf("dest"),
            "b_H": P.buf("H"), "b_HN": P.buf("HN"), "b_slot": P.buf("slot"), "b_Y": P.buf("Y")}
    m_mixed = A.mark()
    mixedT = A.alloc([8, S], BF16)
    b_mixed = P.buf("mixedT")
    if not LIMIT.get("skip_c"):
        phase_C(K, QKV, mixedT, b_mixed, banks, bbank, ident_bf, b_ident)
    if not LIMIT.get("skip_d"):
        phase_D(K, QKV, mixedT, b_mixed, banks, bbank, ident_bf, b_ident)
    if "mixed" in K.debug:
        MIX = nc.dram_tensor("MIX", [8, 128, S], BF16, kind="ExternalOutput").ap()
        K.outs.append("MIX")
        for c in range(8):
            P.op("sp", lambda e, c=c: e.dma_start(out=MIX[c], in_=mixedT[:, c, :]), reads=[b_mixed], dma="mixdump")

    dk = "ExternalOutput" if "hdump" in K.debug else "Internal"
    H = nc.dram_tensor("H", [S, D], F32, kind=dk).ap()
    if "hdump" in K.debug:
        K.outs.append("H")
    HN = nc.dram_tensor("HN", [S + 128, D], BF16, kind="Internal").ap()
    SLOT_TOK = nc.dram_tensor("SLOT_TOK", [NSLOT + 128, 1], I32, kind=("ExternalOutput" if "hdump" in K.debug else "Internal")).ap()
    if "hdump" in K.debug:
        K.outs.append("SLOT_TOK")
    Y = nc.dram_tensor("Y", [NSLOT + 128, D], BF16, kind="Internal").ap()
    K.precast(NEXP)
    if not LIMIT.get("skip_e"):
        phase_E(K, x, mixedT, b_mixed, banks, bbank, H, HN, SLOT_TOK, keep)
    A.release(m_mixed)
    if not LIMIT.get("skip_f"):
        phase_F(K, banks, bbank, ident_bf, b_ident, HN, SLOT_TOK, Y, keep)
    phase_G(K, H, Y, y, keep)

    with nc.Block() as block:
        P.emit(block, stack)
    stack.close()
    K.stats = P.stats
    K.stats["sbuf_peak"] = A.peak
    return nc, K


def phase_AB(K, x, w_in, gain1, QKV, banks, bbank, ident_bf, b_ident):
    nc, P, A, cst = K.nc, K.P, K.A, K.cst
    m0 = A.mark()
    gain_sb = A.alloc([D], F32)
    b_gain = P.buf("gain")
    P.op("sp", lambda e: e.dma_start(out=gain_sb, in_=gain1), writes=[b_gain], dma="c_gain")
    tabs = {}
    b_tabs = {}
    for nm in ("cosA", "sinA", "cosR", "sinR")[:LIMIT.get('ntab', 4)]:
        tabs[nm] = A.alloc([S], F32)
        b_tabs[nm] = P.buf(nm)
        P.op("sp", lambda e, nm=nm: e.dma_start(out=tabs[nm], in_=cst[nm]), writes=[b_tabs[nm]], dma="c_" + nm)
    rmat = {}
    b_rmat = {}
    for nm in ("RA", "RR"):
        rmat[nm] = A.alloc([128], BF16)
        b_rmat[nm] = P.buf(nm)
        P.op("sp", lambda e, nm=nm: e.dma_start(out=rmat[nm], in_=cst[nm]), writes=[b_rmat[nm]], dma="c_" + nm)

    xnT = A.alloc([8, S], BF16)
    b_xnT = P.bufs("xnT", NG)
    xin = [A.alloc([D], F32) for _ in range(4)]
    b_xin = P.bufs("xin", 4)
    xs = [A.alloc([D], BF16) for _ in range(2)]
    b_xs = P.bufs("xs", 2)
    junk = A.alloc([D], F32)
    b_junk = P.buf("junk")
    stat = A.alloc([NT, 4], F32)
    b_stat = P.bufs("stat", NT)

    for t in range(LIMIT.get('nt', NT)):
        sl = t % 2
        x4 = t % 4
        g = t // 4
        P.op("sp", lambda e, t=t, x4=x4: e.dma_start(out=xin[x4], in_=x[t * 128:(t + 1) * 128, :]),
             writes=[b_xin[x4]], dma="xin%d" % x4)
        P.op("dve", lambda e, t=t, sl=sl, x4=x4: e.scalar_tensor_tensor(
            out=junk, in0=xin[x4], scalar=1.0, in1=xin[x4], op0=ALU.mult, op1=ALU.mult,
            accum_out=stat[:, t, 0:1]), reads=[b_xin[x4]], writes=[b_junk, b_stat[t]])
        P.op("act", lambda e, t=t: e.activation(out=stat[:, t, 1:2], in_=stat[:, t, 0:1], func=AF.Ln,
                                                 scale=1.0 / D, bias=EPS),
             reads=[b_stat[t]], writes=[b_stat[t]])
        P.op("act", lambda e, t=t: e.activation(out=stat[:, t, 2:3], in_=stat[:, t, 1:2], func=AF.Exp, scale=-0.5),
             reads=[b_stat[t]], writes=[b_stat[t]])
        P.op("dve", lambda e, t=t, sl=sl, x4=x4: e.scalar_tensor_tensor(
            out=xs[sl], in0=xin[x4], scalar=stat[:, t, 2:3], in1=gain_sb, op0=ALU.mult, op1=ALU.mult),
            reads=[b_xin[x4], b_stat[t], b_gain], writes=[b_xs[sl]])
        pb = banks[sl][:].bitcast(BF16).rearrange("p (a b) -> p a b", b=128)
        for dc in range(8):
            P.op("pe", lambda e, sl=sl, dc=dc, pb=pb: e.transpose(pb[:, dc, :], xs[sl][:, dc * 128:(dc + 1) * 128], ident_bf),
                 reads=[b_xs[sl], b_ident], **({"writes": [bbank[sl]]} if dc == 0 else {"pwrites": [bbank[sl]]}))
        P.op("act", lambda e, t=t, pb=pb: e.copy(out=xnT[:, :, t * 128:(t + 1) * 128], in_=pb),
             reads=[bbank[sl]], pwrites=[b_xnT[g]])

    wch = [A.alloc([8, 128], BF16) for _ in range(3)]
    b_wch = P.bufs("wch", 3)
    stg = [A.alloc([S], BF16) for _ in range(2)]
    b_stg = P.bufs("stg", 2)
    qsb = [A.alloc([512], BF16) for _ in range(2)]
    b_qsb = P.bufs("qsb", 2)
    tmpa = [A.alloc([512], F32) for _ in range(2)]
    b_tmpa = P.bufs("tmpa", 2)
    tmpb = [A.alloc([512], F32) for _ in range(2)]
    b_tmpb = P.bufs("tmpb", 2)
    w_v = w_in.rearrange("(dc p) c -> p dc c", p=128)
    work = []
    for cc in range(LIMIT.get('ncc', 28)):
        for g in range(NG):
            work.append((cc, g))
    pending = []

    def stageA(itn, cc, g):
        ws = cc % 3
        seg = cc // 4
        if g == 0:
            P.op("pool", lambda e, cc=cc, ws=ws: e.dma_start(out=wch[ws], in_=w_v[:, :, cc * 128:(cc + 1) * 128]),
                 writes=[b_wch[ws]], dma="wch%d" % ws)
        rot = seg in (0, 1, 3, 4)
        scale = 0.125 if seg in (0, 4) else 1.0
        ss = cc % 2
        mb = 2 + (itn % 3)
        q2 = itn % 2
        gs = slice(g * 512, (g + 1) * 512)
        for dc in range(8):
            P.op("pe", lambda e, mb=mb, ws=ws, dc=dc, gs=gs: e.matmul(
                banks[mb][:], lhsT=wch[ws][:, dc, :], rhs=xnT[:, dc, gs], start=(dc == 0), stop=(dc == 7)),
                reads=[b_wch[ws], b_xnT[g]], **({"writes": [bbank[mb]]} if dc == 0 else {"pwrites": [bbank[mb]]}))
        if not rot:
            P.op("act", lambda e, mb=mb, ss=ss, gs=gs: e.copy(out=stg[ss][:, gs], in_=banks[mb][:]),
                 reads=[bbank[mb]], pwrites=[b_stg[ss]])
        else:
            P.op("act", lambda e, mb=mb, q2=q2, scale=scale: e.activation(out=qsb[q2], in_=banks[mb][:], func=AF.Copy, scale=scale),
                 reads=[bbank[mb]], writes=[b_qsb[q2]])

    def stageB(itn, cc, g):
        seg = cc // 4
        rot = seg in (0, 1, 3, 4)
        ss = cc % 2
        gs = slice(g * 512, (g + 1) * 512)
        if rot:
            tabc, tabs_ = ("cosA", "sinA") if seg in (0, 1) else ("cosR", "sinR")
            rm = "RA" if seg in (0, 1) else "RR"
            rb = 5 + (itn % 2)
            q2 = itn % 2
            P.op("pe", lambda e, rb=rb, q2=q2, rm=rm: e.matmul(banks[rb][:], lhsT=rmat[rm], rhs=qsb[q2], start=True, stop=True),
                 reads=[b_qsb[q2], b_rmat[rm]], writes=[bbank[rb]])
            P.op("dve", lambda e, q2=q2, tabc=tabc, gs=gs: e.tensor_tensor(
                out=tmpa[q2], in0=qsb[q2], in1=tabs[tabc][:, gs], op=ALU.mult),
                reads=[b_qsb[q2], b_tabs[tabc]], writes=[b_tmpa[q2]])
            P.op("dve", lambda e, rb=rb, q2=q2, tabs_=tabs_, gs=gs: e.tensor_tensor(out=tmpb[q2], in0=banks[rb][:], in1=tabs[tabs_][:, gs], op=ALU.mult),
                 reads=[bbank[rb], b_tabs[tabs_]], writes=[b_tmpb[q2]])
            P.op("pool", lambda e, q2=q2, ss=ss, gs=gs: e.tensor_tensor(out=stg[ss][:, gs], in0=tmpa[q2], in1=tmpb[q2], op=ALU.add),
                 reads=[b_tmpa[q2], b_tmpb[q2]], pwrites=[b_stg[ss]])
        if g == NG - 1 and not LIMIT.get('skip_store'):
            P.op("sp", lambda e, cc=cc, ss=ss: e.dma_start(out=QKV[cc], in_=stg[ss]), reads=[b_stg[ss]], dma="stg%d" % ss)

    for itn, (cc, g) in enumerate(work):
        stageA(itn, cc, g)
        if itn >= 1:
            stageB(itn - 1, *work[itn - 1])
    if work:
        stageB(len(work) - 1, *work[-1])
    P.barrier()
    A.release(m0)


def phase_C(K, QKV, mixedT, b_mixed, banks, bbank, ident_bf, b_ident):
    nc, P, A, cst = K.nc, K.P, K.A, K.cst
    m0 = A.mark()
    LN2 = 0.6931471805599453
    dec = A.alloc([24], F32)
    b_dec = P.buf("dec")
    P.op("sp", lambda e: e.dma_start(out=dec, in_=K.ins["dec_bc"]), writes=[b_dec], dma="c_dec")
    cn = {}
    b_cn = {}
    for nm, w in (("Mf", 128), ("Mb", 128), ("iq", 128), ("iqb", 128), ("Bavg", 128), ("pidx", 2)):
        cn[nm] = A.alloc([w], F32)
        b_cn[nm] = P.buf(nm)
        P.op("sp", lambda e, nm=nm: e.dma_start(out=cn[nm], in_=cst[nm]), writes=[b_cn[nm]], dma="c_" + nm)
    x2 = A.alloc([24], F32)
    tt = A.alloc([24], F32)
    lg = A.alloc([24], F32)
    b_lg = P.buf("lg")
    P.op("act", lambda e: e.activation(out=x2, in_=dec, func=AF.Exp, scale=LN2), reads=[b_dec], writes=[b_lg])
    P.op("dve", lambda e: e.tensor_scalar(out=tt, in0=x2, scalar1=0.2, scalar2=None, op0=ALU.mult), reads=[b_lg], writes=[b_lg])
    for c in (0.25, 1.0 / 3.0, 0.5, 1.0):
        P.op("dve", lambda e, c=c: e.scalar_tensor_tensor(out=tt, in0=tt, scalar=c, in1=x2, op0=ALU.add, op1=ALU.mult),
             reads=[b_lg], writes=[b_lg])
    P.op("dve", lambda e: e.tensor_scalar(out=lg, in0=tt, scalar1=-1.0, scalar2=None, op0=ALU.mult), reads=[b_lg], writes=[b_lg])
    DT4 = A.alloc([8, 4, 128], F32)
    b_DT = P.buf("DT")
    e1 = A.alloc([128], F32)
    e2 = A.alloc([128], F32)
    b_e = P.buf("e12")
    for h in range(8):
        P.op("act", lambda e, h=h: e.activation(out=e1, in_=cn["Mf"], func=AF.Exp, scale=lg[:, h:h + 1]),
             reads=[b_lg, b_cn["Mf"]], writes=[b_e])
        P.op("act", lambda e, h=h: e.activation(out=e2, in_=cn["Mb"], func=AF.Exp, scale=lg[:, 8 + h:9 + h]),
             reads=[b_lg, b_cn["Mb"]], pwrites=[b_e])
        P.op("dve", lambda e, h=h: e.tensor_tensor(out=DT4[:, h], in0=e1.unsqueeze(1).to_broadcast([128, 4, 128]),
                                                    in1=e2.unsqueeze(1).to_broadcast([128, 4, 128]), op=ALU.add),
             reads=[b_e], pwrites=[b_DT])
    kd = A.alloc([16], F32)
    b_kd = P.buf("kd")
    P.op("act", lambda e: e.activation(out=kd[:, 0:8], in_=lg[:, 0:8], func=AF.Exp, scale=cn["pidx"][:, 0:1]),
         reads=[b_lg, b_cn["pidx"]], writes=[b_kd])
    P.op("act", lambda e: e.activation(out=kd[:, 8:16], in_=lg[:, 8:16], func=AF.Exp, scale=cn["pidx"][:, 1:2]),
         reads=[b_lg, b_cn["pidx"]], pwrites=[b_kd])
    qd = A.alloc([8, 128], F32)
    b_qd = P.buf("qd")
    gc = A.alloc([8], F32)
    b_gc = P.buf("gc")
    for hp in range(4):
        P.op("act", lambda e, hp=hp: e.activation(out=qd[:, hp], in_=cn["iq"], func=AF.Exp, scale=lg[:, 16 + hp:17 + hp]),
             reads=[b_lg, b_cn["iq"]], pwrites=[b_qd])
        P.op("act", lambda e, hp=hp: e.activation(out=qd[:, 4 + hp], in_=cn["iqb"], func=AF.Exp, scale=lg[:, 20 + hp:21 + hp]),
             reads=[b_lg, b_cn["iqb"]], pwrites=[b_qd])
    P.op("act", lambda e: e.activation(out=gc, in_=lg[:, 16:24], func=AF.Exp, scale=128.0), reads=[b_lg], writes=[b_gc])

    qT, kT, vT, gT = [A.alloc([S], BF16) for _ in range(4)]
    b_q, b_k, b_v, b_g = P.buf("qT"), P.buf("kT"), P.buf("vT"), P.buf("gT")
    kf = A.alloc([32, 128], BF16)
    kb = A.alloc([32, 128], BF16)
    vt = A.alloc([32, 128], BF16)
    b_kf, b_kb, b_vt = P.buf("kf"), P.buf("kb"), P.buf("vt")
    SBf = A.alloc([32, 128], BF16)
    SBb = A.alloc([32, 128], BF16)
    b_SBf, b_SBb = P.buf("SBf"), P.buf("SBb")
    stf = A.alloc([2, 128], F32)
    stb = A.alloc([2, 128], F32)
    b_stf, b_stb = P.bufs("stf", 2), P.bufs("stb", 2)
    SD = [A.alloc([4, 128], BF16) for _ in range(2)]
    b_SD = P.bufs("SD", 2)
    qdf_t = A.alloc([4, 128], BF16)
    qdb_t = A.alloc([4, 128], BF16)
    b_qdf, b_qdb = P.buf("qdf"), P.buf("qdb")
    o_sb, cen, sq, sd, rs_, sg, y1 = [A.alloc([512], F32) for _ in range(7)]
    b_o, b_cen, b_sq, b_sd, b_rs, b_sg, b_y1 = [P.buf(n) for n in "o cen sq sd rs sg y1".split()]
    o_sb2 = [o_sb, A.alloc([512], F32)]
    b_o2 = [b_o, P.buf("o2")]

    def bfv(i):
        return banks[i][:].bitcast(BF16)[:, 0:512].rearrange("p (a b) -> p a b", b=128)

    def f4(i):
        return banks[i][:].rearrange("p (a b) -> p a b", b=128)

    for hp in range(4):
        K.precast(4)
        for ap_, bb, ci, nm in ((qT, b_q, 12, "q"), (kT, b_k, 16, "k"), (vT, b_v, 20, "v"), (gT, b_g, 24, "g")):
            P.op("sp", lambda e, ap_=ap_, ci=ci, hp=hp: e.dma_start(out=ap_, in_=QKV[ci + hp]), writes=[bb], dma="ld_" + nm)
        for bt in range(8):
            bk = 6 + (bt % 2)
            for j in range(4):
                n = 4 * bt + j
                P.op("pe", lambda e, bk=bk, j=j, n=n: e.transpose(bfv(bk)[:, j, :], kT[:, n * 128:(n + 1) * 128], ident_bf),
                     reads=[b_k, b_ident], **({"writes": [bbank[bk]]} if j == 0 else {"pwrites": [bbank[bk]]}))
            for h in range(2):
                hs = slice(h * 64, (h + 1) * 64)
                P.op("dve", lambda e, bk=bk, bt=bt, hs=hs, h=h, hp=hp: e.tensor_scalar(
                    out=kf[:, 4 * bt:4 * bt + 4, hs], in0=bfv(bk)[:, :, hs], scalar1=kd[:, 2 * hp + h:2 * hp + h + 1],
                    scalar2=None, op0=ALU.mult), reads=[bbank[bk], b_kd], pwrites=[b_kf])
                P.op("dve", lambda e, bk=bk, bt=bt, hs=hs, h=h, hp=hp: e.tensor_scalar(
                    out=kb[:, 4 * bt:4 * bt + 4, hs], in0=bfv(bk)[:, :, hs], scalar1=kd[:, 8 + 2 * hp + h:8 + 2 * hp + h + 1],
                    scalar2=None, op0=ALU.mult), reads=[bbank[bk], b_kd], pwrites=[b_kb])
        for bt in range(8):
            bk = 6 + (bt % 2)
            for j in range(4):
                n = 4 * bt + j
                P.op("pe", lambda e, bk=bk, j=j, n=n: e.transpose(bfv(bk)[:, j, :], vT[:, n * 128:(n + 1) * 128], ident_bf),
                     reads=[b_v, b_ident], **({"writes": [bbank[bk]]} if j == 0 else {"pwrites": [bbank[bk]]}))
            P.op("act", lambda e, bk=bk, bt=bt: e.copy(out=vt[:, 4 * bt:4 * bt + 4, :], in_=bfv(bk)),
                 reads=[bbank[bk]], pwrites=[b_vt])
        P.op("pool", lambda e: e.memset(stf[:, 0, :], 0.0), writes=[b_stf[0]])
        P.op("pool", lambda e: e.memset(SBf[:, 0, :], 0.0), writes=[b_SBf])
        P.op("pool", lambda e: e.memset(stb[:, 1, :], 0.0), writes=[b_stb[1]])
        P.op("pool", lambda e: e.memset(SBb[:, 31, :], 0.0), writes=[b_SBb])
        for bt in range(8):
            bk = 6 + (bt % 2)
            for j in range(4):
                n = 4 * bt + j
                P.op("pe", lambda e, bk=bk, j=j, n=n: e.matmul(f4(bk)[:, j, :], lhsT=kf[:, n, :], rhs=vt[:, n, :], start=True, stop=True),
                     reads=[b_kf, b_vt], **({"writes": [bbank[bk]]} if j == 0 else {"pwrites": [bbank[bk]]}))
            for j in range(4):
                n = 4 * bt + j
                if n == 31:
                    continue
                a, b2 = n % 2, (n + 1) % 2
                P.op("dve", lambda e, bk=bk, j=j, a=a, b2=b2, hp=hp: e.scalar_tensor_tensor(
                    out=stf[:, b2, :], in0=stf[:, a, :], scalar=gc[:, hp:hp + 1], in1=f4(bk)[:, j, :], op0=ALU.mult, op1=ALU.add),
                    reads=[b_stf[a], b_gc, bbank[bk]], writes=[b_stf[b2]])
                P.op("act", lambda e, b2=b2, n=n: e.copy(out=SBf[:, n + 1, :], in_=stf[:, b2, :]), reads=[b_stf[b2]], pwrites=[b_SBf])
        for bt in range(7, -1, -1):
            bk = 6 + (bt % 2)
            for j in range(4):
                n = 4 * bt + j
                P.op("pe", lambda e, bk=bk, j=j, n=n: e.matmul(f4(bk)[:, j, :], lhsT=kb[:, n, :], rhs=vt[:, n, :], start=True, stop=True),
                     reads=[b_kb, b_vt], **({"writes": [bbank[bk]]} if j == 0 else {"pwrites": [bbank[bk]]}))
            for j in range(3, -1, -1):
                n = 4 * bt + j
                if n == 0:
                    continue
                a, b2 = n % 2, (n + 1) % 2
                P.op("dve", lambda e, bk=bk, j=j, n=n, hp=hp: e.scalar_tensor_tensor(
                    out=stb[:, (n - 1) % 2, :], in0=stb[:, n % 2, :], scalar=gc[:, 4 + hp:5 + hp], in1=f4(bk)[:, j, :], op0=ALU.mult, op1=ALU.add),
                    reads=[b_stb[n % 2], b_gc, bbank[bk]], writes=[b_stb[(n - 1) % 2]])
                P.op("act", lambda e, n=n: e.copy(out=SBb[:, n - 1, :], in_=stb[:, (n - 1) % 2, :]), reads=[b_stb[(n - 1) % 2]], pwrites=[b_SBb])
        def A1(g):
            gs = slice(g * 512, (g + 1) * 512)
            q3 = qT[:, gs].rearrange("p (a b) -> p a b", b=128)
            P.op("pool", lambda e, q3=q3, hp=hp: e.tensor_tensor(out=qdf_t, in0=q3, in1=qd[:, hp].unsqueeze(1).to_broadcast([128, 4, 128]), op=ALU.mult),
                 reads=[b_q, b_qd], writes=[b_qdf])
            P.op("pool", lambda e, q3=q3, hp=hp: e.tensor_tensor(out=qdb_t, in0=q3, in1=qd[:, 4 + hp].unsqueeze(1).to_broadcast([128, 4, 128]), op=ALU.mult),
                 reads=[b_q, b_qd], writes=[b_qdb])
            for h in range(2):
                hs = slice(h * 64, (h + 1) * 64)
                for j in range(4):
                    n = 4 * g + j
                    cs = slice(n * 128, (n + 1) * 128)
                    P.op("pe", lambda e, h=h, hs=hs, j=j, cs=cs: e.matmul(f4(h)[:, j, :], lhsT=kT[hs, cs], rhs=qT[hs, cs], start=True, stop=True),
                         reads=[b_k, b_q], **({"writes": [bbank[h]]} if j == 0 else {"pwrites": [bbank[h]]}))
                P.op("dve", lambda e, h=h, hp=hp: e.tensor_tensor(out=SD[h], in0=f4(h), in1=DT4[:, 2 * hp + h], op=ALU.mult),
                     reads=[bbank[h], b_DT], writes=[b_SD[h]])

        def A2(g):
            bo = 2 + (g % 2)
            first = True
            for j in range(4):
                n = 4 * g + j
                for h in range(2):
                    hs = slice(h * 64, (h + 1) * 64)
                    P.op("pe", lambda e, bo=bo, j=j, n=n, h=h, hs=hs: e.matmul(f4(bo)[hs, j, :], lhsT=vt[:, n, hs], rhs=SD[h][:, j, :],
                                                                                 start=True, stop=False, skip_group_check=True),
                         reads=[b_vt, b_SD[h]], **({"writes": [bbank[bo]]} if first else {"pwrites": [bbank[bo]]}))
                    first = False
                for h in range(2):
                    hs = slice(h * 64, (h + 1) * 64)
                    P.op("pe", lambda e, bo=bo, j=j, n=n, hs=hs: e.matmul(f4(bo)[hs, j, :], lhsT=SBf[hs, n, hs], rhs=qdf_t[hs, j, :],
                                                                          start=False, stop=False, skip_group_check=True),
                         reads=[b_SBf, b_qdf], pwrites=[bbank[bo]])
                    P.op("pe", lambda e, bo=bo, j=j, n=n, hs=hs: e.matmul(f4(bo)[hs, j, :], lhsT=SBb[hs, n, hs], rhs=qdb_t[hs, j, :],
                                                                          start=False, stop=True, skip_group_check=True),
                         reads=[b_SBb, b_qdb], pwrites=[bbank[bo]])
            o2 = g % 2
            P.op("act", lambda e, bo=bo, o2=o2: e.copy(out=o_sb2[o2], in_=banks[bo][:]), reads=[bbank[bo]], writes=[b_o2[o2]])

        def B1(g):
            o2 = g % 2
            P.op("pe", lambda e, o2=o2: e.matmul(banks[4][:], lhsT=cn["Bavg"], rhs=o_sb2[o2], start=True, stop=True), reads=[b_cn["Bavg"], b_o2[o2]], writes=[bbank[4]])
            P.op("dve", lambda e, o2=o2: e.tensor_tensor(out=cen, in0=o_sb2[o2], in1=banks[4][:], op=ALU.subtract), reads=[b_o2[o2], bbank[4]], writes=[b_cen])
            P.op("pool", lambda e: e.tensor_tensor(out=sq, in0=cen, in1=cen, op=ALU.mult), reads=[b_cen], writes=[b_sq])

        def B2(g):
            gs = slice(g * 512, (g + 1) * 512)
            P.op("pe", lambda e: e.matmul(banks[5][:], lhsT=cn["Bavg"], rhs=sq, start=True, stop=True), reads=[b_cn["Bavg"], b_sq], writes=[bbank[5]])
            P.op("act", lambda e: e.activation(out=sd, in_=banks[5][:], func=AF.Sqrt, bias=EPS, scale=1.0), reads=[bbank[5]], writes=[b_sd])
            P.op("dve", lambda e: e.reciprocal(out=rs_, in_=sd), reads=[b_sd], writes=[b_rs])
            P.op("act", lambda e, gs=gs: e.activation(out=sg, in_=gT[:, gs], func=AF.Silu), reads=[b_g], writes=[b_sg])
            P.op("dve", lambda e: e.tensor_tensor(out=y1, in0=cen, in1=rs_, op=ALU.mult), reads=[b_cen, b_rs], writes=[b_y1])
            P.op("pool", lambda e, hp=hp, gs=gs: e.tensor_tensor(out=mixedT[:, 4 + hp, gs], in0=y1, in1=sg, op=ALU.mult),
                 reads=[b_y1, b_sg], pwrites=[b_mixed])

        A1(0)
        A2(0)
        for g in range(NG):
            if g + 1 < NG:
                A1(g + 1)
            B1(g)
            if g + 1 < NG:
                A2(g + 1)
            B2(g)
    P.barrier()
    A.release(m0)


def _sl(start, n, step):
    return slice(start, start + (n - 1) * step + 1, step)


def phase_D(K, QKV, mixedT, b_mixed, banks, bbank, ident_bf, b_ident):
    nc, P, A, cst = K.nc, K.P, K.A, K.cst
    m0 = A.mark()
    mask = A.alloc([256], BF16)
    b_mask = P.buf("mask")
    P.op("sp", lambda e: e.dma_start(out=mask, in_=cst["amask"]), writes=[b_mask], dma="c_amask")
    qT, kT, vT = [A.alloc([S], BF16) for _ in range(3)]
    b_q, b_k, b_v = P.buf("aq"), P.buf("ak"), P.buf("av")
    DIL = (1, 4, 16)
    Vp = [[A.alloc([32, 128], BF16) for _ in DIL] for _ in range(2)]
    b_Vp = [[P.buf("Vp%d_%d" % (h, d)) for d in DIL] for h in range(2)]
    for h in range(2):
        for di in range(3):
            P.op("pool", lambda e, h=h, di=di: e.memset(Vp[h][di], 1.0), writes=[b_Vp[h][di]])
    acc = [A.alloc([S], F32) for _ in range(2)]
    b_acc = P.bufs("acc", 2)
    tmp = A.alloc([S], F32)
    b_tmp = P.buf("atmp")
    NSL = 5
    SBANK = (5, 6, 7, 0, 1)
    pt = [A.alloc([256], BF16) for _ in range(NSL)]
    pm = [A.alloc([256], BF16) for _ in range(NSL)]
    b_pt, b_pm = P.bufs("pt", NSL), P.bufs("pm", NSL)

    def bfv(i):
        return banks[i][:].bitcast(BF16)[:, 0:512].rearrange("p (a b) -> p a b", b=128)

    it = 0
    ob = 0
    for hp in range(4):
        for ap_, bb, ci, nm in ((qT, b_q, 0, "aq"), (kT, b_k, 4, "ak"), (vT, b_v, 8, "av")):
            P.op("sp", lambda e, ap_=ap_, ci=ci, hp=hp: e.dma_start(out=ap_, in_=QKV[ci + hp]), writes=[bb], dma="ld_" + nm)
        tb = 0
        for di, d in enumerate(DIL):
            L = S // d
            for bt in range(8):
                bk = tb % 2
                tb += 1
                for j in range(4):
                    i = 4 * bt + j
                    r, s0 = (128 * i) // L, (128 * i) % L
                    st0 = r + d * s0
                    P.op("pe", lambda e, bk=bk, j=j, st0=st0, d=d: e.transpose(bfv(bk)[:, j, :], vT[:, _sl(st0, 128, d)], ident_bf),
                         reads=[b_v, b_ident], **({"writes": [bbank[bk]]} if j == 0 else {"pwrites": [bbank[bk]]}))
                for h in range(2):
                    hs = slice(h * 64, (h + 1) * 64)
                    eng = "act" if h == 0 else "dve"
                    fn = (lambda e, bk=bk, bt=bt, hs=hs, h=h, di=di: e.copy(out=Vp[h][di][:, 4 * bt:4 * bt + 4, hs], in_=bfv(bk)[:, :, hs])) if h == 0 else \
                         (lambda e, bk=bk, bt=bt, hs=hs, h=h, di=di: e.tensor_copy(out=Vp[h][di][:, 4 * bt:4 * bt + 4, hs], in_=bfv(bk)[:, :, hs]))
                    P.op(eng, fn, reads=[bbank[bk]], pwrites=[b_Vp[h][di]])
        for h in range(2):
            K.precast(2)
            hs = slice(h * 64, (h + 1) * 64)
            items = []
            for di, d in enumerate(DIL):
                L = S // d
                for i in range(32):
                    items.append((di, d, L, i))
            started = {}
            LA = NSL - 1

            def stage1(di, d, L, i, slot):
                r, s0 = (128 * i) // L, (128 * i) % L
                qlo, qhi = max(0, s0 - 64), min(L, s0 + 192)
                N = qhi - qlo
                mo = qlo - (s0 - 64)
                sb = SBANK[slot]
                p3 = slot
                k0 = r + d * s0
                q0 = r + d * qlo
                P.op("pe", lambda e, sb=sb, hs=hs, k0=k0, q0=q0, N=N, d=d: e.matmul(
                    banks[sb][:, 0:N], lhsT=kT[hs, _sl(k0, 128, d)], rhs=qT[hs, _sl(q0, N, d)], start=True, stop=True),
                    reads=[b_k, b_q], writes=[bbank[sb]])
                P.op("act", lambda e, sb=sb, p3=p3, N=N: e.activation(out=pt[p3][:, 0:N], in_=banks[sb][:, 0:N], func=AF.Exp),
                     reads=[bbank[sb]], writes=[b_pt[p3]])
                P.op("pool" if (i % 2 == 0) else "dve", lambda e, p3=p3, N=N, mo=mo: e.tensor_tensor(out=pm[p3][:, 0:N], in0=pt[p3][:, 0:N], in1=mask[:, mo:mo + N], op=ALU.mult),
                     reads=[b_pt[p3], b_mask], writes=[b_pm[p3]])

            def stage2(di, d, L, i, slot):
                nonlocal ob
                r, s0 = (128 * i) // L, (128 * i) % L
                qlo, qhi = max(0, s0 - 64), min(L, s0 + 192)
                p3 = slot
                ulo, uhi = r * L + qlo, r * L + qhi
                u = ulo
                while u < uhi:
                    lb = (di, u // 512)
                    ue = min(uhi, (lb[1] + 1) * 512)
                    if lb not in started:
                        started[lb] = 2 + (ob % 3)
                        ob += 1
                        first = True
                    else:
                        first = False
                    pb = started[lb]
                    c0, c1 = u - lb[1] * 512, ue - lb[1] * 512
                    m0_, m1_ = u - ulo, ue - ulo
                    P.op("pe", lambda e, pb=pb, c0=c0, c1=c1, m0_=m0_, m1_=m1_, h=h, di=di, i=i, p3=p3, first=first: e.matmul(
                        banks[pb][:, c0:c1], lhsT=Vp[h][di][:, i, :], rhs=pm[p3][:, m0_:m1_], start=first, stop=False, skip_group_check=True),
                        reads=[b_Vp[h][di], b_pm[p3]], **({"writes": [bbank[pb]]} if first else {"pwrites": [bbank[pb]]}))
                    u = ue
                for lbk in list(started.keys()):
                    if lbk[0] != di:
                        continue
                    lb = lbk[1]
                    rr_, ss_ = (512 * lb) // L, (512 * lb) % L
                    if L >= 512:
                        last_s0 = min(L - 128, ss_ + 512)
                        last_i = (rr_ * L + last_s0) // 128
                    else:
                        last_i = ((rr_ + 1) * L + L - 128) // 128
                    if i == last_i:
                        pb = started.pop(lbk)
                        if d == 1:
                            P.op("act", lambda e, pb=pb, lb=lb, h=h: e.copy(out=acc[h][:, lb * 512:(lb + 1) * 512], in_=banks[pb][:]),
                                 reads=[bbank[pb]], pwrites=[b_acc[h]])
                        elif d == 4:
                            t0 = rr_ + 4 * ss_
                            P.op("dve", lambda e, pb=pb, t0=t0, h=h: e.tensor_tensor(
                                out=acc[h][:, _sl(t0, 512, 4)], in0=acc[h][:, _sl(t0, 512, 4)], in1=banks[pb][:], op=ALU.add),
                                reads=[bbank[pb], b_acc[h]], pwrites=[b_acc[h]])
                        else:
                            av = acc[h].rearrange("p (s r) -> p r s", r=16)[:, rr_:rr_ + 2, :]
                            P.op("dve", lambda e, pb=pb, av=av, h=h: e.tensor_tensor(
                                out=av, in0=av, in1=banks[pb][:].rearrange("p (a b) -> p a b", b=256), op=ALU.add),
                                reads=[bbank[pb], b_acc[h]], pwrites=[b_acc[h]])

            n_it = len(items)
            for idx in range(n_it + LA):
                if idx < n_it:
                    stage1(*items[idx], idx % NSL)
                if idx - LA >= 0:
                    stage2(*items[idx - LA], (idx - LA) % NSL)
            assert not started, started
            os_ = slice((1 - h) * 64, (2 - h) * 64)
            P.op("dve", lambda e, h=h, hs=hs, os_=os_: e.reciprocal(out=tmp[hs, :], in_=acc[h][os_, :]), reads=[b_acc[h]], writes=[b_tmp])
            P.op("pool", lambda e, h=h, hs=hs, hp=hp: e.tensor_tensor(out=mixedT[hs, hp, :], in0=acc[h][hs, :], in1=tmp[hs, :], op=ALU.mult),
                 reads=[b_acc[h], b_tmp], pwrites=[b_mixed])
    P.barrier()
    A.release(m0)


NSLOT = NEXP * CAP
TRASH = NSLOT


def phase_E(K, x, mixedT, b_mixed, banks, bbank, H, HN, SLOT_TOK, keep):
    nc, P, A, cst, ins = K.nc, K.P, K.A, K.cst, K.ins
    m0 = A.mark()
    ones_bf = A.alloc([128], BF16)
    b_ones = P.buf("ones")
    P.op("pool", lambda e: e.memset(ones_bf, 1.0), writes=[b_ones])
    cn = {}
    b_cn = {}
    for nm, w, dt in (("ident_f", 128, F32), ("Ltri", 128, F32), ("ones_f", 128, F32), ("eidx", 32, F32), ("tokid", 32, I32)):
        cn[nm] = A.alloc([w], dt)
        b_cn[nm] = P.buf(nm)
        P.op("sp", lambda e, nm=nm: e.dma_start(out=cn[nm], in_=cst[nm]), writes=[b_cn[nm]], dma="c_" + nm)
    gmix = A.alloc([8], F32)
    gain2 = A.alloc([D], F32)
    br = A.alloc([36], F32)
    wr = A.alloc([8, 36], F32)
    b_gmix, b_gain2, b_br, b_wr = P.buf("gmix"), P.buf("gain2"), P.buf("br"), P.buf("wr")
    P.op("sp", lambda e: e.dma_start(out=gmix, in_=ins["gmix"]), writes=[b_gmix], dma="c_gmix")
    P.op("sp", lambda e: e.dma_start(out=gain2, in_=ins["gain2_bc"]), writes=[b_gain2], dma="c_gain2")
    P.op("sp", lambda e: e.dma_start(out=br, in_=ins["br_bc"]), writes=[b_br], dma="c_br")
    P.op("sp", lambda e: e.dma_start(out=wr, in_=ins["wr"].rearrange("(dc p) c -> p dc c", p=128)), writes=[b_wr], dma="c_wr")
    Wout = A.alloc([8, D], BF16)
    b_Wout = P.buf("Wout")
    m1 = A.mark()
    wtmp = A.alloc([4, D], F32)
    b_wtmp = P.buf("wtmp")
    wo_v = ins["w_out"].rearrange("(c p) d -> p c d", p=128)
    for half in range(2):
        P.op("sp", lambda e, half=half: e.dma_start(out=wtmp, in_=wo_v[:, 4 * half:4 * half + 4, :]), writes=[b_wtmp], dma="c_wout")
        for c in range(4):
            cc = 4 * half + c
            P.op("dve", lambda e, c=c, cc=cc: e.tensor_scalar(out=Wout[:, cc, :], in0=wtmp[:, c, :], scalar1=gmix[:, cc:cc + 1], scalar2=None, op0=ALU.mult),
                 reads=[b_wtmp, b_gmix], pwrites=[b_Wout])
    zb = A.alloc([D], BF16)
    b_zb = P.buf("zb")
    P.op("pool", lambda e: e.memset(zb, 0.0), writes=[b_zb])
    b_HN, b_H, b_slot = keep["b_HN"], keep["b_H"], keep["b_slot"]
    P.op("sp", lambda e: e.dma_start(out=HN[S:S + 128, :], in_=zb), reads=[b_zb], pwrites=[b_HN], dma="hnz")
    nsl = (NSLOT + 128) // 128
    sinit = A.alloc([nsl], I32)
    b_sinit = P.buf("sinit")
    P.op("sp", lambda e: e.dma_start(out=sinit, in_=cst["slotinit"]), writes=[b_sinit], dma="c_sinit")
    P.op("sp", lambda e: e.dma_start(out=SLOT_TOK.rearrange("(p a) o -> p (a o)", p=128), in_=sinit), reads=[b_sinit], writes=[b_slot], dma="slot_init")

    sqb = A.alloc([4, 512], BF16)
    b_sqb = P.buf("sqb")
    lnv = A.alloc([512], F32)
    rbc = A.alloc([512], F32)
    b_lnv, b_rbc = P.buf("lnv"), P.buf("rbc")
    for g in range(NG):
        gs = slice(g * 512, (g + 1) * 512)
        P.op("pool", lambda e, gs=gs: e.tensor_tensor(out=sqb, in0=mixedT[:, 0:4, gs], in1=mixedT[:, 0:4, gs], op=ALU.mult),
             reads=[b_mixed], writes=[b_sqb])
        for c in range(4):
            P.op("pe", lambda e, c=c: e.matmul(banks[0][:], lhsT=ones_bf, rhs=sqb[:, c, :], start=(c == 0), stop=(c == 3)),
                 reads=[b_ones, b_sqb], **({"writes": [bbank[0]]} if c == 0 else {"pwrites": [bbank[0]]}))
        P.op("act", lambda e: e.activation(out=lnv, in_=banks[0][:], func=AF.Ln, scale=1.0 / 512, bias=EPS), reads=[bbank[0]], writes=[b_lnv])
        P.op("act", lambda e: e.activation(out=rbc, in_=lnv, func=AF.Exp, scale=-0.5), reads=[b_lnv], writes=[b_rbc])
        P.op("dve", lambda e, gs=gs: e.tensor_tensor(out=mixedT[:, 0:4, gs], in0=mixedT[:, 0:4, gs],
                                                      in1=rbc.unsqueeze(1).to_broadcast([128, 4, 512]), op=ALU.mult),
             reads=[b_rbc, b_mixed], pwrites=[b_mixed])
    P.barrier()
    A.release(m1)
    xin = [A.alloc([D], F32) for _ in range(2)]
    b_xin = P.bufs("exin", 2)
    hsb = [A.alloc([D], F32) for _ in range(2)]
    b_hsb = P.bufs("hsb", 2)
    junk = A.alloc([D], F32)
    b_junk = P.buf("ejunk")
    hn32_ = [A.alloc([D], F32) for _ in range(2)]
    b_hn32_ = P.bufs("hn32", 2)
    hnb = [A.alloc([D], BF16) for _ in range(2)]
    b_hnb = P.bufs("hnb", 2)
    hnT_ = [A.alloc([8, 128], F32) for _ in range(2)]
    b_hnT_ = P.bufs("hnT", 2)
    st = A.alloc([NT, 4], F32)
    b_st = P.bufs("est", NT)
    TB = 8
    LG = A.alloc([TB, 36], F32)
    gmax, gsum, ggate, m1_, m2_, dd, ee, den, p1, p2 = [A.alloc([TB], F32) for _ in range(10)]
    goh, gsh = A.alloc([TB, 4], F32), A.alloc([TB, 4], F32)
    sel, oh1, sel2, oh2 = [A.alloc([TB, 8], F32) for _ in range(4)]
    T32, E1, E2, Osum, base = [A.alloc([TB, 32], F32) for _ in range(5)]
    rk, eid, vv, dst = [A.alloc([TB, 2], F32) for _ in range(4)]
    Ocum = A.alloc([32], F32)
    b_Ocum = P.buf("Ocum")
    P.op("pool", lambda e: e.memset(Ocum, 0.0), writes=[b_Ocum])
    gates, dest = keep["gates"], keep["dest"]
    b_gates, b_dest = keep["b_gates"], keep["b_dest"]
    for t in range(NT):
        sl = t % 2
        hn32, b_hn32, hnT, b_hnT = hn32_[sl], b_hn32_[sl], hnT_[sl], b_hnT_[sl]
        ts_ = slice(t * 128, (t + 1) * 128)
        P.op("sp", lambda e, sl=sl, ts_=ts_: e.dma_start(out=xin[sl], in_=x[ts_, :]), writes=[b_xin[sl]], dma="exin%d" % sl)
        for half in range(2):
            bk = (1 + half) if (t % 2 == 0) else (0, 5)[half]
            for c in range(8):
                P.op("pe", lambda e, bk=bk, c=c, ts_=ts_, half=half: e.matmul(
                    banks[bk][:], lhsT=mixedT[:, c, ts_], rhs=Wout[:, c, half * 512:(half + 1) * 512], start=(c == 0), stop=(c == 7)),
                    reads=[b_mixed, b_Wout], **({"writes": [bbank[bk]]} if c == 0 else {"pwrites": [bbank[bk]]}))
            P.op("dve", lambda e, bk=bk, sl=sl, half=half: e.tensor_tensor(
                out=hsb[sl][:, half * 512:(half + 1) * 512], in0=xin[sl][:, half * 512:(half + 1) * 512], in1=banks[bk][:], op=ALU.add),
                reads=[bbank[bk], b_xin[sl]], **({"writes": [b_hsb[sl]]} if half == 0 else {"pwrites": [b_hsb[sl]]}))
        P.op("sp", lambda e, sl=sl, ts_=ts_: e.dma_start(out=H[ts_, :], in_=hsb[sl]), reads=[b_hsb[sl]], pwrites=[b_H], dma="hst%d" % sl)
        P.op("dve", lambda e, sl=sl, t=t: e.scalar_tensor_tensor(out=junk, in0=hsb[sl], scalar=1.0, in1=hsb[sl], op0=ALU.mult, op1=ALU.mult,
                                                             accum_out=st[:, t, 0:1]), reads=[b_hsb[sl]], writes=[b_junk, b_st[t]])
        P.op("act", lambda e, t=t: e.activation(out=st[:, t, 1:2], in_=st[:, t, 0:1], func=AF.Ln, scale=1.0 / D, bias=EPS), reads=[b_st[t]], writes=[b_st[t]])
        P.op("act", lambda e, t=t: e.activation(out=st[:, t, 2:3], in_=st[:, t, 1:2], func=AF.Exp, scale=-0.5), reads=[b_st[t]], writes=[b_st[t]])
        P.op("dve", lambda e, sl=sl, t=t, hn32=hn32: e.scalar_tensor_tensor(out=hn32, in0=hsb[sl], scalar=st[:, t, 2:3], in1=gain2, op0=ALU.mult, op1=ALU.mult),
             reads=[b_hsb[sl], b_st[t], b_gain2], writes=[b_hn32])
        P.op("act", lambda e, sl=sl, hn32=hn32: e.copy(out=hnb[sl], in_=hn32), reads=[b_hn32], writes=[b_hnb[sl]])
        P.op("sp", lambda e, sl=sl, ts_=ts_: e.dma_start(out=HN[ts_, :], in_=hnb[sl]), reads=[b_hnb[sl]], pwrites=[b_HN], dma="hnst%d" % sl)
        for half in range(2):
            bk = 3 + half
            pv = banks[bk][:].rearrange("p (a b) -> p a b", b=128)
            for j in range(4):
                dc = 4 * half + j
                P.op("pe", lambda e, pv=pv, j=j, dc=dc, hn32=hn32: e.transpose(pv[:, j, :], hn32[:, dc * 128:(dc + 1) * 128], cn["ident_f"]),
                     reads=[b_hn32, b_cn["ident_f"]], **({"writes": [bbank[bk]]} if j == 0 else {"pwrites": [bbank[bk]]}))
            P.op("act", lambda e, pv=pv, half=half, hnT=hnT: e.copy(out=hnT[:, 4 * half:4 * half + 4, :], in_=pv), reads=[bbank[bk]],
                 **({"writes": [b_hnT]} if half == 0 else {"pwrites": [b_hnT]}))
        for dc in range(8):
            P.op("pe", lambda e, dc=dc, hnT=hnT: e.matmul(banks[7][:, 384:420], lhsT=hnT[:, dc, :], rhs=wr[:, dc, :], start=(dc == 0), stop=(dc == 7)),
                 reads=[b_hnT, b_wr], **({"writes": [bbank[7]]} if dc == 0 else {"pwrites": [bbank[7]]}))
        j8 = t % TB
        if j8 == 0:
            bLG = P.buf("LG%d" % (t // TB))
        P.op("dve", lambda e, j8=j8: e.tensor_tensor(out=LG[:, j8, :], in0=banks[7][:, 384:420], in1=br, op=ALU.add),
             reads=[bbank[7], b_br], **({"writes": [bLG]} if j8 == 0 else {"pwrites": [bLG]}))
        if j8 != TB - 1:
            continue
        t0 = t - (TB - 1)
        bR = P.buf("RB%d" % (t // TB))

        def dv(fn, reads=(), writes=(), eng="dve"):
            P.op(eng, fn, reads=[bR, bLG] + list(reads), writes=[bR] + list(writes))
        gl = LG[:, :, 0:4]
        bc3 = lambda ap2, n: ap2.unsqueeze(2).to_broadcast([128, TB, n])
        dv(lambda e: e.tensor_reduce(out=gmax, in_=gl, axis=AX.X, op=ALU.max))
        dv(lambda e: e.tensor_tensor(out=goh, in0=gl, in1=bc3(gmax, 4), op=ALU.is_equal))
        dv(lambda e: e.tensor_tensor(out=gsh, in0=gl, in1=bc3(gmax, 4), op=ALU.subtract))
        dv(lambda e: e.activation(out=gsh, in_=gsh, func=AF.Exp), eng="act")
        dv(lambda e: e.tensor_reduce(out=gsum, in_=gsh, axis=AX.X, op=ALU.add))
        dv(lambda e: e.reciprocal(out=ggate, in_=gsum))
        for g in range(4):
            dv(lambda e, g=g: e.tensor_tensor(out=T32[:, :, g * 8:(g + 1) * 8], in0=LG[:, :, 4 + g * 8:12 + g * 8],
                                              in1=goh[:, :, g:g + 1].to_broadcast([128, TB, 8]), op=ALU.mult))
        dv(lambda e: e.tensor_reduce(out=sel, in_=T32.rearrange("p t (g j) -> p t j g", j=8), axis=AX.X, op=ALU.add))
        dv(lambda e: e.tensor_reduce(out=m1_, in_=sel, axis=AX.X, op=ALU.max))
        dv(lambda e: e.tensor_tensor(out=oh1, in0=sel, in1=bc3(m1_, 8), op=ALU.is_equal))
        dv(lambda e: e.scalar_tensor_tensor(out=sel2, in0=oh1, scalar=-1.0e30, in1=sel, op0=ALU.mult, op1=ALU.add))
        dv(lambda e: e.tensor_reduce(out=m2_, in_=sel2, axis=AX.X, op=ALU.max))
        dv(lambda e: e.tensor_tensor(out=oh2, in0=sel2, in1=bc3(m2_, 8), op=ALU.is_equal))
        dv(lambda e: e.tensor_tensor(out=dd, in0=m2_, in1=m1_, op=ALU.subtract))
        dv(lambda e: e.activation(out=ee, in_=dd, func=AF.Exp), eng="act")
        dv(lambda e: e.tensor_scalar(out=den, in0=ee, scalar1=1.0, scalar2=None, op0=ALU.add))
        dv(lambda e: e.reciprocal(out=p1, in_=den))
        dv(lambda e: e.tensor_tensor(out=p2, in0=ee, in1=p1, op=ALU.mult))
        P.op("dve", lambda e, t0=t0: e.tensor_tensor(out=gates[:, t0:t0 + TB, 0], in0=p1, in1=ggate, op=ALU.mult), reads=[bR], pwrites=[b_gates])
        P.op("dve", lambda e, t0=t0: e.tensor_tensor(out=gates[:, t0:t0 + TB, 1], in0=p2, in1=ggate, op=ALU.mult), reads=[bR], pwrites=[b_gates])
        for g in range(4):
            dv(lambda e, g=g: e.tensor_tensor(out=E1[:, :, g * 8:(g + 1) * 8], in0=oh1, in1=goh[:, :, g:g + 1].to_broadcast([128, TB, 8]), op=ALU.mult))
            dv(lambda e, g=g: e.tensor_tensor(out=E2[:, :, g * 8:(g + 1) * 8], in0=oh2, in1=goh[:, :, g:g + 1].to_broadcast([128, TB, 8]), op=ALU.mult))
        dv(lambda e: e.tensor_tensor(out=Osum, in0=E1, in1=E2, op=ALU.add))
        Of = Osum.rearrange("p t e -> p (t e)")
        P.op("pe", lambda e, Of=Of: e.matmul(banks[6][:, 0:TB * 32], lhsT=cn["Ltri"], rhs=Of, start=True, stop=True),
             reads=[bR, b_cn["Ltri"]], writes=[bbank[6]])
        P.op("pe", lambda e, Of=Of: e.matmul(banks[7][:, 0:TB * 32], lhsT=cn["ones_f"], rhs=Of, start=True, stop=True),
             reads=[bR, b_cn["ones_f"]], writes=[bbank[7]])
        cs3 = banks[7][:, 0:TB * 32].rearrange("p (t e) -> p t e", e=32)
        dv(lambda e: e.tensor_copy(out=base[:, 0, :], in_=Ocum), reads=[b_Ocum])
        for jj in range(1, TB):
            dv(lambda e, jj=jj: e.tensor_tensor(out=base[:, jj, :], in0=base[:, jj - 1, :], in1=cs3[:, jj - 1, :], op=ALU.add), reads=[bbank[7]])
        P.op("dve", lambda e: e.tensor_tensor(out=Ocum, in0=base[:, TB - 1, :], in1=cs3[:, TB - 1, :], op=ALU.add), reads=[bR, bbank[7]], writes=[b_Ocum])
        dv(lambda e: e.tensor_tensor(out=base, in0=base, in1=banks[6][:, 0:TB * 32].rearrange("p (t e) -> p t e", e=32), op=ALU.add), reads=[bbank[6]])
        for k, Ek in ((0, E1), (1, E2)):
            dv(lambda e, Ek=Ek: e.tensor_tensor(out=T32, in0=Ek, in1=base, op=ALU.mult))
            dv(lambda e, k=k: e.tensor_reduce(out=rk[:, :, k], in_=T32, axis=AX.X, op=ALU.add))
            dv(lambda e, Ek=Ek: e.tensor_tensor(out=T32, in0=Ek, in1=cn["eidx"].unsqueeze(1).to_broadcast([128, TB, 32]), op=ALU.mult), reads=[b_cn["eidx"]])
            dv(lambda e, k=k: e.tensor_reduce(out=eid[:, :, k], in_=T32, axis=AX.X, op=ALU.add))
        dv(lambda e: e.tensor_scalar(out=vv, in0=rk, scalar1=float(CAP), scalar2=None, op0=ALU.is_lt))
        dv(lambda e: e.scalar_tensor_tensor(out=dst, in0=eid, scalar=float(CAP), in1=rk, op0=ALU.mult, op1=ALU.add))
        dv(lambda e: e.scalar_tensor_tensor(out=dst, in0=dst, scalar=-float(TRASH), in1=vv, op0=ALU.add, op1=ALU.mult))
        dv(lambda e: e.tensor_scalar(out=dst, in0=dst, scalar1=float(TRASH), scalar2=None, op0=ALU.add))
        P.op("dve", lambda e, t0=t0: e.tensor_copy(out=dest[:, t0:t0 + TB, :], in_=dst), reads=[bR], pwrites=[b_dest])
        for tt_ in range(t0, t0 + TB):
            for k in range(2):
                P.op("pool", lambda e, tt_=tt_, k=k: e.indirect_dma_start(
                    out=SLOT_TOK[:, :], out_offset=bass.IndirectOffsetOnAxis(ap=dest[:, tt_, k:k + 1], axis=0),
                    in_=cn["tokid"][:, tt_:tt_ + 1], in_offset=None),
                    reads=[b_dest, b_cn["tokid"], b_slot], pwrites=[b_slot], dma="scat")
    P.barrier()
    A.release(m0)


def phase_F(K, banks, bbank, ident_bf, b_ident, HN, SLOT_TOK, Y, keep):
    nc, P, A, cst, ins = K.nc, K.P, K.A, K.cst, K.ins
    m0 = A.mark()
    b_HN, b_slot, b_Y = keep["b_HN"], keep["b_slot"], keep["b_Y"]
    NM = CAP // 128
    zf = A.alloc([D], BF16)
    b_zf = P.buf("zf")
    P.op("pool", lambda e: e.memset(zf, 0.0), writes=[b_zf])
    P.op("sp", lambda e: e.dma_start(out=Y[NSLOT:NSLOT + 128, :], in_=zf), reads=[b_zf], pwrites=[b_Y], dma="yz")
    NW = 3
    Wg = [A.alloc([8, FF], BF16) for _ in range(NW)]
    Wu = [A.alloc([8, FF], BF16) for _ in range(NW)]
    Wd = [A.alloc([4, D], BF16) for _ in range(NW)]
    b_Wg, b_Wu, b_Wd = P.bufs("Wg", NW), P.bufs("Wu", NW), P.bufs("Wd", NW)
    idx = [A.alloc([NM], I32) for _ in range(NW)]
    b_idx = P.bufs("idx", NW)
    Xe = [A.alloc([D], BF16) for _ in range(NM)]
    b_Xe = P.bufs("Xe", NM)
    XeT = [A.alloc([8, CAP], BF16) for _ in range(2)]
    b_XeT = P.bufs("XeT", 2)
    sgt = [A.alloc([CAP], F32) for _ in range(2)]
    b_sgt = P.bufs("sgt", 2)
    hT = [A.alloc([4, CAP], BF16) for _ in range(2)]
    b_hT = P.bufs("hT", 2)
    ysb = [A.alloc([D], BF16) for _ in range(2)]
    b_ysb = P.bufs("ysb", 2)
    wg_v = K.WEB["g"].rearrange("e (dc p) f -> e p dc f", p=128)
    wu_v = K.WEB["u"].rearrange("e (dc p) f -> e p dc f", p=128)
    wd_v = K.WEB["d"].rearrange("e (fc p) d -> e p fc d", p=128)
    slot_v = SLOT_TOK[0:NSLOT, :].rearrange("(e p m) o -> e p (m o)", p=128, m=NM)
    y_v = Y[0:NSLOT, :].rearrange("(e p m) d -> e p m d", p=128, m=NM)
    xi = 0
    yi = 0
    gi = 0
    xslot = {}

    def stage_load(ex):
        ww = ex % NW
        P.op("sp", lambda e: e.dma_start(out=Wg[ww], in_=wg_v[ex]), reads=[K.b_web], writes=[b_Wg[ww]], dma="wg%d" % ww)
        P.op("sp", lambda e: e.dma_start(out=Wu[ww], in_=wu_v[ex]), reads=[K.b_web], writes=[b_Wu[ww]], dma="wu%d" % ww)
        P.op("sp", lambda e: e.dma_start(out=Wd[ww], in_=wd_v[ex]), reads=[K.b_web], writes=[b_Wd[ww]], dma="wd%d" % ww)
        P.op("sp", lambda e: e.dma_start(out=idx[ww], in_=slot_v[ex]), reads=[b_slot], writes=[b_idx[ww]], dma="idx%d" % ww)

    def stage_gather(ex):
        ww = ex % NW
        for m in range(NM):
            P.op("pool", lambda e, ww=ww, m=m: e.indirect_dma_start(
                out=Xe[m], out_offset=None, in_=HN[:, :], in_offset=bass.IndirectOffsetOnAxis(ap=idx[ww][:, m:m + 1], axis=0)),
                reads=[b_idx[ww], b_HN], writes=[b_Xe[m]], dma="gx%d" % m)

    def stage_transpose(ex):
        ws = ex % 2
        for m in range(NM):
            bk = m % 2
            pb = banks[bk][:].bitcast(BF16).rearrange("p (a b) -> p a b", b=128)
            for dc in range(8):
                P.op("pe", lambda e, pb=pb, dc=dc, m=m: e.transpose(pb[:, dc, :], Xe[m][:, dc * 128:(dc + 1) * 128], ident_bf),
                     reads=[b_Xe[m], b_ident], **({"writes": [bbank[bk]]} if dc == 0 else {"pwrites": [bbank[bk]]}))
            P.op("act", lambda e, pb=pb, ws=ws, m=m: e.copy(out=XeT[ws][:, :, m * 128:(m + 1) * 128], in_=pb), reads=[bbank[bk]],
                 **({"writes": [b_XeT[ws]]} if m == 0 else {"pwrites": [b_XeT[ws]]}))

    stage_load(0)
    stage_load(1)
    stage_gather(0)
    stage_transpose(0)
    for ex in range(NEXP):
        ws = ex % 2
        ww = ex % NW
        if ex + 2 < NEXP:
            stage_load(ex + 2)
        if ex + 1 < NEXP:
            stage_gather(ex + 1)
        for fc in range(4):
            bg = 2 + (gi % 2)
            bu = 4 + (gi % 2)
            s2 = gi % 2
            gi += 1
            for dc in range(8):
                P.op("pe", lambda e, bg=bg, ws=ws, ww=ww, dc=dc, fc=fc: e.matmul(banks[bg][:, 0:CAP], lhsT=Wg[ww][:, dc, fc * 128:(fc + 1) * 128], rhs=XeT[ws][:, dc, :],
                                                                         start=(dc == 0), stop=(dc == 7)),
                     reads=[b_Wg[ww], b_XeT[ws]], **({"writes": [bbank[bg]]} if dc == 0 else {"pwrites": [bbank[bg]]}))
            for dc in range(8):
                P.op("pe", lambda e, bu=bu, ws=ws, ww=ww, dc=dc, fc=fc: e.matmul(banks[bu][:, 0:CAP], lhsT=Wu[ww][:, dc, fc * 128:(fc + 1) * 128], rhs=XeT[ws][:, dc, :],
                                                                         start=(dc == 0), stop=(dc == 7)),
                     reads=[b_Wu[ww], b_XeT[ws]], **({"writes": [bbank[bu]]} if dc == 0 else {"pwrites": [bbank[bu]]}))
            P.op("act", lambda e, bg=bg, s2=s2: e.activation(out=sgt[s2], in_=banks[bg][:, 0:CAP], func=AF.Silu), reads=[bbank[bg]], writes=[b_sgt[s2]])
            P.op("dve", lambda e, bu=bu, s2=s2, ws=ws, fc=fc: e.tensor_tensor(out=hT[ws][:, fc, :], in0=sgt[s2], in1=banks[bu][:, 0:CAP], op=ALU.mult),
                 reads=[b_sgt[s2], bbank[bu]], **({"writes": [b_hT[ws]]} if fc == 0 else {"pwrites": [b_hT[ws]]}))
        if ex + 1 < NEXP:
            stage_transpose(ex + 1)
        for m in range(NM):
            y2 = yi % 2
            yi += 1
            for half in range(2):
                by_ = 6 + half
                for fc in range(4):
                    P.op("pe", lambda e, by_=by_, ws=ws, ww=ww, fc=fc, m=m, half=half: e.matmul(
                        banks[by_][:], lhsT=hT[ws][:, fc, m * 128:(m + 1) * 128], rhs=Wd[ww][:, fc, half * 512:(half + 1) * 512], start=(fc == 0), stop=(fc == 3)),
                        reads=[b_hT[ws], b_Wd[ww]], **({"writes": [bbank[by_]]} if fc == 0 else {"pwrites": [bbank[by_]]}))
                if half == 0:
                    P.op("act", lambda e, by_=by_, y2=y2: e.copy(out=ysb[y2][:, 0:512], in_=banks[by_][:]), reads=[bbank[by_]], writes=[b_ysb[y2]])
                else:
                    P.op("dve", lambda e, by_=by_, y2=y2: e.tensor_copy(out=ysb[y2][:, 512:1024], in_=banks[by_][:]), reads=[bbank[by_]], pwrites=[b_ysb[y2]])
            P.op("sp", lambda e, ex=ex, m=m, y2=y2: e.dma_start(out=y_v[ex][:, m, :], in_=ysb[y2]), reads=[b_ysb[y2]], pwrites=[b_Y], dma="yst%d" % y2)
    P.barrier()
    A.release(m0)


def phase_G(K, H, Y, y, keep):
    nc, P, A, cst, ins = K.nc, K.P, K.A, K.cst, K.ins
    m0 = A.mark()
    gates, dest = keep["gates"], keep["dest"]
    b_gates, b_dest, b_H, b_Y = keep["b_gates"], keep["b_dest"], keep["b_H"], keep["b_Y"]
    fg = A.alloc([D], F32)
    b_fg = P.buf("fg")
    P.op("sp", lambda e: e.dma_start(out=fg, in_=ins["gain3_bc"]), writes=[b_fg], dma="c_gain3")
    hs_ = [A.alloc([D], F32) for _ in range(3)]
    y1 = [A.alloc([D], BF16) for _ in range(3)]
    y2 = [A.alloc([D], BF16) for _ in range(3)]
    ot = [A.alloc([D], F32) for _ in range(3)]
    b_hs, b_y1, b_y2, b_ot = P.bufs("ghs", 3), P.bufs("gy1", 3), P.bufs("gy2", 3), P.bufs("got", 3)
    junk = A.alloc([D], F32)
    b_junk = P.buf("gjunk")
    st = A.alloc([NT, 4], F32)
    b_st = P.bufs("gst", NT)
    for t in range(NT):
        sl = t % 3
        ts_ = slice(t * 128, (t + 1) * 128)
        P.op("sp", lambda e, sl=sl, ts_=ts_: e.dma_start(out=hs_[sl], in_=H[ts_, :]), reads=[b_H], writes=[b_hs[sl]], dma="gh%d" % sl)
        for k, (yy, bb) in enumerate(((y1, b_y1), (y2, b_y2))):
            P.op("pool", lambda e, sl=sl, t=t, k=k, yy=yy: e.indirect_dma_start(
                out=yy[sl], out_offset=None, in_=Y[:, :], in_offset=bass.IndirectOffsetOnAxis(ap=dest[:, t, k:k + 1], axis=0)), reads=[b_dest, b_Y], writes=[bb[sl]], dma="gy%d_%d" % (k, sl))
        P.op("dve", lambda e, sl=sl, t=t: e.scalar_tensor_tensor(out=ot[sl], in0=y1[sl], scalar=gates[:, t, 0:1], in1=hs_[sl], op0=ALU.mult, op1=ALU.add),
             reads=[b_y1[sl], b_gates, b_hs[sl]], writes=[b_ot[sl]])
        P.op("dve", lambda e, sl=sl, t=t: e.scalar_tensor_tensor(out=ot[sl], in0=y2[sl], scalar=gates[:, t, 1:2], in1=ot[sl], op0=ALU.mult, op1=ALU.add),
             reads=[b_y2[sl], b_gates, b_ot[sl]], writes=[b_ot[sl]])
        P.op("dve", lambda e, sl=sl, t=t: e.scalar_tensor_tensor(out=junk, in0=ot[sl], scalar=1.0, in1=ot[sl], op0=ALU.mult, op1=ALU.mult,
                                                             accum_out=st[:, t, 0:1]), reads=[b_ot[sl]], writes=[b_junk, b_st[t]])
        P.op("act", lambda e, t=t: e.activation(out=st[:, t, 1:2], in_=st[:, t, 0:1], func=AF.Ln, scale=1.0 / D, bias=EPS), reads=[b_st[t]], writes=[b_st[t]])
        P.op("act", lambda e, t=t: e.activation(out=st[:, t, 2:3], in_=st[:, t, 1:2], func=AF.Exp, scale=-0.5), reads=[b_st[t]], writes=[b_st[t]])
        P.op("dve", lambda e, sl=sl, t=t: e.scalar_tensor_tensor(out=ot[sl], in0=ot[sl], scalar=st[:, t, 2:3], in1=fg, op0=ALU.mult, op1=ALU.mult),
             reads=[b_ot[sl], b_st[t], b_fg], writes=[b_ot[sl]])
        P.op("sp", lambda e, sl=sl, ts_=ts_: e.dma_start(out=y[ts_, :], in_=ot[sl]), reads=[b_ot[sl]], dma="yout%d" % sl)
    A.release(m0)


_CACHE = {}


def kernel(**inputs):
    x = np.ascontiguousarray(np.asarray(inputs["x"], dtype=np.float32))
    dbg = tuple(sorted(k for k, v in DEBUG.items() if v))
    if dbg not in _CACHE:
        _CACHE[dbg] = build(dbg)
    nc, K = _CACHE[dbg]
    cst = _consts()
    common = {
        "w_in": np.ascontiguousarray(inputs["w_in"][0]),
        "gain1_bc": np.ascontiguousarray(np.broadcast_to(inputs["mix_norm_gain"][0][None, :], (128, D))),
    }
    df = np.asarray(inputs["ret_decay_fwd"][0], np.float32)
    db = np.asarray(inputs["ret_decay_bwd"][0], np.float32)
    dec = np.zeros((128, 24), np.float32)
    dec[:, 0:8] = df[None, :]
    dec[:, 8:16] = db[None, :]
    for hp in range(4):
        dec[0:64, 16 + hp] = df[2 * hp]
        dec[64:128, 16 + hp] = df[2 * hp + 1]
        dec[0:64, 20 + hp] = db[2 * hp]
        dec[64:128, 20 + hp] = db[2 * hp + 1]
    common["dec_bc"] = dec
    f32 = lambda a: np.ascontiguousarray(np.asarray(a, dtype=np.float32))
    bc = lambda v: np.ascontiguousarray(np.broadcast_to(np.asarray(v, np.float32)[None, :], (128, len(v))))
    common["w_out"] = f32(inputs["w_out"][0])
    gcat = np.concatenate([np.asarray(inputs["attn_out_gain"][0], np.float32), np.asarray(inputs["ret_out_gain"][0], np.float32)])
    common["gmix"] = np.ascontiguousarray(gcat.reshape(8, 128).T)
    common["gain2_bc"] = bc(inputs["ffn_norm_gain"][0])
    common["gain3_bc"] = bc(inputs["final_norm_gain"])
    common["br_bc"] = bc(np.concatenate([np.asarray(inputs["b_route_group"][0], np.float32), np.asarray(inputs["b_route_expert"][0], np.float32)]))
    common["wr"] = np.ascontiguousarray(np.concatenate([np.asarray(inputs["w_route_group"][0], np.float32),
                                                        np.asarray(inputs["w_route_expert"][0], np.float32)], axis=1))
    common["w_eg"] = f32(inputs["w_expert_gate"][0])
    common["w_eu"] = f32(inputs["w_expert_up"][0])
    common["w_ed"] = f32(inputs["w_expert_down"][0])
    common.update(cst)
    in_maps = []
    ncores = LIMIT.get("ncores", 8)
    for b in range(ncores):
        m = {"x": x[b]}
        m.update(common)
        in_maps.append({k: m[k] for k in K.in_names})
    res = run_bass_kernel_spmd(nc, in_maps, core_ids=list(range(ncores)))
    kernel.last = res
    out = np.stack([np.asarray(r["y"]) for r in res.results] + [np.zeros((S, D), np.float32)] * (8 - ncores), axis=0).astype(np.float32)
    return out
```

```python
import contextlib
import numpy as np
import ml_dtypes
import concourse.bass as bass
import concourse.mybir as mybir
from concourse.bass_utils import run_bass_kernel_spmd

F32 = mybir.dt.float32
BF16 = mybir.dt.bfloat16
I32 = mybir.dt.int32
U32 = mybir.dt.uint32
ALU = mybir.AluOpType
AF = mybir.ActivationFunctionType
AX = mybir.AxisListType

S = 4096
D = 1024
NT = 32
NG = 8
EPS = 1e-6
CAP = 512
NEXP = 32
FF = 512

LIMIT = {}
DEBUG = {}

ENGS = ("pe", "act", "dve", "pool", "sp")


class Buf:
    __slots__ = ("name", "writers", "readers", "excl")

    def __init__(self, name, excl=False):
        self.name = name
        self.writers = {}
        self.readers = {}
        self.excl = excl


class Op:
    __slots__ = ("eng", "fn", "deps", "dma", "tok", "signal", "idx")


class Prog:
    def __init__(self, nc):
        self.nc = nc
        self.ops = {e: [] for e in ENGS}
        self.dma_sems = {}
        self.same_engine_sync = True
        self.bar = set()

    def buf(self, name="b"):
        return Buf(name)

    def bufs(self, name, n, excl=False):
        return [Buf("%s%d" % (name, i), excl) for i in range(n)]

    def barrier(self):
        toks = set()
        for e in ENGS:
            for o in reversed(self.ops[e]):
                if o.dma is None:
                    toks.add(("c", e, o.idx))
                    break
        for k, c in self.dma_sems.items():
            toks.add(("d", k, c[0]))
        self.bar = toks

    def op(self, eng, fn, reads=(), writes=(), pwrites=(), dma=None):
        o = Op()
        o.eng = eng
        o.fn = fn
        o.dma = dma
        o.signal = False
        lst = self.ops[eng]
        o.idx = len(lst)
        if dma is None:
            tok = ("c", eng, o.idx)
            key = eng
        else:
            cnt = self.dma_sems.setdefault(dma, [0])
            cnt[0] += 1
            tok = ("d", dma, cnt[0])
            key = "dma:" + dma
        deps = set(self.bar)
        for b in reads:
            deps.update(b.writers.values())
            if b.excl:
                deps.update(v for k, v in b.readers.items() if k != eng)
        for b in writes:
            deps.update(b.writers.values())
            deps.update(b.readers.values())
        for b in pwrites:
            deps.update(b.readers.values())
            deps.update(v for k, v in b.writers.items() if k != key)
        o.tok = tok
        o.deps = deps
        for b in reads:
            b.readers[key] = tok
        for b in writes:
            b.writers = {key: tok}
            b.readers = {}
        for b in pwrites:
            b.writers[key] = tok
        lst.append(o)
        return o

    def emit(self, block, stack):
        nc = self.nc
        plan = {}
        for e in ENGS:
            known = {}
            plist = []
            for o in self.ops[e]:
                need = {}
                for t in o.deps:
                    if t[0] == "c":
                        if t[1] == e and (e == "pe" or e == "sp" or not self.same_engine_sync):
                            continue
                        if t[1] == e and t[2] >= o.idx:
                            continue
                        k = ("c", t[1])
                    else:
                        k = ("d", t[1])
                    if known.get(k, -1) >= t[2]:
                        continue
                    if need.get(k, -1) < t[2]:
                        need[k] = t[2]
                for k, v in need.items():
                    known[k] = v
                    if k[0] == "c":
                        self.ops[k[1]][v].signal = True
                plist.append(need)
            plan[e] = plist
        self.plan = plan
        sems = {}
        for e in ENGS:
            sems[("c", e)] = stack.enter_context(nc.semaphore("s_" + e))
        for k in self.dma_sems:
            sems[("d", k)] = stack.enter_context(nc.semaphore("d_" + k))
        sigcount = {}
        self.sigcount = sigcount
        for e in ENGS:
            c = 0
            arr = []
            for o in self.ops[e]:
                if o.signal:
                    c += 1
                arr.append(c)
            sigcount[e] = arr
        handles = {"pe": "tensor", "act": "scalar", "dve": "vector", "pool": "gpsimd", "sp": "sync"}
        dma_totals = {k: v[0] for k, v in self.dma_sems.items()}

        def make(e):
            def body(eng):
                for o, need in zip(self.ops[e], plan[e]):
                    for k, v in need.items():
                        if k[0] == "c":
                            eng.wait_ge(sems[k], sigcount[k[1]][v])
                        else:
                            eng.wait_ge(sems[k], 16 * v)
                    inst = o.fn(eng)
                    if o.dma is not None:
                        inst.then_inc(sems[("d", o.dma)], 16)
                    elif o.signal:
                        inst.then_inc(sems[("c", e)], 1)
                if e == "sp":
                    for k, v in dma_totals.items():
                        eng.wait_ge(sems[("d", k)], 16 * v)
            return body

        for e in ENGS:
            getattr(block, handles[e])(make(e))
        self.stats = {e: len(self.ops[e]) for e in ENGS}
        self.stats["nsem"] = len(sems)


class Arena:
    def __init__(self, nc, stack, nbytes):
        self.t = stack.enter_context(nc.sbuf_tensor("arena", [128, nbytes // 4], F32))
        self.top = 0
        self.cap = nbytes
        self.peak = 0

    def mark(self):
        return self.top

    def release(self, m):
        self.top = m

    def alloc(self, free_shape, dt, parts=128):
        esz = 4 if dt in (F32, I32, U32) else 2
        n = int(np.prod(free_shape)) * esz
        n = (n + 63) // 64 * 64
        off = self.top
        self.top += n
        self.peak = max(self.peak, self.top)
        assert self.top <= self.cap, ("SBUF arena overflow", self.top, self.cap)
        a = self.t[:, off // 4:(off + n) // 4]
        if dt != F32:
            a = a.bitcast(dt)
        a = a[:, 0:int(np.prod(free_shape))]
        if len(free_shape) == 2:
            a = a.rearrange("p (a b) -> p a b", b=free_shape[1])
        elif len(free_shape) == 3:
            a = a.rearrange("p (a b c) -> p a b c", b=free_shape[1], c=free_shape[2])
        return a


def _consts():
    c = {}
    c["ident_bf"] = np.eye(128, dtype=np.float32).astype(ml_dtypes.bfloat16)
    c["ident_f"] = np.eye(128, dtype=np.float32)
    pos = np.arange(S, dtype=np.float32)
    fa = (np.float32(500000.0) ** (-np.arange(0, 16, 2, dtype=np.float32) / np.float32(16))).astype(np.float32)
    fr = (np.float32(10000.0) ** (-np.linspace(0.0, 1.0, 32, dtype=np.float32))).astype(np.float32)
    cosA = np.ones((128, S), np.float32)
    sinA = np.zeros((128, S), np.float32)
    cosR = np.zeros((128, S), np.float32)
    sinR = np.zeros((128, S), np.float32)
    RA = np.zeros((128, 128), np.float32)
    RR = np.zeros((128, 128), np.float32)
    for p in range(128):
        dd = p % 64
        hb = p - dd
        if dd < 16:
            ang = (pos * fa[dd % 8]).astype(np.float32)
            cosA[p] = np.cos(ang)
            sinA[p] = np.sin(ang)
            if dd < 8:
                RA[hb + dd + 8, p] = -1.0
            else:
                RA[hb + dd - 8, p] = 1.0
        ang = (pos * fr[dd % 32]).astype(np.float32)
        cosR[p] = np.cos(ang)
        sinR[p] = np.sin(ang)
        if dd < 32:
            RR[hb + dd + 32, p] = -1.0
        else:
            RR[hb + dd - 32, p] = 1.0
    c["cosA"], c["sinA"], c["cosR"], c["sinR"] = cosA, sinA, cosR, sinR
    BIG = 1.0e7
    jj = np.arange(128, dtype=np.float32)[:, None]
    ii = np.arange(128, dtype=np.float32)[None, :]
    c["Mf"] = np.where(ii >= jj, ii - jj, BIG).astype(np.float32)
    c["Mb"] = np.where(jj > ii, jj - ii, BIG).astype(np.float32)
    c["iq"] = np.broadcast_to(ii + 1.0, (128, 128)).astype(np.float32).copy()
    c["iqb"] = np.broadcast_to(128.0 - ii, (128, 128)).astype(np.float32).copy()
    bav = np.zeros((128, 128), np.float32)
    bav[0:64, 0:64] = 1.0 / 64
    bav[64:128, 64:128] = 1.0 / 64
    c["Bavg"] = bav
    c["pidx"] = np.stack([127.0 - np.arange(128), np.arange(128)], axis=1).astype(np.float32)
    c["Ltri"] = (jj < ii).astype(np.float32)
    c["ones_f"] = np.ones((128, 128), np.float32)
    c["eidx"] = np.broadcast_to(np.arange(32, dtype=np.float32)[None, :], (128, 32)).copy()
    c["tokid"] = (np.arange(32, dtype=np.int32)[None, :] * 128 + np.arange(128, dtype=np.int32)[:, None]).astype(np.int32)
    c["slotinit"] = np.full((128, (NSLOT + 128) // 128), S, np.int32)
    aa = np.arange(128)[:, None]
    cc_ = np.arange(256)[None, :]
    c["amask"] = ((cc_ >= aa) & (cc_ <= aa + 128)).astype(np.float32).astype(ml_dtypes.bfloat16)
    c["RA"] = RA.astype(ml_dtypes.bfloat16)
    c["RR"] = RR.astype(ml_dtypes.bfloat16)
    return c


class KB:
    pass


def _dram_in(K, name, shape, dt):
    K.in_names.append(name)
    return K.nc.dram_tensor(name, list(shape), dt, kind="ExternalInput").ap()


def build(debug=()):
    nc = bass.Bass("TRN2", target_bir_lowering=False)
    K = KB()
    K.nc = nc
    K.in_names = []
    K.debug = set(debug)
    K.outs = []
    stack = contextlib.ExitStack()
    K.stack = stack
    P = Prog(nc)
    K.P = P
    x = _dram_in(K, "x", [S, D], F32)
    w_in = _dram_in(K, "w_in", [D, 3584], F32)
    gain1 = _dram_in(K, "gain1_bc", [128, D], F32)
    cst = {}
    for nm, shp, dt in [("ident_bf", [128, 128], BF16), ("ident_f", [128, 128], F32),
                        ("cosA", [128, S], F32), ("sinA", [128, S], F32),
                        ("cosR", [128, S], F32), ("sinR", [128, S], F32),
                        ("RA", [128, 128], BF16), ("RR", [128, 128], BF16)]:
        cst[nm] = _dram_in(K, nm, shp, dt)
    for nm, shp in [("Mf", [128, 128]), ("Mb", [128, 128]), ("iq", [128, 128]), ("iqb", [128, 128]),
                    ("Bavg", [128, 128]), ("pidx", [128, 2])]:
        cst[nm] = _dram_in(K, nm, shp, F32)
    cst["amask"] = _dram_in(K, "amask", [128, 256], BF16)
    K.cst = cst
    K.ins = {}
    K.ins["dec_bc"] = _dram_in(K, "dec_bc", [128, 24], F32)
    for nm, shp in [("w_out", [D, D]), ("gmix", [128, 8]), ("gain2_bc", [128, D]), ("br_bc", [128, 36]), ("wr", [D, 36]),
                    ("w_eg", [NEXP, D, FF]), ("w_eu", [NEXP, D, FF]), ("w_ed", [NEXP, FF, D]), ("gain3_bc", [128, D])]:
        K.ins[nm] = _dram_in(K, nm, shp, F32)
    for nm, shp in [("Ltri", [128, 128]), ("ones_f", [128, 128]), ("eidx", [128, 32])]:
        cst[nm] = _dram_in(K, nm, shp, F32)
    cst["tokid"] = _dram_in(K, "tokid", [128, 32], I32)
    cst["slotinit"] = _dram_in(K, "slotinit", [128, (NSLOT + 128) // 128], I32)
    qkv_kind = "ExternalOutput" if "qkv" in K.debug else "Internal"
    QKV = nc.dram_tensor("QKV", [28, 128, S], BF16, kind=qkv_kind).ap()
    if "qkv" in K.debug:
        K.outs.append("QKV")
    y = nc.dram_tensor("y", [S, D], F32, kind="ExternalOutput").ap()
    K.outs.append("y")

    A = Arena(nc, stack, 196 * 1024)
    K.A = A
    banks = [stack.enter_context(nc.psum_tensor("bank%d" % i, [128, 512], F32)) for i in range(8)]
    bbank = P.bufs("bank", 8, excl=True)

    ident_bf = A.alloc([128], BF16)
    b_ident = P.buf("ident")
    P.op("sp", lambda e: e.dma_start(out=ident_bf, in_=cst["ident_bf"]), writes=[b_ident], dma="c_ident")

    WEB = {nm: nc.dram_tensor("WEB_" + nm, shp, BF16, kind="Internal").ap()
           for nm, shp in (("g", [NEXP, D, FF]), ("u", [NEXP, D, FF]), ("d", [NEXP, FF, D]))}
    K.WEB = WEB
    b_web = P.buf("web")
    K.b_web = b_web
    K.precast_next = 0

    def precast(n):
        for _ in range(n):
            ex = K.precast_next
            if ex >= NEXP:
                return
            K.precast_next += 1
            for nm, src in (("g", "w_eg"), ("u", "w_eu"), ("d", "w_ed")):
                P.op("pool", lambda e, nm=nm, src=src, ex=ex: e.dma_start(out=WEB[nm][ex], in_=K.ins[src][ex]),
                     pwrites=[b_web], dma="precast")
    K.precast = precast
    if not LIMIT.get("skip_ab"):
        phase_AB(K, x, w_in, gain1, QKV, banks, bbank, ident_bf, b_ident)
    keep = {"gates": A.alloc([NT, 2], F32), "dest": A.alloc([NT, 2], I32),
            "b_gates": P.buf("gates"), "b_dest": P.buf("dest"),
            "b_H": P.buf("H"), "b_HN": P.buf("HN"), "b_slot": P.buf("slot"), "b_Y": P.buf("Y")}
    m_mixed = A.mark()
    mixedT = A.alloc([8, S], BF16)
    b_mixed = P.buf("mixedT")
    if not LIMIT.get("skip_c"):
        phase_C(K, QKV, mixedT, b_mixed, banks, bbank, ident_bf, b_ident)
    if not LIMIT.get("skip_d"):
        phase_D(K, QKV, mixedT, b_mixed, banks, bbank, ident_bf, b_ident)
    if "mixed" in K.debug:
        MIX = nc.dram_tensor("MIX", [8, 128, S], BF16, kind="ExternalOutput").ap()
        K.outs.append("MIX")
        for c in range(8):
            P.op("sp", lambda e, c=c: e.dma_start(out=MIX[c], in_=mixedT[:, c, :]), reads=[b_mixed], dma="mixdump")

    dk = "ExternalOutput" if "hdump" in K.debug else "Internal"
    H = nc.dram_tensor("H", [S, D], F32, kind=dk).ap()
    if "hdump" in K.debug:
        K.outs.append("H")
    HN = nc.dram_tensor("HN", [S + 128, D], BF16, kind="Internal").ap()
    SLOT_TOK = nc.dram_tensor("SLOT_TOK", [NSLOT + 128, 1], I32, kind=("ExternalOutput" if "hdump" in K.debug else "Internal")).ap()
    if "hdump" in K.debug:
        K.outs.append("SLOT_TOK")
    Y = nc.dram_tensor("Y", [NSLOT + 128, D], BF16, kind="Internal").ap()
    K.precast(NEXP)
    if not LIMIT.get("skip_e"):
        phase_E(K, x, mixedT, b_mixed, banks, bbank, H, HN, SLOT_TOK, keep)
    A.release(m_mixed)
    if not LIMIT.get("skip_f"):
        phase_F(K, banks, bbank, ident_bf, b_ident, HN, SLOT_TOK, Y, keep)
    phase_G(K, H, Y, y, keep)

    with nc.Block() as block:
        P.emit(block, stack)
    stack.close()
    K.stats = P.stats
    K.stats["sbuf_peak"] = A.peak
    return nc, K


def phase_AB(K, x, w_in, gain1, QKV, banks, bbank, ident_bf, b_ident):
    nc, P, A, cst = K.nc, K.P, K.A, K.cst
    m0 = A.mark()
    gain_sb = A.alloc([D], F32)
    b_gain = P.buf("gain")
    P.op("sp", lambda e: e.dma_start(out=gain_sb, in_=gain1), writes=[b_gain], dma="c_gain")
    tabs = {}
    b_tabs = {}
    for nm in ("cosA", "sinA", "cosR", "sinR")[:LIMIT.get('ntab', 4)]:
        tabs[nm] = A.alloc([S], F32)
        b_tabs[nm] = P.buf(nm)
        P.op("sp", lambda e, nm=nm: e.dma_start(out=tabs[nm], in_=cst[nm]), writes=[b_tabs[nm]], dma="c_" + nm)
    rmat = {}
    b_rmat = {}
    for nm in ("RA", "RR"):
        rmat[nm] = A.alloc([128], BF16)
        b_rmat[nm] = P.buf(nm)
        P.op("sp", lambda e, nm=nm: e.dma_start(out=rmat[nm], in_=cst[nm]), writes=[b_rmat[nm]], dma="c_" + nm)

    xnT = A.alloc([8, S], BF16)
    b_xnT = P.bufs("xnT", NG)
    xin = [A.alloc([D], F32) for _ in range(4)]
    b_xin = P.bufs("xin", 4)
    xs = [A.alloc([D], BF16) for _ in range(2)]
    b_xs = P.bufs("xs", 2)
    junk = A.alloc([D], F32)
    b_junk = P.buf("junk")
    stat = A.alloc([NT, 4], F32)
    b_stat = P.bufs("stat", NT)

    def a_part1(t):
        x4 = t % 4
        P.op("sp", lambda e, t=t, x4=x4: e.dma_start(out=xin[x4], in_=x[t * 128:(t + 1) * 128, :]),
             writes=[b_xin[x4]], dma="xin%d" % x4)
        P.op("dve", lambda e, t=t, x4=x4: e.scalar_tensor_tensor(
            out=junk, in0=xin[x4], scalar=1.0, in1=xin[x4], op0=ALU.mult, op1=ALU.mult,
            accum_out=stat[:, t, 0:1]), reads=[b_xin[x4]], writes=[b_junk, b_stat[t]])
        P.op("act", lambda e, t=t: e.activation(out=stat[:, t, 1:2], in_=stat[:, t, 0:1], func=AF.Ln,
                                                 scale=1.0 / D, bias=EPS),
             reads=[b_stat[t]], writes=[b_stat[t]])
        P.op("act", lambda e, t=t: e.activation(out=stat[:, t, 2:3], in_=stat[:, t, 1:2], func=AF.Exp, scale=-0.5),
             reads=[b_stat[t]], writes=[b_stat[t]])

    def a_part2(t):
        sl = t % 2
        x4 = t % 4
        g = t // 4
        P.op("dve", lambda e, t=t, sl=sl, x4=x4: e.scalar_tensor_tensor(
            out=xs[sl], in0=xin[x4], scalar=stat[:, t, 2:3], in1=gain_sb, op0=ALU.mult, op1=ALU.mult),
            reads=[b_xin[x4], b_stat[t], b_gain], writes=[b_xs[sl]])
        pb = banks[sl][:].bitcast(BF16).rearrange("p (a b) -> p a b", b=128)
        for dc in range(8):
            P.op("pe", lambda e, sl=sl, dc=dc, pb=pb: e.transpose(pb[:, dc, :], xs[sl][:, dc * 128:(dc + 1) * 128], ident_bf),
                 reads=[b_xs[sl], b_ident], **({"writes": [bbank[sl]]} if dc == 0 else {"pwrites": [bbank[sl]]}))
        P.op("act", lambda e, t=t, pb=pb: e.copy(out=xnT[:, :, t * 128:(t + 1) * 128], in_=pb),
             reads=[bbank[sl]], pwrites=[b_xnT[g]])

    nta = LIMIT.get('nt', NT)
    if nta:
        a_part1(0)
    for t in range(nta):
        if t + 1 < nta:
            a_part1(t + 1)
        a_part2(t)

    wch = [A.alloc([8, 128], BF16) for _ in range(3)]
    b_wch = P.bufs("wch", 3)
    stg = [A.alloc([S], BF16) for _ in range(2)]
    b_stg = P.bufs("stg", 2)
    qsb = [A.alloc([512], BF16) for _ in range(2)]
    b_qsb = P.bufs("qsb", 2)
    tmpa = [A.alloc([512], F32) for _ in range(2)]
    b_tmpa = P.bufs("tmpa", 2)
    tmpb = [A.alloc([512], F32) for _ in range(2)]
    b_tmpb = P.bufs("tmpb", 2)
    w_v = w_in.rearrange("(dc p) c -> p dc c", p=128)
    work = []
    for cc in range(LIMIT.get('ncc', 28)):
        for g in range(NG):
            work.append((cc, g))
    pending = []

    def stageA(itn, cc, g):
        ws = cc % 3
        seg = cc // 4
        if g == 0:
            P.op("pool", lambda e, cc=cc, ws=ws: e.dma_start(out=wch[ws], in_=w_v[:, :, cc * 128:(cc + 1) * 128]),
                 writes=[b_wch[ws]], dma="wch%d" % ws)
        rot = seg in (0, 1, 3, 4)
        scale = 0.125 if seg in (0, 4) else 1.0
        ss = cc % 2
        mb = 2 + (itn % 3)
        q2 = itn % 2
        gs = slice(g * 512, (g + 1) * 512)
        for dc in range(8):
            P.op("pe", lambda e, mb=mb, ws=ws, dc=dc, gs=gs: e.matmul(
                banks[mb][:], lhsT=wch[ws][:, dc, :], rhs=xnT[:, dc, gs], start=(dc == 0), stop=(dc == 7)),
                reads=[b_wch[ws], b_xnT[g]], **({"writes": [bbank[mb]]} if dc == 0 else {"pwrites": [bbank[mb]]}))
        if not rot:
            P.op("act", lambda e, mb=mb, ss=ss, gs=gs: e.copy(out=stg[ss][:, gs], in_=banks[mb][:]),
                 reads=[bbank[mb]], pwrites=[b_stg[ss]])
        else:
            P.op("act", lambda e, mb=mb, q2=q2, scale=scale: e.activation(out=qsb[q2], in_=banks[mb][:], func=AF.Copy, scale=scale),
                 reads=[bbank[mb]], writes=[b_qsb[q2]])

    def stageB(itn, cc, g):
        seg = cc // 4
        rot = seg in (0, 1, 3, 4)
        ss = cc % 2
        gs = slice(g * 512, (g + 1) * 512)
        if rot:
            tabc, tabs_ = ("cosA", "sinA") if seg in (0, 1) else ("cosR", "sinR")
            rm = "RA" if seg in (0, 1) else "RR"
            rb = 5 + (itn % 2)
            q2 = itn % 2
            P.op("pe", lambda e, rb=rb, q2=q2, rm=rm: e.matmul(banks[rb][:], lhsT=rmat[rm], rhs=qsb[q2], start=True, stop=True),
                 reads=[b_qsb[q2], b_rmat[rm]], writes=[bbank[rb]])
            P.op("dve", lambda e, q2=q2, tabc=tabc, gs=gs: e.tensor_tensor(
                out=tmpa[q2], in0=qsb[q2], in1=tabs[tabc][:, gs], op=ALU.mult),
                reads=[b_qsb[q2], b_tabs[tabc]], writes=[b_tmpa[q2]])
            P.op("dve", lambda e, rb=rb, q2=q2, tabs_=tabs_, gs=gs: e.tensor_tensor(out=tmpb[q2], in0=banks[rb][:], in1=tabs[tabs_][:, gs], op=ALU.mult),
                 reads=[bbank[rb], b_tabs[tabs_]], writes=[b_tmpb[q2]])
            P.op("pool", lambda e, q2=q2, ss=ss, gs=gs: e.tensor_tensor(out=stg[ss][:, gs], in0=tmpa[q2], in1=tmpb[q2], op=ALU.add),
                 reads=[b_tmpa[q2], b_tmpb[q2]], pwrites=[b_stg[ss]])
        if g == NG - 1 and not LIMIT.get('skip_store'):
            P.op("sp", lambda e, cc=cc, ss=ss: e.dma_start(out=QKV[cc], in_=stg[ss]), reads=[b_stg[ss]], dma="stg%d" % ss)

    for itn, (cc, g) in enumerate(work):
        stageA(itn, cc, g)
        if itn >= 1:
            stageB(itn - 1, *work[itn - 1])
    if work:
        stageB(len(work) - 1, *work[-1])
    P.barrier()
    A.release(m0)


def phase_C(K, QKV, mixedT, b_mixed, banks, bbank, ident_bf, b_ident):
    nc, P, A, cst = K.nc, K.P, K.A, K.cst
    m0 = A.mark()
    LN2 = 0.6931471805599453
    dec = A.alloc([24], F32)
    b_dec = P.buf("dec")
    P.op("sp", lambda e: e.dma_start(out=dec, in_=K.ins["dec_bc"]), writes=[b_dec], dma="c_dec")
    cn = {}
    b_cn = {}
    for nm, w in (("Mf", 128), ("Mb", 128), ("iq", 128), ("iqb", 128), ("Bavg", 128), ("pidx", 2)):
        cn[nm] = A.alloc([w], F32)
        b_cn[nm] = P.buf(nm)
        P.op("sp", lambda e, nm=nm: e.dma_start(out=cn[nm], in_=cst[nm]), writes=[b_cn[nm]], dma="c_" + nm)
    x2 = A.alloc([24], F32)
    tt = A.alloc([24], F32)
    lg = A.alloc([24], F32)
    b_lg = P.buf("lg")
    P.op("act", lambda e: e.activation(out=x2, in_=dec, func=AF.Exp, scale=LN2), reads=[b_dec], writes=[b_lg])
    P.op("dve", lambda e: e.tensor_scalar(out=tt, in0=x2, scalar1=0.2, scalar2=None, op0=ALU.mult), reads=[b_lg], writes=[b_lg])
    for c in (0.25, 1.0 / 3.0, 0.5, 1.0):
        P.op("dve", lambda e, c=c: e.scalar_tensor_tensor(out=tt, in0=tt, scalar=c, in1=x2, op0=ALU.add, op1=ALU.mult),
             reads=[b_lg], writes=[b_lg])
    P.op("dve", lambda e: e.tensor_scalar(out=lg, in0=tt, scalar1=-1.0, scalar2=None, op0=ALU.mult), reads=[b_lg], writes=[b_lg])
    DT4 = A.alloc([8, 4, 128], F32)
    b_DT = P.buf("DT")
    e1 = A.alloc([128], F32)
    e2 = A.alloc([128], F32)
    b_e = P.buf("e12")
    for h in range(8):
        P.op("act", lambda e, h=h: e.activation(out=e1, in_=cn["Mf"], func=AF.Exp, scale=lg[:, h:h + 1]),
             reads=[b_lg, b_cn["Mf"]], writes=[b_e])
        P.op("act", lambda e, h=h: e.activation(out=e2, in_=cn["Mb"], func=AF.Exp, scale=lg[:, 8 + h:9 + h]),
             reads=[b_lg, b_cn["Mb"]], pwrites=[b_e])
        P.op("dve", lambda e, h=h: e.tensor_tensor(out=DT4[:, h], in0=e1.unsqueeze(1).to_broadcast([128, 4, 128]),
                                                    in1=e2.unsqueeze(1).to_broadcast([128, 4, 128]), op=ALU.add),
             reads=[b_e], pwrites=[b_DT])
    kd = A.alloc([16], F32)
    b_kd = P.buf("kd")
    P.op("act", lambda e: e.activation(out=kd[:, 0:8], in_=lg[:, 0:8], func=AF.Exp, scale=cn["pidx"][:, 0:1]),
         reads=[b_lg, b_cn["pidx"]], writes=[b_kd])
    P.op("act", lambda e: e.activation(out=kd[:, 8:16], in_=lg[:, 8:16], func=AF.Exp, scale=cn["pidx"][:, 1:2]),
         reads=[b_lg, b_cn["pidx"]], pwrites=[b_kd])
    qd = A.alloc([8, 128], F32)
    b_qd = P.buf("qd")
    gc = A.alloc([8], F32)
    b_gc = P.buf("gc")
    for hp in range(4):
        P.op("act", lambda e, hp=hp: e.activation(out=qd[:, hp], in_=cn["iq"], func=AF.Exp, scale=lg[:, 16 + hp:17 + hp]),
             reads=[b_lg, b_cn["iq"]], pwrites=[b_qd])
        P.op("act", lambda e, hp=hp: e.activation(out=qd[:, 4 + hp], in_=cn["iqb"], func=AF.Exp, scale=lg[:, 20 + hp:21 + hp]),
             reads=[b_lg, b_cn["iqb"]], pwrites=[b_qd])
    P.op("act", lambda e: e.activation(out=gc, in_=lg[:, 16:24], func=AF.Exp, scale=128.0), reads=[b_lg], writes=[b_gc])

    qT, kT, vT, gT = [A.alloc([S], BF16) for _ in range(4)]
    b_q, b_k, b_v, b_g = P.buf("qT"), P.buf("kT"), P.buf("vT"), P.buf("gT")
    kf = A.alloc([32, 128], BF16)
    kb = A.alloc([32, 128], BF16)
    vt = A.alloc([32, 128], BF16)
    b_kf, b_kb, b_vt = P.buf("kf"), P.buf("kb"), P.buf("vt")
    SBf = A.alloc([32, 128], BF16)
    SBb = A.alloc([32, 128], BF16)
    b_SBf, b_SBb = P.buf("SBf"), P.buf("SBb")
    stf = A.alloc([2, 128], F32)
    stb = A.alloc([2, 128], F32)
    b_stf, b_stb = P.bufs("stf", 2), P.bufs("stb", 2)
    SD = [A.alloc([4, 128], BF16) for _ in range(2)]
    b_SD = P.bufs("SD", 2)
    qdf_t = A.alloc([4, 128], BF16)
    qdb_t = A.alloc([4, 128], BF16)
    b_qdf, b_qdb = P.buf("qdf"), P.buf("qdb")
    o_sb, cen, sq, sd, rs_, sg, y1 = [A.alloc([512], F32) for _ in range(7)]
    b_o, b_cen, b_sq, b_sd, b_rs, b_sg, b_y1 = [P.buf(n) for n in "o cen sq sd rs sg y1".split()]
    o_sb2 = [o_sb, A.alloc([512], F32)]
    b_o2 = [b_o, P.buf("o2")]

    def bfv(i):
        return banks[i][:].bitcast(BF16)[:, 0:512].rearrange("p (a b) -> p a b", b=128)

    def f4(i):
        return banks[i][:].rearrange("p (a b) -> p a b", b=128)

    for hp in range(4):
        K.precast(4)
        for ap_, bb, ci, nm in ((qT, b_q, 12, "q"), (kT, b_k, 16, "k"), (vT, b_v, 20, "v"), (gT, b_g, 24, "g")):
            P.op("sp", lambda e, ap_=ap_, ci=ci, hp=hp: e.dma_start(out=ap_, in_=QKV[ci + hp]), writes=[bb], dma="ld_" + nm)
        for bt in range(8):
            bk = 6 + (bt % 2)
            for j in range(4):
                n = 4 * bt + j
                P.op("pe", lambda e, bk=bk, j=j, n=n: e.transpose(bfv(bk)[:, j, :], kT[:, n * 128:(n + 1) * 128], ident_bf),
                     reads=[b_k, b_ident], **({"writes": [bbank[bk]]} if j == 0 else {"pwrites": [bbank[bk]]}))
            for h in range(2):
                hs = slice(h * 64, (h + 1) * 64)
                P.op("dve", lambda e, bk=bk, bt=bt, hs=hs, h=h, hp=hp: e.tensor_scalar(
                    out=kf[:, 4 * bt:4 * bt + 4, hs], in0=bfv(bk)[:, :, hs], scalar1=kd[:, 2 * hp + h:2 * hp + h + 1],
                    scalar2=None, op0=ALU.mult), reads=[bbank[bk], b_kd], pwrites=[b_kf])
                P.op("dve", lambda e, bk=bk, bt=bt, hs=hs, h=h, hp=hp: e.tensor_scalar(
                    out=kb[:, 4 * bt:4 * bt + 4, hs], in0=bfv(bk)[:, :, hs], scalar1=kd[:, 8 + 2 * hp + h:8 + 2 * hp + h + 1],
                    scalar2=None, op0=ALU.mult), reads=[bbank[bk], b_kd], pwrites=[b_kb])
        for bt in range(8):
            bk = 6 + (bt % 2)
            for j in range(4):
                n = 4 * bt + j
                P.op("pe", lambda e, bk=bk, j=j, n=n: e.transpose(bfv(bk)[:, j, :], vT[:, n * 128:(n + 1) * 128], ident_bf),
                     reads=[b_v, b_ident], **({"writes": [bbank[bk]]} if j == 0 else {"pwrites": [bbank[bk]]}))
            P.op("act", lambda e, bk=bk, bt=bt: e.copy(out=vt[:, 4 * bt:4 * bt + 4, :], in_=bfv(bk)),
                 reads=[bbank[bk]], pwrites=[b_vt])
        P.op("pool", lambda e: e.memset(stf[:, 0, :], 0.0), writes=[b_stf[0]])
        P.op("pool", lambda e: e.memset(SBf[:, 0, :], 0.0), writes=[b_SBf])
        P.op("pool", lambda e: e.memset(stb[:, 1, :], 0.0), writes=[b_stb[1]])
        P.op("pool", lambda e: e.memset(SBb[:, 31, :], 0.0), writes=[b_SBb])
        for bt in range(8):
            bk = 6 + (bt % 2)
            for j in range(4):
                n = 4 * bt + j
                P.op("pe", lambda e, bk=bk, j=j, n=n: e.matmul(f4(bk)[:, j, :], lhsT=kf[:, n, :], rhs=vt[:, n, :], start=True, stop=True),
                     reads=[b_kf, b_vt], **({"writes": [bbank[bk]]} if j == 0 else {"pwrites": [bbank[bk]]}))
            for j in range(4):
                n = 4 * bt + j
                if n == 31:
                    continue
                a, b2 = n % 2, (n + 1) % 2
                P.op("dve", lambda e, bk=bk, j=j, a=a, b2=b2, hp=hp: e.scalar_tensor_tensor(
                    out=stf[:, b2, :], in0=stf[:, a, :], scalar=gc[:, hp:hp + 1], in1=f4(bk)[:, j, :], op0=ALU.mult, op1=ALU.add),
                    reads=[b_stf[a], b_gc, bbank[bk]], writes=[b_stf[b2]])
                P.op("act", lambda e, b2=b2, n=n: e.copy(out=SBf[:, n + 1, :], in_=stf[:, b2, :]), reads=[b_stf[b2]], pwrites=[b_SBf])
        for bt in range(7, -1, -1):
            bk = 6 + (bt % 2)
            for j in range(4):
                n = 4 * bt + j
                P.op("pe", lambda e, bk=bk, j=j, n=n: e.matmul(f4(bk)[:, j, :], lhsT=kb[:, n, :], rhs=vt[:, n, :], start=True, stop=True),
                     reads=[b_kb, b_vt], **({"writes": [bbank[bk]]} if j == 0 else {"pwrites": [bbank[bk]]}))
            for j in range(3, -1, -1):
                n = 4 * bt + j
                if n == 0:
                    continue
                a, b2 = n % 2, (n + 1) % 2
                P.op("dve", lambda e, bk=bk, j=j, n=n, hp=hp: e.scalar_tensor_tensor(
                    out=stb[:, (n - 1) % 2, :], in0=stb[:, n % 2, :], scalar=gc[:, 4 + hp:5 + hp], in1=f4(bk)[:, j, :], op0=ALU.mult, op1=ALU.add),
                    reads=[b_stb[n % 2], b_gc, bbank[bk]], writes=[b_stb[(n - 1) % 2]])
                P.op("act", lambda e, n=n: e.copy(out=SBb[:, n - 1, :], in_=stb[:, (n - 1) % 2, :]), reads=[b_stb[(n - 1) % 2]], pwrites=[b_SBb])
        def A1(g):
            gs = slice(g * 512, (g + 1) * 512)
            q3 = qT[:, gs].rearrange("p (a b) -> p a b", b=128)
            P.op("pool", lambda e, q3=q3, hp=hp: e.tensor_tensor(out=qdf_t, in0=q3, in1=qd[:, hp].unsqueeze(1).to_broadcast([128, 4, 128]), op=ALU.mult),
                 reads=[b_q, b_qd], writes=[b_qdf])
            P.op("pool", lambda e, q3=q3, hp=hp: e.tensor_tensor(out=qdb_t, in0=q3, in1=qd[:, 4 + hp].unsqueeze(1).to_broadcast([128, 4, 128]), op=ALU.mult),
                 reads=[b_q, b_qd], writes=[b_qdb])
            for h in range(2):
                hs = slice(h * 64, (h + 1) * 64)
                for j in range(4):
                    n = 4 * g + j
                    cs = slice(n * 128, (n + 1) * 128)
                    P.op("pe", lambda e, h=h, hs=hs, j=j, cs=cs: e.matmul(f4(h)[:, j, :], lhsT=kT[hs, cs], rhs=qT[hs, cs], start=True, stop=True),
                         reads=[b_k, b_q], **({"writes": [bbank[h]]} if j == 0 else {"pwrites": [bbank[h]]}))
                P.op("dve", lambda e, h=h, hp=hp: e.tensor_tensor(out=SD[h], in0=f4(h), in1=DT4[:, 2 * hp + h], op=ALU.mult),
                     reads=[bbank[h], b_DT], writes=[b_SD[h]])

        def A2(g):
            bo = 2 + (g % 2)
            first = True
            for j in range(4):
                n = 4 * g + j
                for h in range(2):
                    hs = slice(h * 64, (h + 1) * 64)
                    P.op("pe", lambda e, bo=bo, j=j, n=n, h=h, hs=hs: e.matmul(f4(bo)[hs, j, :], lhsT=vt[:, n, hs], rhs=SD[h][:, j, :],
                                                                                 start=True, stop=False, skip_group_check=True),
                         reads=[b_vt, b_SD[h]], **({"writes": [bbank[bo]]} if first else {"pwrites": [bbank[bo]]}))
                    first = False
                for h in range(2):
                    hs = slice(h * 64, (h + 1) * 64)
                    P.op("pe", lambda e, bo=bo, j=j, n=n, hs=hs: e.matmul(f4(bo)[hs, j, :], lhsT=SBf[hs, n, hs], rhs=qdf_t[hs, j, :],
                                                                          start=False, stop=False, skip_group_check=True),
                         reads=[b_SBf, b_qdf], pwrites=[bbank[bo]])
                    P.op("pe", lambda e, bo=bo, j=j, n=n, hs=hs: e.matmul(f4(bo)[hs, j, :], lhsT=SBb[hs, n, hs], rhs=qdb_t[hs, j, :],
                                                                          start=False, stop=True, skip_group_check=True),
                         reads=[b_SBb, b_qdb], pwrites=[bbank[bo]])
            o2 = g % 2
            P.op("act", lambda e, bo=bo, o2=o2: e.copy(out=o_sb2[o2], in_=banks[bo][:]), reads=[bbank[bo]], writes=[b_o2[o2]])

        def B1(g):
            o2 = g % 2
            P.op("pe", lambda e, o2=o2: e.matmul(banks[4][:], lhsT=cn["Bavg"], rhs=o_sb2[o2], start=True, stop=True), reads=[b_cn["Bavg"], b_o2[o2]], writes=[bbank[4]])
            P.op("dve", lambda e, o2=o2: e.tensor_tensor(out=cen, in0=o_sb2[o2], in1=banks[4][:], op=ALU.subtract), reads=[b_o2[o2], bbank[4]], writes=[b_cen])
            P.op("pool", lambda e: e.tensor_tensor(out=sq, in0=cen, in1=cen, op=ALU.mult), reads=[b_cen], writes=[b_sq])

        def B2(g):
            gs = slice(g * 512, (g + 1) * 512)
            P.op("pe", lambda e: e.matmul(banks[5][:], lhsT=cn["Bavg"], rhs=sq, start=True, stop=True), reads=[b_cn["Bavg"], b_sq], writes=[bbank[5]])
            P.op("act", lambda e: e.activation(out=sd, in_=banks[5][:], func=AF.Sqrt, bias=EPS, scale=1.0), reads=[bbank[5]], writes=[b_sd])
            P.op("dve", lambda e: e.reciprocal(out=rs_, in_=sd), reads=[b_sd], writes=[b_rs])
            P.op("act", lambda e, gs=gs: e.activation(out=sg, in_=gT[:, gs], func=AF.Silu), reads=[b_g], writes=[b_sg])
            P.op("dve", lambda e: e.tensor_tensor(out=y1, in0=cen, in1=rs_, op=ALU.mult), reads=[b_cen, b_rs], writes=[b_y1])
            P.op("pool", lambda e, hp=hp, gs=gs: e.tensor_tensor(out=mixedT[:, 4 + hp, gs], in0=y1, in1=sg, op=ALU.mult),
                 reads=[b_y1, b_sg], pwrites=[b_mixed])

        A1(0)
        A2(0)
        for g in range(NG):
            if g + 1 < NG:
                A1(g + 1)
            B1(g)
            if g + 1 < NG:
                A2(g + 1)
            B2(g)
    P.barrier()
    A.release(m0)


def _sl(start, n, step):
    return slice(start, start + (n - 1) * step + 1, step)


def phase_D(K, QKV, mixedT, b_mixed, banks, bbank, ident_bf, b_ident):
    nc, P, A, cst = K.nc, K.P, K.A, K.cst
    m0 = A.mark()
    mask = A.alloc([256], BF16)
    b_mask = P.buf("mask")
    P.op("sp", lambda e: e.dma_start(out=mask, in_=cst["amask"]), writes=[b_mask], dma="c_amask")
    qT, kT, vT = [A.alloc([S], BF16) for _ in range(3)]
    b_q, b_k, b_v = P.buf("aq"), P.buf("ak"), P.buf("av")
    DIL = (1, 4, 16)
    Vp = [[A.alloc([32, 128], BF16) for _ in DIL] for _ in range(2)]
    b_Vp = [[P.buf("Vp%d_%d" % (h, d)) for d in DIL] for h in range(2)]
    for h in range(2):
        for di in range(3):
            P.op("pool", lambda e, h=h, di=di: e.memset(Vp[h][di], 1.0), writes=[b_Vp[h][di]])
    acc = [A.alloc([S], F32) for _ in range(2)]
    b_acc = P.bufs("acc", 2)
    tmp = A.alloc([S], F32)
    b_tmp = P.buf("atmp")
    NSL = 5
    SBANK = (5, 6, 7, 0, 1)
    pt = [A.alloc([256], BF16) for _ in range(NSL)]
    pm = [A.alloc([256], BF16) for _ in range(NSL)]
    b_pt, b_pm = P.bufs("pt", NSL), P.bufs("pm", NSL)

    def bfv(i):
        return banks[i][:].bitcast(BF16)[:, 0:512].rearrange("p (a b) -> p a b", b=128)

    it = 0
    ob = 0
    for hp in range(4):
        for ap_, bb, ci, nm in ((qT, b_q, 0, "aq"), (kT, b_k, 4, "ak"), (vT, b_v, 8, "av")):
            P.op("sp", lambda e, ap_=ap_, ci=ci, hp=hp: e.dma_start(out=ap_, in_=QKV[ci + hp]), writes=[bb], dma="ld_" + nm)
        tb = 0
        for di, d in enumerate(DIL):
            L = S // d
            for bt in range(8):
                bk = tb % 2
                tb += 1
                for j in range(4):
                    i = 4 * bt + j
                    r, s0 = (128 * i) // L, (128 * i) % L
                    st0 = r + d * s0
                    P.op("pe", lambda e, bk=bk, j=j, st0=st0, d=d: e.transpose(bfv(bk)[:, j, :], vT[:, _sl(st0, 128, d)], ident_bf),
                         reads=[b_v, b_ident], **({"writes": [bbank[bk]]} if j == 0 else {"pwrites": [bbank[bk]]}))
                for h in range(2):
                    hs = slice(h * 64, (h + 1) * 64)
                    eng = "act" if h == 0 else "dve"
                    fn = (lambda e, bk=bk, bt=bt, hs=hs, h=h, di=di: e.copy(out=Vp[h][di][:, 4 * bt:4 * bt + 4, hs], in_=bfv(bk)[:, :, hs])) if h == 0 else \
                         (lambda e, bk=bk, bt=bt, hs=hs, h=h, di=di: e.tensor_copy(out=Vp[h][di][:, 4 * bt:4 * bt + 4, hs], in_=bfv(bk)[:, :, hs]))
                    P.op(eng, fn, reads=[bbank[bk]], pwrites=[b_Vp[h][di]])
        for h in range(2):
            K.precast(2)
            hs = slice(h * 64, (h + 1) * 64)
            items = []
            for di, d in enumerate(DIL):
                L = S // d
                for i in range(32):
                    items.append((di, d, L, i))
            started = {}
            LA = NSL - 1

            def stage1(di, d, L, i, slot):
                r, s0 = (128 * i) // L, (128 * i) % L
                qlo, qhi = max(0, s0 - 64), min(L, s0 + 192)
                N = qhi - qlo
                mo = qlo - (s0 - 64)
                sb = SBANK[slot]
                p3 = slot
                k0 = r + d * s0
                q0 = r + d * qlo
                P.op("pe", lambda e, sb=sb, hs=hs, k0=k0, q0=q0, N=N, d=d: e.matmul(
                    banks[sb][:, 0:N], lhsT=kT[hs, _sl(k0, 128, d)], rhs=qT[hs, _sl(q0, N, d)], start=True, stop=True),
                    reads=[b_k, b_q], writes=[bbank[sb]])
                P.op("act", lambda e, sb=sb, p3=p3, N=N: e.activation(out=pt[p3][:, 0:N], in_=banks[sb][:, 0:N], func=AF.Exp),
                     reads=[bbank[sb]], writes=[b_pt[p3]])
                P.op("pool" if (i % 2 == 0) else "dve", lambda e, p3=p3, N=N, mo=mo: e.tensor_tensor(out=pm[p3][:, 0:N], in0=pt[p3][:, 0:N], in1=mask[:, mo:mo + N], op=ALU.mult),
                     reads=[b_pt[p3], b_mask], writes=[b_pm[p3]])

            def stage2(di, d, L, i, slot):
                nonlocal ob
                r, s0 = (128 * i) // L, (128 * i) % L
                qlo, qhi = max(0, s0 - 64), min(L, s0 + 192)
                p3 = slot
                ulo, uhi = r * L + qlo, r * L + qhi
                u = ulo
                while u < uhi:
                    lb = (di, u // 512)
                    ue = min(uhi, (lb[1] + 1) * 512)
                    if lb not in started:
                        started[lb] = 2 + (ob % 3)
                        ob += 1
                        first = True
                    else:
                        first = False
                    pb = started[lb]
                    c0, c1 = u - lb[1] * 512, ue - lb[1] * 512
                    m0_, m1_ = u - ulo, ue - ulo
                    P.op("pe", lambda e, pb=pb, c0=c0, c1=c1, m0_=m0_, m1_=m1_, h=h, di=di, i=i, p3=p3, first=first: e.matmul(
                        banks[pb][:, c0:c1], lhsT=Vp[h][di][:, i, :], rhs=pm[p3][:, m0_:m1_], start=first, stop=False, skip_group_check=True),
                        reads=[b_Vp[h][di], b_pm[p3]], **({"writes": [bbank[pb]]} if first else {"pwrites": [bbank[pb]]}))
                    u = ue
                for lbk in list(started.keys()):
                    if lbk[0] != di:
                        continue
                    lb = lbk[1]
                    rr_, ss_ = (512 * lb) // L, (512 * lb) % L
                    if L >= 512:
                        last_s0 = min(L - 128, ss_ + 512)
                        last_i = (rr_ * L + last_s0) // 128
                    else:
                        last_i = ((rr_ + 1) * L + L - 128) // 128
                    if i == last_i:
                        pb = started.pop(lbk)
                        if d == 1:
                            P.op("act", lambda e, pb=pb, lb=lb, h=h: e.copy(out=acc[h][:, lb * 512:(lb + 1) * 512], in_=banks[pb][:]),
                                 reads=[bbank[pb]], pwrites=[b_acc[h]])
                        elif d == 4:
                            t0 = rr_ + 4 * ss_
                            P.op("dve", lambda e, pb=pb, t0=t0, h=h: e.tensor_tensor(
                                out=acc[h][:, _sl(t0, 512, 4)], in0=acc[h][:, _sl(t0, 512, 4)], in1=banks[pb][:], op=ALU.add),
                                reads=[bbank[pb], b_acc[h]], pwrites=[b_acc[h]])
                        else:
                            av = acc[h].rearrange("p (s r) -> p r s", r=16)[:, rr_:rr_ + 2, :]
                            P.op("dve", lambda e, pb=pb, av=av, h=h: e.tensor_tensor(
                                out=av, in0=av, in1=banks[pb][:].rearrange("p (a b) -> p a b", b=256), op=ALU.add),
                                reads=[bbank[pb], b_acc[h]], pwrites=[b_acc[h]])

            n_it = len(items)
            for idx in range(n_it + LA):
                if idx < n_it:
                    stage1(*items[idx], idx % NSL)
                if idx - LA >= 0:
                    stage2(*items[idx - LA], (idx - LA) % NSL)
            assert not started, started
            os_ = slice((1 - h) * 64, (2 - h) * 64)
            P.op("dve", lambda e, h=h, hs=hs, os_=os_: e.reciprocal(out=tmp[hs, :], in_=acc[h][os_, :]), reads=[b_acc[h]], writes=[b_tmp])
            P.op("pool", lambda e, h=h, hs=hs, hp=hp: e.tensor_tensor(out=mixedT[hs, hp, :], in0=acc[h][hs, :], in1=tmp[hs, :], op=ALU.mult),
                 reads=[b_acc[h], b_tmp], pwrites=[b_mixed])
    P.barrier()
    A.release(m0)


NSLOT = NEXP * CAP
TRASH = NSLOT


def phase_E(K, x, mixedT, b_mixed, banks, bbank, H, HN, SLOT_TOK, keep):
    nc, P, A, cst, ins = K.nc, K.P, K.A, K.cst, K.ins
    m0 = A.mark()
    ones_bf = A.alloc([128], BF16)
    b_ones = P.buf("ones")
    P.op("pool", lambda e: e.memset(ones_bf, 1.0), writes=[b_ones])
    cn = {}
    b_cn = {}
    for nm, w, dt in (("ident_f", 128, F32), ("Ltri", 128, F32), ("ones_f", 128, F32), ("eidx", 32, F32), ("tokid", 32, I32)):
        cn[nm] = A.alloc([w], dt)
        b_cn[nm] = P.buf(nm)
        P.op("sp", lambda e, nm=nm: e.dma_start(out=cn[nm], in_=cst[nm]), writes=[b_cn[nm]], dma="c_" + nm)
    gmix = A.alloc([8], F32)
    gain2 = A.alloc([D], F32)
    br = A.alloc([36], F32)
    wr = A.alloc([8, 36], F32)
    b_gmix, b_gain2, b_br, b_wr = P.buf("gmix"), P.buf("gain2"), P.buf("br"), P.buf("wr")
    P.op("sp", lambda e: e.dma_start(out=gmix, in_=ins["gmix"]), writes=[b_gmix], dma="c_gmix")
    P.op("sp", lambda e: e.dma_start(out=gain2, in_=ins["gain2_bc"]), writes=[b_gain2], dma="c_gain2")
    P.op("sp", lambda e: e.dma_start(out=br, in_=ins["br_bc"]), writes=[b_br], dma="c_br")
    P.op("sp", lambda e: e.dma_start(out=wr, in_=ins["wr"].rearrange("(dc p) c -> p dc c", p=128)), writes=[b_wr], dma="c_wr")
    Wout = A.alloc([8, D], BF16)
    b_Wout = P.buf("Wout")
    m1 = A.mark()
    wtmp = A.alloc([4, D], F32)
    b_wtmp = P.buf("wtmp")
    wo_v = ins["w_out"].rearrange("(c p) d -> p c d", p=128)
    for half in range(2):
        P.op("sp", lambda e, half=half: e.dma_start(out=wtmp, in_=wo_v[:, 4 * half:4 * half + 4, :]), writes=[b_wtmp], dma="c_wout")
        for c in range(4):
            cc = 4 * half + c
            P.op("dve", lambda e, c=c, cc=cc: e.tensor_scalar(out=Wout[:, cc, :], in0=wtmp[:, c, :], scalar1=gmix[:, cc:cc + 1], scalar2=None, op0=ALU.mult),
                 reads=[b_wtmp, b_gmix], pwrites=[b_Wout])
    zb = A.alloc([D], BF16)
    b_zb = P.buf("zb")
    P.op("pool", lambda e: e.memset(zb, 0.0), writes=[b_zb])
    b_HN, b_H, b_slot = keep["b_HN"], keep["b_H"], keep["b_slot"]
    P.op("sp", lambda e: e.dma_start(out=HN[S:S + 128, :], in_=zb), reads=[b_zb], pwrites=[b_HN], dma="hnz")
    nsl = (NSLOT + 128) // 128
    sinit = A.alloc([nsl], I32)
    b_sinit = P.buf("sinit")
    P.op("sp", lambda e: e.dma_start(out=sinit, in_=cst["slotinit"]), writes=[b_sinit], dma="c_sinit")
    P.op("sp", lambda e: e.dma_start(out=SLOT_TOK.rearrange("(p a) o -> p (a o)", p=128), in_=sinit), reads=[b_sinit], writes=[b_slot], dma="slot_init")

    sqb = A.alloc([4, 512], BF16)
    b_sqb = P.buf("sqb")
    lnv = A.alloc([512], F32)
    rbc = A.alloc([512], F32)
    b_lnv, b_rbc = P.buf("lnv"), P.buf("rbc")
    for g in range(NG):
        gs = slice(g * 512, (g + 1) * 512)
        P.op("pool", lambda e, gs=gs: e.tensor_tensor(out=sqb, in0=mixedT[:, 0:4, gs], in1=mixedT[:, 0:4, gs], op=ALU.mult),
             reads=[b_mixed], writes=[b_sqb])
        for c in range(4):
            P.op("pe", lambda e, c=c: e.matmul(banks[0][:], lhsT=ones_bf, rhs=sqb[:, c, :], start=(c == 0), stop=(c == 3)),
                 reads=[b_ones, b_sqb], **({"writes": [bbank[0]]} if c == 0 else {"pwrites": [bbank[0]]}))
        P.op("act", lambda e: e.activation(out=lnv, in_=banks[0][:], func=AF.Ln, scale=1.0 / 512, bias=EPS), reads=[bbank[0]], writes=[b_lnv])
        P.op("act", lambda e: e.activation(out=rbc, in_=lnv, func=AF.Exp, scale=-0.5), reads=[b_lnv], writes=[b_rbc])
        P.op("dve", lambda e, gs=gs: e.tensor_tensor(out=mixedT[:, 0:4, gs], in0=mixedT[:, 0:4, gs],
                                                      in1=rbc.unsqueeze(1).to_broadcast([128, 4, 512]), op=ALU.mult),
             reads=[b_rbc, b_mixed], pwrites=[b_mixed])
    P.barrier()
    A.release(m1)
    xin = [A.alloc([D], F32) for _ in range(2)]
    b_xin = P.bufs("exin", 2)
    hsb = [A.alloc([D], F32) for _ in range(2)]
    b_hsb = P.bufs("hsb", 2)
    junk = A.alloc([D], F32)
    b_junk = P.buf("ejunk")
    hn32_ = [A.alloc([D], F32) for _ in range(2)]
    b_hn32_ = P.bufs("hn32", 2)
    hnb = [A.alloc([D], BF16) for _ in range(2)]
    b_hnb = P.bufs("hnb", 2)
    hnT_ = [A.alloc([8, 128], F32) for _ in range(2)]
    b_hnT_ = P.bufs("hnT", 2)
    st = A.alloc([NT, 4], F32)
    b_st = P.bufs("est", NT)
    TB = 8
    LG = A.alloc([TB, 36], F32)
    gmax, gsum, ggate, m1_, m2_, dd, ee, den, p1, p2 = [A.alloc([TB], F32) for _ in range(10)]
    goh, gsh = A.alloc([TB, 4], F32), A.alloc([TB, 4], F32)
    sel, oh1, sel2, oh2 = [A.alloc([TB, 8], F32) for _ in range(4)]
    T32, E1, E2, Osum, base = [A.alloc([TB, 32], F32) for _ in range(5)]
    rk, eid, vv, dst = [A.alloc([TB, 2], F32) for _ in range(4)]
    Ocum = A.alloc([32], F32)
    b_Ocum = P.buf("Ocum")
    P.op("pool", lambda e: e.memset(Ocum, 0.0), writes=[b_Ocum])
    gates, dest = keep["gates"], keep["dest"]
    b_gates, b_dest = keep["b_gates"], keep["b_dest"]
    for t in range(NT):
        sl = t % 2
        hn32, b_hn32, hnT, b_hnT = hn32_[sl], b_hn32_[sl], hnT_[sl], b_hnT_[sl]
        ts_ = slice(t * 128, (t + 1) * 128)
        P.op("sp", lambda e, sl=sl, ts_=ts_: e.dma_start(out=xin[sl], in_=x[ts_, :]), writes=[b_xin[sl]], dma="exin%d" % sl)
        for half in range(2):
            bk = 1 + half
            for c in range(8):
                P.op("pe", lambda e, bk=bk, c=c, ts_=ts_, half=half: e.matmul(
                    banks[bk][:], lhsT=mixedT[:, c, ts_], rhs=Wout[:, c, half * 512:(half + 1) * 512], start=(c == 0), stop=(c == 7)),
                    reads=[b_mixed, b_Wout], **({"writes": [bbank[bk]]} if c == 0 else {"pwrites": [bbank[bk]]}))
            P.op("dve", lambda e, bk=bk, sl=sl, half=half: e.tensor_tensor(
                out=hsb[sl][:, half * 512:(half + 1) * 512], in0=xin[sl][:, half * 512:(half + 1) * 512], in1=banks[bk][:], op=ALU.add),
                reads=[bbank[bk], b_xin[sl]], **({"writes": [b_hsb[sl]]} if half == 0 else {"pwrites": [b_hsb[sl]]}))
        P.op("sp", lambda e, sl=sl, ts_=ts_: e.dma_start(out=H[ts_, :], in_=hsb[sl]), reads=[b_hsb[sl]], pwrites=[b_H], dma="hst%d" % sl)
        P.op("dve", lambda e, sl=sl, t=t: e.scalar_tensor_tensor(out=junk, in0=hsb[sl], scalar=1.0, in1=hsb[sl], op0=ALU.mult, op1=ALU.mult,
                                                             accum_out=st[:, t, 0:1]), reads=[b_hsb[sl]], writes=[b_junk, b_st[t]])
        P.op("act", lambda e, t=t: e.activation(out=st[:, t, 1:2], in_=st[:, t, 0:1], func=AF.Ln, scale=1.0 / D, bias=EPS), reads=[b_st[t]], writes=[b_st[t]])
        P.op("act", lambda e, t=t: e.activation(out=st[:, t, 2:3], in_=st[:, t, 1:2], func=AF.Exp, scale=-0.5), reads=[b_st[t]], writes=[b_st[t]])
        P.op("dve", lambda e, sl=sl, t=t, hn32=hn32: e.scalar_tensor_tensor(out=hn32, in0=hsb[sl], scalar=st[:, t, 2:3], in1=gain2, op0=ALU.mult, op1=ALU.mult),
             reads=[b_hsb[sl], b_st[t], b_gain2], writes=[b_hn32])
        P.op("act", lambda e, sl=sl, hn32=hn32: e.copy(out=hnb[sl], in_=hn32), reads=[b_hn32], writes=[b_hnb[sl]])
        P.op("sp", lambda e, sl=sl, ts_=ts_: e.dma_start(out=HN[ts_, :], in_=hnb[sl]), reads=[b_hnb[sl]], pwrites=[b_HN], dma="hnst%d" % sl)
        for half in range(2):
            bk = 3 + half
            pv = banks[bk][:].rearrange("p (a b) -> p a b", b=128)
            for j in range(4):
                dc = 4 * half + j
                P.op("pe", lambda e, pv=pv, j=j, dc=dc, hn32=hn32: e.transpose(pv[:, j, :], hn32[:, dc * 128:(dc + 1) * 128], cn["ident_f"]),
                     reads=[b_hn32, b_cn["ident_f"]], **({"writes": [bbank[bk]]} if j == 0 else {"pwrites": [bbank[bk]]}))
            P.op("act", lambda e, pv=pv, half=half, hnT=hnT: e.copy(out=hnT[:, 4 * half:4 * half + 4, :], in_=pv), reads=[bbank[bk]],
                 **({"writes": [b_hnT]} if half == 0 else {"pwrites": [b_hnT]}))
        for dc in range(8):
            P.op("pe", lambda e, dc=dc, hnT=hnT: e.matmul(banks[5][:, 0:36], lhsT=hnT[:, dc, :], rhs=wr[:, dc, :], start=(dc == 0), stop=(dc == 7)),
                 reads=[b_hnT, b_wr], **({"writes": [bbank[5]]} if dc == 0 else {"pwrites": [bbank[5]]}))
        j8 = t % TB
        if j8 == 0:
            bLG = P.buf("LG%d" % (t // TB))
        P.op("dve", lambda e, j8=j8: e.tensor_tensor(out=LG[:, j8, :], in0=banks[5][:, 0:36], in1=br, op=ALU.add),
             reads=[bbank[5], b_br], **({"writes": [bLG]} if j8 == 0 else {"pwrites": [bLG]}))
        if j8 != TB - 1:
            continue
        t0 = t - (TB - 1)
        bR = P.buf("RB%d" % (t // TB))

        def dv(fn, reads=(), writes=(), eng="dve"):
            P.op(eng, fn, reads=[bR, bLG] + list(reads), writes=[bR] + list(writes))
        gl = LG[:, :, 0:4]
        bc3 = lambda ap2, n: ap2.unsqueeze(2).to_broadcast([128, TB, n])
        dv(lambda e: e.tensor_reduce(out=gmax, in_=gl, axis=AX.X, op=ALU.max))
        dv(lambda e: e.tensor_tensor(out=goh, in0=gl, in1=bc3(gmax, 4), op=ALU.is_equal))
        dv(lambda e: e.tensor_tensor(out=gsh, in0=gl, in1=bc3(gmax, 4), op=ALU.subtract))
        dv(lambda e: e.activation(out=gsh, in_=gsh, func=AF.Exp), eng="act")
        dv(lambda e: e.tensor_reduce(out=gsum, in_=gsh, axis=AX.X, op=ALU.add))
        dv(lambda e: e.reciprocal(out=ggate, in_=gsum))
        for g in range(4):
            dv(lambda e, g=g: e.tensor_tensor(out=T32[:, :, g * 8:(g + 1) * 8], in0=LG[:, :, 4 + g * 8:12 + g * 8],
                                              in1=goh[:, :, g:g + 1].to_broadcast([128, TB, 8]), op=ALU.mult))
        dv(lambda e: e.tensor_reduce(out=sel, in_=T32.rearrange("p t (g j) -> p t j g", j=8), axis=AX.X, op=ALU.add))
        dv(lambda e: e.tensor_reduce(out=m1_, in_=sel, axis=AX.X, op=ALU.max))
        dv(lambda e: e.tensor_tensor(out=oh1, in0=sel, in1=bc3(m1_, 8), op=ALU.is_equal))
        dv(lambda e: e.scalar_tensor_tensor(out=sel2, in0=oh1, scalar=-1.0e30, in1=sel, op0=ALU.mult, op1=ALU.add))
        dv(lambda e: e.tensor_reduce(out=m2_, in_=sel2, axis=AX.X, op=ALU.max))
        dv(lambda e: e.tensor_tensor(out=oh2, in0=sel2, in1=bc3(m2_, 8), op=ALU.is_equal))
        dv(lambda e: e.tensor_tensor(out=dd, in0=m2_, in1=m1_, op=ALU.subtract))
        dv(lambda e: e.activation(out=ee, in_=dd, func=AF.Exp), eng="act")
        dv(lambda e: e.tensor_scalar(out=den, in0=ee, scalar1=1.0, scalar2=None, op0=ALU.add))
        dv(lambda e: e.reciprocal(out=p1, in_=den))
        dv(lambda e: e.tensor_tensor(out=p2, in0=ee, in1=p1, op=ALU.mult))
        P.op("dve", lambda e, t0=t0: e.tensor_tensor(out=gates[:, t0:t0 + TB, 0], in0=p1, in1=ggate, op=ALU.mult), reads=[bR], pwrites=[b_gates])
        P.op("dve", lambda e, t0=t0: e.tensor_tensor(out=gates[:, t0:t0 + TB, 1], in0=p2, in1=ggate, op=ALU.mult), reads=[bR], pwrites=[b_gates])
        for g in range(4):
            dv(lambda e, g=g: e.tensor_tensor(out=E1[:, :, g * 8:(g + 1) * 8], in0=oh1, in1=goh[:, :, g:g + 1].to_broadcast([128, TB, 8]), op=ALU.mult))
            dv(lambda e, g=g: e.tensor_tensor(out=E2[:, :, g * 8:(g + 1) * 8], in0=oh2, in1=goh[:, :, g:g + 1].to_broadcast([128, TB, 8]), op=ALU.mult))
        dv(lambda e: e.tensor_tensor(out=Osum, in0=E1, in1=E2, op=ALU.add))
        Of = Osum.rearrange("p t e -> p (t e)")
        P.op("pe", lambda e, Of=Of: e.matmul(banks[6][:, 0:TB * 32], lhsT=cn["Ltri"], rhs=Of, start=True, stop=True),
             reads=[bR, b_cn["Ltri"]], writes=[bbank[6]])
        P.op("pe", lambda e, Of=Of: e.matmul(banks[7][:, 0:TB * 32], lhsT=cn["ones_f"], rhs=Of, start=True, stop=True),
             reads=[bR, b_cn["ones_f"]], writes=[bbank[7]])
        cs3 = banks[7][:, 0:TB * 32].rearrange("p (t e) -> p t e", e=32)
        dv(lambda e: e.tensor_copy(out=base[:, 0, :], in_=Ocum), reads=[b_Ocum])
        for jj in range(1, TB):
            dv(lambda e, jj=jj: e.tensor_tensor(out=base[:, jj, :], in0=base[:, jj - 1, :], in1=cs3[:, jj - 1, :], op=ALU.add), reads=[bbank[7]])
        P.op("dve", lambda e: e.tensor_tensor(out=Ocum, in0=base[:, TB - 1, :], in1=cs3[:, TB - 1, :], op=ALU.add), reads=[bR, bbank[7]], writes=[b_Ocum])
        dv(lambda e: e.tensor_tensor(out=base, in0=base, in1=banks[6][:, 0:TB * 32].rearrange("p (t e) -> p t e", e=32), op=ALU.add), reads=[bbank[6]])
        for k, Ek in ((0, E1), (1, E2)):
            dv(lambda e, Ek=Ek: e.tensor_tensor(out=T32, in0=Ek, in1=base, op=ALU.mult))
            dv(lambda e, k=k: e.tensor_reduce(out=rk[:, :, k], in_=T32, axis=AX.X, op=ALU.add))
            dv(lambda e, Ek=Ek: e.tensor_tensor(out=T32, in0=Ek, in1=cn["eidx"].unsqueeze(1).to_broadcast([128, TB, 32]), op=ALU.mult), reads=[b_cn["eidx"]])
            dv(lambda e, k=k: e.tensor_reduce(out=eid[:, :, k], in_=T32, axis=AX.X, op=ALU.add))
        dv(lambda e: e.tensor_scalar(out=vv, in0=rk, scalar1=float(CAP), scalar2=None, op0=ALU.is_lt))
        dv(lambda e: e.scalar_tensor_tensor(out=dst, in0=eid, scalar=float(CAP), in1=rk, op0=ALU.mult, op1=ALU.add))
        dv(lambda e: e.scalar_tensor_tensor(out=dst, in0=dst, scalar=-float(TRASH), in1=vv, op0=ALU.add, op1=ALU.mult))
        dv(lambda e: e.tensor_scalar(out=dst, in0=dst, scalar1=float(TRASH), scalar2=None, op0=ALU.add))
        P.op("dve", lambda e, t0=t0: e.tensor_copy(out=dest[:, t0:t0 + TB, :], in_=dst), reads=[bR], pwrites=[b_dest])
        for tt_ in range(t0, t0 + TB):
            for k in range(2):
                P.op("pool", lambda e, tt_=tt_, k=k: e.indirect_dma_start(
                    out=SLOT_TOK[:, :], out_offset=bass.IndirectOffsetOnAxis(ap=dest[:, tt_, k:k + 1], axis=0),
                    in_=cn["tokid"][:, tt_:tt_ + 1], in_offset=None),
                    reads=[b_dest, b_cn["tokid"], b_slot], pwrites=[b_slot], dma="scat")
    P.barrier()
    A.release(m0)


def phase_F(K, banks, bbank, ident_bf, b_ident, HN, SLOT_TOK, Y, keep):
    nc, P, A, cst, ins = K.nc, K.P, K.A, K.cst, K.ins
    m0 = A.mark()
    b_HN, b_slot, b_Y = keep["b_HN"], keep["b_slot"], keep["b_Y"]
    NM = CAP // 128
    zf = A.alloc([D], BF16)
    b_zf = P.buf("zf")
    P.op("pool", lambda e: e.memset(zf, 0.0), writes=[b_zf])
    P.op("sp", lambda e: e.dma_start(out=Y[NSLOT:NSLOT + 128, :], in_=zf), reads=[b_zf], pwrites=[b_Y], dma="yz")
    NW = 3
    Wg = [A.alloc([8, FF], BF16) for _ in range(NW)]
    Wu = [A.alloc([8, FF], BF16) for _ in range(NW)]
    Wd = [A.alloc([4, D], BF16) for _ in range(NW)]
    b_Wg, b_Wu, b_Wd = P.bufs("Wg", NW), P.bufs("Wu", NW), P.bufs("Wd", NW)
    idx = [A.alloc([NM], I32) for _ in range(NW)]
    b_idx = P.bufs("idx", NW)
    Xe = [A.alloc([D], BF16) for _ in range(NM)]
    b_Xe = P.bufs("Xe", NM)
    XeT = [A.alloc([8, CAP], BF16) for _ in range(2)]
    b_XeT = P.bufs("XeT", 2)
    sgt = [A.alloc([CAP], F32) for _ in range(2)]
    b_sgt = P.bufs("sgt", 2)
    hT = [A.alloc([4, CAP], BF16) for _ in range(2)]
    b_hT = P.bufs("hT", 2)
    ysb = [A.alloc([D], BF16) for _ in range(2)]
    b_ysb = P.bufs("ysb", 2)
    wg_v = K.WEB["g"].rearrange("e (dc p) f -> e p dc f", p=128)
    wu_v = K.WEB["u"].rearrange("e (dc p) f -> e p dc f", p=128)
    wd_v = K.WEB["d"].rearrange("e (fc p) d -> e p fc d", p=128)
    slot_v = SLOT_TOK[0:NSLOT, :].rearrange("(e p m) o -> e p (m o)", p=128, m=NM)
    y_v = Y[0:NSLOT, :].rearrange("(e p m) d -> e p m d", p=128, m=NM)
    xi = 0
    yi = 0
    gi = 0
    xslot = {}

    def stage_load(ex):
        ww = ex % NW
        P.op("sp", lambda e: e.dma_start(out=Wg[ww], in_=wg_v[ex]), reads=[K.b_web], writes=[b_Wg[ww]], dma="wg%d" % ww)
        P.op("sp", lambda e: e.dma_start(out=Wu[ww], in_=wu_v[ex]), reads=[K.b_web], writes=[b_Wu[ww]], dma="wu%d" % ww)
        P.op("sp", lambda e: e.dma_start(out=Wd[ww], in_=wd_v[ex]), reads=[K.b_web], writes=[b_Wd[ww]], dma="wd%d" % ww)
        P.op("sp", lambda e: e.dma_start(out=idx[ww], in_=slot_v[ex]), reads=[b_slot], writes=[b_idx[ww]], dma="idx%d" % ww)

    def stage_gather(ex):
        ww = ex % NW
        for m in range(NM):
            P.op("pool", lambda e, ww=ww, m=m: e.indirect_dma_start(
                out=Xe[m], out_offset=None, in_=HN[:, :], in_offset=bass.IndirectOffsetOnAxis(ap=idx[ww][:, m:m + 1], axis=0)),
                reads=[b_idx[ww], b_HN], writes=[b_Xe[m]], dma="gx%d" % m)

    def stage_transpose(ex):
        ws = ex % 2
        for m in range(NM):
            bk = m % 2
            pb = banks[bk][:].bitcast(BF16).rearrange("p (a b) -> p a b", b=128)
            for dc in range(8):
                P.op("pe", lambda e, pb=pb, dc=dc, m=m: e.transpose(pb[:, dc, :], Xe[m][:, dc * 128:(dc + 1) * 128], ident_bf),
                     reads=[b_Xe[m], b_ident], **({"writes": [bbank[bk]]} if dc == 0 else {"pwrites": [bbank[bk]]}))
            P.op("act", lambda e, pb=pb, ws=ws, m=m: e.copy(out=XeT[ws][:, :, m * 128:(m + 1) * 128], in_=pb), reads=[bbank[bk]],
                 **({"writes": [b_XeT[ws]]} if m == 0 else {"pwrites": [b_XeT[ws]]}))

    stage_load(0)
    stage_load(1)
    stage_gather(0)
    stage_transpose(0)
    for ex in range(NEXP):
        ws = ex % 2
        ww = ex % NW
        if ex + 2 < NEXP:
            stage_load(ex + 2)
        if ex + 1 < NEXP:
            stage_gather(ex + 1)
        for fc in range(4):
            bg = 2 + (gi % 2)
            bu = 4 + (gi % 2)
            s2 = gi % 2
            gi += 1
            for dc in range(8):
                P.op("pe", lambda e, bg=bg, ws=ws, ww=ww, dc=dc, fc=fc: e.matmul(banks[bg][:, 0:CAP], lhsT=Wg[ww][:, dc, fc * 128:(fc + 1) * 128], rhs=XeT[ws][:, dc, :],
                                                                         start=(dc == 0), stop=(dc == 7)),
                     reads=[b_Wg[ww], b_XeT[ws]], **({"writes": [bbank[bg]]} if dc == 0 else {"pwrites": [bbank[bg]]}))
            for dc in range(8):
                P.op("pe", lambda e, bu=bu, ws=ws, ww=ww, dc=dc, fc=fc: e.matmul(banks[bu][:, 0:CAP], lhsT=Wu[ww][:, dc, fc * 128:(fc + 1) * 128], rhs=XeT[ws][:, dc, :],
                                                                         start=(dc == 0), stop=(dc == 7)),
                     reads=[b_Wu[ww], b_XeT[ws]], **({"writes": [bbank[bu]]} if dc == 0 else {"pwrites": [bbank[bu]]}))
            P.op("act", lambda e, bg=bg, s2=s2: e.activation(out=sgt[s2], in_=banks[bg][:, 0:CAP], func=AF.Silu), reads=[bbank[bg]], writes=[b_sgt[s2]])
            P.op("dve", lambda e, bu=bu, s2=s2, ws=ws, fc=fc: e.tensor_tensor(out=hT[ws][:, fc, :], in0=sgt[s2], in1=banks[bu][:, 0:CAP], op=ALU.mult),
                 reads=[b_sgt[s2], bbank[bu]], **({"writes": [b_hT[ws]]} if fc == 0 else {"pwrites": [b_hT[ws]]}))
        if ex + 1 < NEXP:
            stage_transpose(ex + 1)
        for m in range(NM):
            y2 = yi % 2
            yi += 1
            for half in range(2):
                by_ = 6 + half
                for fc in range(4):
                    P.op("pe", lambda e, by_=by_, ws=ws, ww=ww, fc=fc, m=m, half=half: e.matmul(
                        banks[by_][:], lhsT=hT[ws][:, fc, m * 128:(m + 1) * 128], rhs=Wd[ww][:, fc, half * 512:(half + 1) * 512], start=(fc == 0), stop=(fc == 3)),
                        reads=[b_hT[ws], b_Wd[ww]], **({"writes": [bbank[by_]]} if fc == 0 else {"pwrites": [bbank[by_]]}))
                if half == 0:
                    P.op("act", lambda e, by_=by_, y2=y2: e.copy(out=ysb[y2][:, 0:512], in_=banks[by_][:]), reads=[bbank[by_]], writes=[b_ysb[y2]])
                else:
                    P.op("dve", lambda e, by_=by_, y2=y2: e.tensor_copy(out=ysb[y2][:, 512:1024], in_=banks[by_][:]), reads=[bbank[by_]], pwrites=[b_ysb[y2]])
            P.op("sp", lambda e, ex=ex, m=m, y2=y2: e.dma_start(out=y_v[ex][:, m, :], in_=ysb[y2]), reads=[b_ysb[y2]], pwrites=[b_Y], dma="yst%d" % y2)
    P.barrier()
    A.release(m0)


def phase_G(K, H, Y, y, keep):
    nc, P, A, cst, ins = K.nc, K.P, K.A, K.cst, K.ins
    m0 = A.mark()
    gates, dest = keep["gates"], keep["dest"]
    b_gates, b_dest, b_H, b_Y = keep["b_gates"], keep["b_dest"], keep["b_H"], keep["b_Y"]
    fg = A.alloc([D], F32)
    b_fg = P.buf("fg")
    P.op("sp", lambda e: e.dma_start(out=fg, in_=ins["gain3_bc"]), writes=[b_fg], dma="c_gain3")
    hs_ = [A.alloc([D], F32) for _ in range(3)]
    y1 = [A.alloc([D], BF16) for _ in range(3)]
    y2 = [A.alloc([D], BF16) for _ in range(3)]
    ot = [A.alloc([D], F32) for _ in range(3)]
    b_hs, b_y1, b_y2, b_ot = P.bufs("ghs", 3), P.bufs("gy1", 3), P.bufs("gy2", 3), P.bufs("got", 3)
    junk = A.alloc([D], F32)
    b_junk = P.buf("gjunk")
    st = A.alloc([NT, 4], F32)
    b_st = P.bufs("gst", NT)
    def g_part1(t):
        sl = t % 3
        ts_ = slice(t * 128, (t + 1) * 128)
        P.op("sp", lambda e, sl=sl, ts_=ts_: e.dma_start(out=hs_[sl], in_=H[ts_, :]), reads=[b_H], writes=[b_hs[sl]], dma="gh%d" % sl)
        for k, (yy, bb) in enumerate(((y1, b_y1), (y2, b_y2))):
            P.op("pool", lambda e, sl=sl, t=t, k=k, yy=yy: e.indirect_dma_start(
                out=yy[sl], out_offset=None, in_=Y[:, :], in_offset=bass.IndirectOffsetOnAxis(ap=dest[:, t, k:k + 1], axis=0)), reads=[b_dest, b_Y], writes=[bb[sl]], dma="gy%d_%d" % (k, sl))
        P.op("dve", lambda e, sl=sl, t=t: e.scalar_tensor_tensor(out=ot[sl], in0=y1[sl], scalar=gates[:, t, 0:1], in1=hs_[sl], op0=ALU.mult, op1=ALU.add),
             reads=[b_y1[sl], b_gates, b_hs[sl]], writes=[b_ot[sl]])
        P.op("dve", lambda e, sl=sl, t=t: e.scalar_tensor_tensor(out=ot[sl], in0=y2[sl], scalar=gates[:, t, 1:2], in1=ot[sl], op0=ALU.mult, op1=ALU.add),
             reads=[b_y2[sl], b_gates, b_ot[sl]], writes=[b_ot[sl]])
        P.op("dve", lambda e, sl=sl, t=t: e.scalar_tensor_tensor(out=junk, in0=ot[sl], scalar=1.0, in1=ot[sl], op0=ALU.mult, op1=ALU.mult,
                                                             accum_out=st[:, t, 0:1]), reads=[b_ot[sl]], writes=[b_junk, b_st[t]])
        P.op("act", lambda e, t=t: e.activation(out=st[:, t, 1:2], in_=st[:, t, 0:1], func=AF.Ln, scale=1.0 / D, bias=EPS), reads=[b_st[t]], writes=[b_st[t]])
        P.op("act", lambda e, t=t: e.activation(out=st[:, t, 2:3], in_=st[:, t, 1:2], func=AF.Exp, scale=-0.5), reads=[b_st[t]], writes=[b_st[t]])

    def g_part2(t):
        sl = t % 3
        ts_ = slice(t * 128, (t + 1) * 128)
        P.op("dve", lambda e, sl=sl, t=t: e.scalar_tensor_tensor(out=ot[sl], in0=ot[sl], scalar=st[:, t, 2:3], in1=fg, op0=ALU.mult, op1=ALU.mult),
             reads=[b_ot[sl], b_st[t], b_fg], writes=[b_ot[sl]])
        P.op("sp", lambda e, sl=sl, ts_=ts_: e.dma_start(out=y[ts_, :], in_=ot[sl]), reads=[b_ot[sl]], dma="yout%d" % sl)

    g_part1(0)
    for t in range(NT):
        if t + 1 < NT:
            g_part1(t + 1)
        g_part2(t)
    A.release(m0)


_CACHE = {}


def kernel(**inputs):
    x = np.ascontiguousarray(np.asarray(inputs["x"], dtype=np.float32))
    dbg = tuple(sorted(k for k, v in DEBUG.items() if v))
    if dbg not in _CACHE:
        _CACHE[dbg] = build(dbg)
    nc, K = _CACHE[dbg]
    cst = _consts()
    common = {
        "w_in": np.ascontiguousarray(inputs["w_in"][0]),
        "gain1_bc": np.ascontiguousarray(np.broadcast_to(inputs["mix_norm_gain"][0][None, :], (128, D))),
    }
    df = np.asarray(inputs["ret_decay_fwd"][0], np.float32)
    db = np.asarray(inputs["ret_decay_bwd"][0], np.float32)
    dec = np.zeros((128, 24), np.float32)
    dec[:, 0:8] = df[None, :]
    dec[:, 8:16] = db[None, :]
    for hp in range(4):
        dec[0:64, 16 + hp] = df[2 * hp]
        dec[64:128, 16 + hp] = df[2 * hp + 1]
        dec[0:64, 20 + hp] = db[2 * hp]
        dec[64:128, 20 + hp] = db[2 * hp + 1]
    common["dec_bc"] = dec
    f32 = lambda a: np.ascontiguousarray(np.asarray(a, dtype=np.float32))
    bc = lambda v: np.ascontiguousarray(np.broadcast_to(np.asarray(v, np.float32)[None, :], (128, len(v))))
    common["w_out"] = f32(inputs["w_out"][0])
    gcat = np.concatenate([np.asarray(inputs["attn_out_gain"][0], np.float32), np.asarray(inputs["ret_out_gain"][0], np.float32)])
    common["gmix"] = np.ascontiguousarray(gcat.reshape(8, 128).T)
    common["gain2_bc"] = bc(inputs["ffn_norm_gain"][0])
    common["gain3_bc"] = bc(inputs["final_norm_gain"])
    common["br_bc"] = bc(np.concatenate([np.asarray(inputs["b_route_group"][0], np.float32), np.asarray(inputs["b_route_expert"][0], np.float32)]))
    common["wr"] = np.ascontiguousarray(np.concatenate([np.asarray(inputs["w_route_group"][0], np.float32),
                                                        np.asarray(inputs["w_route_expert"][0], np.float32)], axis=1))
    common["w_eg"] = f32(inputs["w_expert_gate"][0])
    common["w_eu"] = f32(inputs["w_expert_up"][0])
    common["w_ed"] = f32(inputs["w_expert_down"][0])
    common.update(cst)
    in_maps = []
    ncores = LIMIT.get("ncores", 8)
    for b in range(ncores):
        m = {"x": x[b]}
        m.update(common)
        in_maps.append({k: m[k] for k in K.in_names})
    res = run_bass_kernel_spmd(nc, in_maps, core_ids=list(range(ncores)))
    kernel.last = res
    out = np.stack([np.asarray(r["y"]) for r in res.results] + [np.zeros((S, D), np.float32)] * (8 - ncores), axis=0).astype(np.float32)
    return out
```
